# Optimizing a Trainium2 kernel written in Bass

```python
import math
import jax, jax.numpy as jnp
from jax import lax
import numpy as np

D_MODEL = 1024
BATCH = 16
SEQ = 2048
DEPTH = 4

GRID_W = 64
CTX_LEN = 256

N_HEADS_NA = 8
HEAD_DIM_NA = 64
D_NA = N_HEADS_NA * HEAD_DIM_NA
WIN_R = 8
WIN_C = 16
Q_BLK_C = 16
HALO_C = Q_BLK_C + WIN_C

N_HEADS_SSD = 8
HEAD_DIM_SSD = 64
D_SSD = N_HEADS_SSD * HEAD_DIM_SSD
N_GROUPS_SSD = 2
D_STATE = 128
D_CONV = 5
CHUNK = 128
DT_MIN = 1e-3
DT_MAX = 1e-1
ROPE_BASE = 10000.0

D_MIX = D_NA + D_SSD
GN = N_GROUPS_SSD * D_STATE
CONV_CH = D_SSD + 2 * GN
OFF_K = D_NA
OFF_V = 2 * D_NA
OFF_Z = 3 * D_NA
OFF_XBC = OFF_Z + D_SSD
OFF_DT = OFF_XBC + CONV_CH
D_IN_PROJ = OFF_DT + 2 * N_HEADS_SSD

D_FF = 2816
N_EXPERTS = 8
TOP_K = 2
D_FF_EXPERT = 3584
N_DENSE = (DEPTH + 1) // 2
N_MOE = DEPTH // 2

DEEPNORM_ALPHA = (2.0 * DEPTH) ** 0.25
DEEPNORM_BETA = (8.0 * DEPTH) ** -0.25
LN_EPS = 1e-5
RMS_EPS = 1e-5

kernel_name = 'hybrid_natten_ssd_moe_deepnorm_dit'


def _layernorm(x, g, b):
    xf = x.astype(jnp.float32)
    mu = jnp.mean(xf, axis=-1, keepdims=True)
    var = jnp.mean(jnp.square(xf - mu), axis=-1, keepdims=True)
    y = (xf - mu) * lax.rsqrt(var + LN_EPS) * g.astype(jnp.float32) + b.astype(jnp.float32)
    return y.astype(x.dtype)


def _split_proj(p):
    return (p[..., :OFF_K], p[..., OFF_K:OFF_V], p[..., OFF_V:OFF_Z],
            p[..., OFF_Z:OFF_XBC], p[..., OFF_XBC:OFF_DT], p[..., OFF_DT:])


def _heads(t):
    b, l, _ = t.shape
    return t.reshape(b, l, N_HEADS_NA, HEAD_DIM_NA)


def _context_attention(q, k, v):
    b, l = q.shape[0], q.shape[1]
    s = jnp.einsum('bqhd,bkhd->bhqk', q, k).astype(jnp.float32) * (HEAD_DIM_NA ** -0.5)
    p = jax.nn.softmax(s, axis=-1).astype(v.dtype)
    return jnp.einsum('bhqk,bkhd->bqhd', p, v).reshape(b, l, D_NA)


def _neighbourhood_attention(q, k, v, k_ctx, v_ctx, rpb, rows):
    b = q.shape[0]
    kr = min(WIN_R, rows)
    ncb = GRID_W // Q_BLK_C
    scale = HEAD_DIM_NA ** -0.5
    cols = np.arange(GRID_W)
    col_start = np.clip(cols - WIN_C // 2, 0, GRID_W - WIN_C).reshape(ncb, Q_BLK_C)
    halo_start = np.clip(np.arange(ncb) * Q_BLK_C - WIN_C // 2, 0, GRID_W - HALO_C)
    halo_cols = halo_start[:, None] + np.arange(HALO_C)
    key_col = halo_cols[:, None, :]
    in_win = jnp.asarray((key_col >= col_start[:, :, None]) & (key_col < col_start[:, :, None] + WIN_C))
    col_bias_idx = np.clip(key_col - cols.reshape(ncb, Q_BLK_C)[:, :, None] + WIN_C - 1, 0, 2 * WIN_C - 2)
    qg = q.reshape(b, rows, ncb, Q_BLK_C, N_HEADS_NA, HEAD_DIM_NA)
    kg = k.reshape(b, rows, GRID_W, N_HEADS_NA, HEAD_DIM_NA)
    vg = v.reshape(b, rows, GRID_W, N_HEADS_NA, HEAD_DIM_NA)
    n_loc = kr * HALO_C

    def row_block(i):
        r0 = jnp.clip(i - kr // 2, 0, rows - kr)
        k_rows = lax.dynamic_slice_in_dim(kg, r0, kr, axis=1)
        v_rows = lax.dynamic_slice_in_dim(vg, r0, kr, axis=1)
        k_halo = k_rows[:, :, halo_cols]
        v_halo = v_rows[:, :, halo_cols]
        q_row = lax.dynamic_index_in_dim(qg, i, axis=1, keepdims=False)
        s_loc = jnp.einsum('bcqhd,brckhd->bhcqrk', q_row, k_halo).astype(jnp.float32) * scale
        row_bias_idx = r0 + jnp.arange(kr) - i + WIN_R - 1
        bias = rpb[:, row_bias_idx][:, :, col_bias_idx].transpose(0, 2, 3, 1, 4)
        s_loc = jnp.where(in_win[:, :, None, :], s_loc + bias.astype(jnp.float32), -jnp.inf)
        s_ctx = jnp.einsum('bcqhd,bmhd->bhcqm', q_row, k_ctx).astype(jnp.float32) * scale
        s = jnp.concatenate([s_loc.reshape(s_loc.shape[:4] + (n_loc,)), s_ctx], axis=-1)
        p = jax.nn.softmax(s, axis=-1).astype(v.dtype)
        p_loc = p[..., :n_loc].reshape(s_loc.shape)
        o = (jnp.einsum('bhcqrk,brckhd->bcqhd', p_loc, v_halo)
             + jnp.einsum('bhcqm,bmhd->bcqhd', p[..., n_loc:], v_ctx))
        return o.reshape(b, GRID_W, N_HEADS_NA, HEAD_DIM_NA)

    out = lax.map(row_block, jnp.arange(rows))
    return out.transpose(1, 0, 2, 3, 4).reshape(b, rows * GRID_W, D_NA)


def _axial_rope(u):
    l = u.shape[1]
    t = jnp.arange(l)
    half = D_STATE // 2
    quarter = half // 2
    inv_freq = ROPE_BASE ** (-jnp.arange(quarter, dtype=jnp.float32) / quarter)
    ang_r = (t // GRID_W).astype(jnp.float32)[:, None] * inv_freq
    ang_c = (t % GRID_W).astype(jnp.float32)[:, None] * inv_freq

    def rot(w, ang):
        cos = jnp.cos(ang)[:, None, :].astype(w.dtype)
        sin = jnp.sin(ang)[:, None, :].astype(w.dtype)
        w1, w2 = w[..., :quarter], w[..., quarter:]
        return jnp.concatenate([w1 * cos - w2 * sin, w1 * sin + w2 * cos], axis=-1)

    return jnp.concatenate([rot(u[..., :half], ang_r), rot(u[..., half:], ang_c)], axis=-1)


def _dwconv(u, w, bias):
    k = w.shape[0]
    y = lax.conv_general_dilated(u, w[:, None, :].astype(u.dtype), window_strides=(1,),
                                 padding=[(k // 2, k // 2)], dimension_numbers=('NWC', 'WIO', 'NWC'),
                                 feature_group_count=u.shape[-1])
    return y + bias


def _segsum(a):
    t = a.shape[-1]
    cs = jnp.cumsum(a, axis=-1)
    diff = cs[..., :, None] - cs[..., None, :]
    return jnp.where(jnp.tril(jnp.ones((t, t), dtype=bool)), diff, -jnp.inf)


def _ssd_chunked(xh, da, bm, cm, h0):
    b, l, nh, p = xh.shape
    g, n = bm.shape[2], bm.shape[3]
    r = nh // g
    nc = l // CHUNK
    dtp = xh.dtype
    xc = xh.reshape(b, nc, CHUNK, g, r, p)
    bc = bm.reshape(b, nc, CHUNK, g, n)
    cc = cm.reshape(b, nc, CHUNK, g, n)
    a = da.reshape(b, nc, CHUNK, g, r).transpose(0, 3, 4, 1, 2)
    a_cs = jnp.cumsum(a, axis=-1)
    decay_in = jnp.exp(_segsum(a)).astype(dtp)
    cb = jnp.einsum('bclgn,bcsgn->bgcls', cc, bc)
    y_diag = jnp.einsum('bgrcls,bcsgrp->bclgrp', cb[:, :, None] * decay_in, xc)
    to_end = jnp.exp(a_cs[..., -1:] - a_cs).astype(dtp).transpose(0, 3, 4, 1, 2)[..., None]
    states = jnp.einsum('bclgn,bclgrp->bcgrpn', bc, xc * to_end)
    states = jnp.concatenate([h0.reshape(b, 1, g, r, p, n).astype(dtp), states], axis=1)
    totals = jnp.pad(a_cs[..., -1], ((0, 0), (0, 0), (0, 0), (1, 0)))
    decay_ch = jnp.exp(_segsum(totals)).astype(dtp)
    carried = jnp.einsum('bgrzc,bcgrpn->bzgrpn', decay_ch, states)
    from_start = jnp.exp(a_cs).astype(dtp).transpose(0, 3, 4, 1, 2)[..., None]
    y_off = jnp.einsum('bclgn,bcgrpn->bclgrp', cc, carried[:, :-1]) * from_start
    y = (y_diag + y_off).reshape(b, l, nh, p)
    return y, carried[:, -1].reshape(b, nh, p, n)


def _ssd_stream(z, xbc, dt_raw, conv_w, conv_b, dt_bias, a_log, d_skip, norm_w, h0_f, h0_b, rotary):
    b, l, _ = xbc.shape
    u = jax.nn.silu(_dwconv(xbc, conv_w, conv_b))
    xs = u[..., :D_SSD].reshape(b, l, N_HEADS_SSD, HEAD_DIM_SSD)
    bm = u[..., D_SSD:D_SSD + GN].reshape(b, l, N_GROUPS_SSD, D_STATE)
    cm = u[..., D_SSD + GN:].reshape(b, l, N_GROUPS_SSD, D_STATE)
    if rotary:
        bm = _axial_rope(bm)
        cm = _axial_rope(cm)
    dt = jax.nn.softplus(dt_raw.astype(jnp.float32).reshape(b, l, 2, N_HEADS_SSD) + dt_bias.astype(jnp.float32))
    da = dt * (-jnp.exp(a_log.astype(jnp.float32)))
    dtc = dt.astype(xs.dtype)
    y_f, h_f = _ssd_chunked(xs * dtc[:, :, 0, :, None], da[:, :, 0], bm, cm, h0_f)
    fl = lambda t: jnp.flip(t, axis=1)
    y_b, h_b = _ssd_chunked(fl(xs * dtc[:, :, 1, :, None]), fl(da[:, :, 1]), fl(bm), fl(cm), h0_b)
    y = y_f + fl(y_b) + d_skip[:, None] * xs
    yg = (y.reshape(b, l, D_SSD) * jax.nn.silu(z)).astype(jnp.float32)
    yg = yg.reshape(b, l, N_GROUPS_SSD, D_SSD // N_GROUPS_SSD)
    yg = yg * lax.rsqrt(jnp.mean(yg * yg, axis=-1, keepdims=True) + RMS_EPS)
    out = (yg.reshape(b, l, D_SSD) * norm_w.astype(jnp.float32)).astype(z.dtype)
    return out, h_f, h_b


def _swiglu(h, wg, wu, wd):
    return (jax.nn.silu(h @ wg) * (h @ wu)) @ wd


def _moe(h, router, wg, wu, wd):
    logits = (h @ router).astype(jnp.float32)
    top_v, top_i = lax.top_k(logits, TOP_K)
    gates = jax.nn.softmax(top_v, axis=-1)
    combine = jnp.sum(jax.nn.one_hot(top_i, N_EXPERTS, dtype=jnp.float32) * gates[..., None], axis=-2)
    combine = combine.astype(h.dtype)
    out = jnp.zeros_like(h)
    for e in range(N_EXPERTS):
        out = out + combine[..., e:e + 1] * _swiglu(h, wg[e], wu[e], wd[e])
    return out


def setup_inputs(seed: int = 0) -> dict:
    key = jax.random.key(seed)
    ks = jax.random.split(key, 24)
    f32 = jnp.float32
    nrm = lambda k, shape, s: jax.random.normal(k, shape, f32) * s
    dt0 = jnp.exp(jax.random.uniform(ks[11], (DEPTH, 2, N_HEADS_SSD), f32, math.log(DT_MIN), math.log(DT_MAX)))
    return {
        'x': nrm(ks[0], (BATCH, SEQ, D_MODEL), 1.0),
        'c': nrm(ks[1], (BATCH, D_MODEL), 1.0),
        'ctx': nrm(ks[2], (BATCH, CTX_LEN, D_MODEL), 1.0),
        'c_ctx': nrm(ks[3], (D_MODEL,), 1.0),
        'w_mod': nrm(ks[4], (DEPTH, D_MODEL, 6 * D_MODEL), 0.5 * D_MODEL ** -0.5),
        'b_mod': nrm(ks[5], (DEPTH, 6 * D_MODEL), 0.01),
        'w_in': nrm(ks[6], (DEPTH, D_MODEL, D_IN_PROJ), D_MODEL ** -0.5),
        'w_out': nrm(ks[7], (DEPTH, D_MIX, D_MODEL), DEEPNORM_BETA * D_MIX ** -0.5),
        'na_rpb': nrm(ks[8], (DEPTH, N_HEADS_NA, 2 * WIN_R - 1, 2 * WIN_C - 1), 0.1),
        'conv_w': nrm(ks[9], (DEPTH, D_CONV, CONV_CH), D_CONV ** -0.5),
        'conv_b': nrm(ks[10], (DEPTH, CONV_CH), 0.01),
        'dt_bias': dt0 + jnp.log(-jnp.expm1(-dt0)),
        'a_log': jnp.log(jax.random.uniform(ks[12], (DEPTH, 2, N_HEADS_SSD), f32, 1.0, 16.0)),
        'd_skip': 1.0 + nrm(ks[13], (DEPTH, N_HEADS_SSD), 0.01),
        'ssd_norm_w': 1.0 + nrm(ks[14], (DEPTH, D_SSD), 0.01),
        'ln_g': 1.0 + nrm(ks[15], (DEPTH, 2, D_MODEL), 0.01),
        'ln_b': nrm(ks[16], (DEPTH, 2, D_MODEL), 0.01),
        'ffn_w_gate': nrm(ks[17], (N_DENSE, D_MODEL, D_FF), D_MODEL ** -0.5),
        'ffn_w_up': nrm(ks[18], (N_DENSE, D_MODEL, D_FF), D_MODEL ** -0.5),
        'ffn_w_down': nrm(ks[19], (N_DENSE, D_FF, D_MODEL), DEEPNORM_BETA * D_FF ** -0.5),
        'router_w': nrm(ks[20], (N_MOE, D_MODEL, N_EXPERTS), D_MODEL ** -0.5),
        'moe_w_gate': nrm(ks[21], (N_MOE, N_EXPERTS, D_MODEL, D_FF_EXPERT), D_MODEL ** -0.5),
        'moe_w_up': nrm(ks[22], (N_MOE, N_EXPERTS, D_MODEL, D_FF_EXPERT), D_MODEL ** -0.5),
        'moe_w_down': nrm(ks[23], (N_MOE, N_EXPERTS, D_FF_EXPERT, D_MODEL), DEEPNORM_BETA * D_FF_EXPERT ** -0.5),
    }


def reference(x, c, ctx, c_ctx, w_mod, b_mod, w_in, w_out, na_rpb, conv_w, conv_b, dt_bias, a_log,
              d_skip, ssd_norm_w, ln_g, ln_b, ffn_w_gate, ffn_w_up, ffn_w_down, router_w,
              moe_w_gate, moe_w_up, moe_w_down):
    b, seq, _ = x.shape
    rows = seq // GRID_W
    xc = ctx
    for layer in range(DEPTH):
        last = layer == DEPTH - 1
        mod = (jax.nn.silu(c) @ w_mod[layer] + b_mod[layer])[:, None, :]
        mod_c = (jax.nn.silu(c_ctx) @ w_mod[layer] + b_mod[layer])[None, None, :]
        sh1, sc1, g1, sh2, sc2, g2 = jnp.split(mod, 6, axis=-1)
        sh1c, sc1c, g1c, sh2c, sc2c, g2c = jnp.split(mod_c, 6, axis=-1)
        q, k, v, z, xbc, dtr = _split_proj((x * (1.0 + sc1) + sh1) @ w_in[layer])
        qc, kc, vc, zc, xbcc, dtrc = _split_proj((xc * (1.0 + sc1c) + sh1c) @ w_in[layer])
        kc_h, vc_h = _heads(kc), _heads(vc)
        a_lat = _neighbourhood_attention(_heads(q), _heads(k), _heads(v), kc_h, vc_h, na_rpb[layer], rows)
        h_zero = jnp.zeros((b, N_HEADS_SSD, HEAD_DIM_SSD, D_STATE), x.dtype)
        s_ctx, hf, hb = _ssd_stream(zc, xbcc, dtrc, conv_w[layer], conv_b[layer], dt_bias[layer], a_log[layer],
                                    d_skip[layer], ssd_norm_w[layer], h_zero, h_zero, False)
        s_lat, _, _ = _ssd_stream(z, xbc, dtr, conv_w[layer], conv_b[layer], dt_bias[layer], a_log[layer],
                                  d_skip[layer], ssd_norm_w[layer], hf, hb, True)
        mix = jnp.concatenate([a_lat, s_lat], axis=-1) @ w_out[layer]
        x = _layernorm(DEEPNORM_ALPHA * x + (1.0 + g1) * mix, ln_g[layer, 0], ln_b[layer, 0])
        h2 = x * (1.0 + sc2) + sh2
        if layer % 2 == 0:
            wg, wu, wd = ffn_w_gate[layer // 2], ffn_w_up[layer // 2], ffn_w_down[layer // 2]
            chan = lambda t: _swiglu(t, wg, wu, wd)
        else:
            rw, eg, eu, ed = router_w[layer // 2], moe_w_gate[layer // 2], moe_w_up[layer // 2], moe_w_down[layer // 2]
            chan = lambda t: _moe(t, rw, eg, eu, ed)
        x = _layernorm(DEEPNORM_ALPHA * x + (1.0 + g2) * chan(h2), ln_g[layer, 1], ln_b[layer, 1])
        if not last:
            a_c = _context_attention(_heads(qc), kc_h, vc_h)
            mix_c = jnp.concatenate([a_c, s_ctx], axis=-1) @ w_out[layer]
            xc = _layernorm(DEEPNORM_ALPHA * xc + (1.0 + g1c) * mix_c, ln_g[layer, 0], ln_b[layer, 0])
            xc = _layernorm(DEEPNORM_ALPHA * xc + (1.0 + g2c) * chan(xc * (1.0 + sc2c) + sh2c),
                            ln_g[layer, 1], ln_b[layer, 1])
    return x
```

```python
from contextlib import ExitStack
import numpy as np
import concourse.bass as bass
import concourse.mybir as mybir
from concourse.bass_utils import run_bass_kernel_spmd

F32 = mybir.dt.float32
BF16 = mybir.dt.bfloat16
ALU = mybir.AluOpType
AF = mybir.ActivationFunctionType
AX = mybir.AxisListType

EPOCH = 30000
NB, L, C, T, NT = 2, 2048, 256, 2304, 18
BLKS = [(0, 512), (512, 512), (1024, 512), (1536, 512), (2048, 256)]
ALPHA = (2.0 * 4) ** 0.25
NEG = -30000.0
C_ID, C_ONE, C_TRF, C_TRB, C_MNF, C_MNB, C_PM, C_SEL = 0, 128, 256, 384, 512, 640, 768, 896
C_MN4F, C_MN4B = 1920, 2432
NCONST = 896 + 1024 + 1024
P_BMOD, P_CW, P_CB, P_DTB, P_ALOG, P_DSK, P_NW, P_LNG, P_LNB = 0, 48, 88, 96, 112, 128, 136, 648, 664
NP_ = 680


class Buf:
    __slots__ = ("name", "w", "r", "dsem", "dcount")

    def __init__(self, name="b"):
        self.name = name
        self.w = {}
        self.r = {}
        self.dsem = None
        self.dcount = 0


class Sched:
    def __init__(self, nc):
        self.nc = nc
        self.engs = {"pe": nc.tensor, "act": nc.scalar, "dve": nc.vector, "pool": nc.gpsimd, "sp": nc.sync}
        self.sem, self.cnt = {}, {}
        self.seq = {e: 0 for e in self.engs}
        self.seen = {e: {} for e in self.engs}
        self.last = {}
        self.dtoks = {}
        self.nsem = self.nwait = self.ninst = 0
        for e in self.engs:
            self._new_epoch(e)

    def _alloc_sem(self, name):
        self.nsem += 1
        return self.nc.alloc_semaphore("%s_%d" % (name, self.nsem))

    def _new_epoch(self, e):
        self.sem[e] = self._alloc_sem("s_" + e)
        self.cnt[e] = 0

    def _wait(self, eng, tok):
        key, seq, sem, val = tok
        if eng == "pe" and key == "pe":
            return
        if self.seen[eng].get(key, -1) >= seq:
            return
        self.engs[eng].wait_ge(sem, val)
        self.nwait += 1
        self.seen[eng][key] = seq

    def _deps(self, eng, reads, writes, partial):
        for b in reads:
            for t in b.w.values():
                self._wait(eng, t)
        for b in writes:
            for t in b.r.values():
                self._wait(eng, t)
            if not partial:
                for t in b.w.values():
                    self._wait(eng, t)

    def _commit(self, tok, reads, writes, partial):
        key = tok[0]
        for b in reads:
            b.r[key] = tok
        for b in writes:
            if partial and not b.r:
                b.w[key] = tok
            else:
                b.w = {key: tok}
            b.r = {}

    def op(self, eng, fn, reads=(), writes=(), partial=False):
        self._deps(eng, reads, writes, partial)
        ins = fn()
        if self.cnt[eng] >= EPOCH:
            self._new_epoch(eng)
        self.cnt[eng] += 1
        self.seq[eng] += 1
        ins.then_inc(self.sem[eng], 1)
        tok = (eng, self.seq[eng], self.sem[eng], self.cnt[eng])
        self.last[eng] = tok
        self._commit(tok, reads, writes, partial)
        self.ninst += 1
        return tok

    def dma(self, q, out, in_, reads=(), writes=(), partial=False, semb=None, **kw):
        self._deps(q, reads, writes, partial)
        ins = self.engs[q].dma_start(out=out, in_=in_, **kw)
        b = semb if semb is not None else writes[0]
        if b.dsem is None:
            b.dsem = self._alloc_sem("d_" + b.name)
        b.dcount += 16
        ins.then_inc(b.dsem, 16)
        tok = (("d", b.dsem.num), b.dcount, b.dsem, b.dcount)
        self.dtoks[tok[0]] = tok
        self._commit(tok, reads, writes, partial)
        self.ninst += 1
        return tok

    def barrier(self):
        toks = list(self.last.values()) + list(self.dtoks.values())
        for e in self.engs:
            for t in toks:
                self._wait(e, t)
        self.dtoks = {}


def build(n_layers=4, debug=False):
    nc = bass.Bass("TRN2", target_bir_lowering=False)
    S = Sched(nc)
    uid = [0]

    def D(name, shape, dt=F32, kind="ExternalInput"):
        if kind == "Internal" and debug:
            kind = "ExternalOutput"
        return nc.dram_tensor(name, shape, dt, kind=kind).ap()

    def A(st, name, shape, dt):
        uid[0] += 1
        return st.enter_context(nc.sbuf_tensor("%s_%d" % (name, uid[0]), shape, dt))[:]

    gb = {}

    def gbuf(name):
        if name not in gb:
            gb[name] = Buf(name)
        return gb[name]

    def dump(name, ap, buf, dt_=F32):
        if not debug:
            return
        o = nc.dram_tensor("dbg_" + name, list(ap.shape), dt_, kind="ExternalOutput").ap()
        S.dma("sp", o, ap, reads=[buf], writes=[gbuf("dbg_" + name)])

    x_d = D("x", [NB, L, 1024]); ctx_d = D("ctx", [NB, C, 1024]); cT_d = D("cT", [128, 8, 3])
    const_d = D("consts", [128, NCONST]); rope_d = D("rope", [128, 2, L]); prm_d = D("prm", [4, 128, NP_])
    wmod_d = D("w_mod", [4, 1024, 6144]); win_d = D("w_in", [4, 1024, 3088]); wout_d = D("w_out", [4, 1024, 1024])
    btab_d = D("btab", [4, 8, 128, 25 * 128])
    fg_d = D("ffn_w_gate", [2, 1024, 2816]); fu_d = D("ffn_w_up", [2, 1024, 2816]); fd_d = D("ffn_w_down", [2, 2816, 1024])
    rt_d = D("router", [2, 128, 8, 8])
    mg_d = D("moe_w_gate", [2, 8, 1024, 3584]); mu_d = D("moe_w_up", [2, 8, 1024, 3584]); md_d = D("moe_w_down", [2, 8, 3584, 1024])
    out_d = D("out", [NB, L, 1024], kind="ExternalOutput")
    XT = [D("XT%d" % b, [128, 8, T], kind="Internal") for b in range(NB)]
    X1T = [D("X1T%d" % b, [128, 8, T], kind="Internal") for b in range(NB)]
    QT = [D("QT%d" % b, [128, 4, T], BF16, kind="Internal") for b in range(NB)]
    KT = [D("KT%d" % b, [128, 4, T], BF16, kind="Internal") for b in range(NB)]
    VV = [D("VV%d" % b, [128, NT, 520], BF16, kind="Internal") for b in range(NB)]
    ZS = [D("ZS%d" % b, [128, NT, 512], BF16, kind="Internal") for b in range(NB)]
    CAT = [D("CAT%d" % b, [128, 8, T], BF16, kind="Internal") for b in range(NB)]
    BXT = [gbuf("XT%d" % b) for b in range(NB)]; BX1T = [gbuf("X1T%d" % b) for b in range(NB)]
    BQT = [gbuf("QT%d" % b) for b in range(NB)]; BKT = [gbuf("KT%d" % b) for b in range(NB)]
    BVV = [gbuf("VV%d" % b) for b in range(NB)]; BZS = [gbuf("ZS%d" % b) for b in range(NB)]
    BCAT = [gbuf("CAT%d" % b) for b in range(NB)]
    Bout = gbuf("out")

    ps = [nc.alloc_psum_tensor("ps%d" % i, [128, 512], F32).ap() for i in range(8)]
    Bps = [Buf("ps%d" % i) for i in range(8)]
    pctr = [0]

    def P():
        i = pctr[0] % 8
        pctr[0] += 1
        return ps[i], Bps[i]

    V, ACT, PE, GP = nc.vector, nc.scalar, nc.tensor, nc.gpsimd

    def mm(out, lhsT, rhs, rd, wb, first, last, start=None):
        S.op("pe", lambda: PE.matmul(out, lhsT=lhsT, rhs=rhs, start=(first if start is None else start), stop=last), reads=rd, writes=[wb], partial=not first)

    def mmacc(out, pairs, rd, wb):
        n = len(pairs)
        for i, (l_, r_) in enumerate(pairs):
            mm(out, l_, r_, rd, wb, i == 0, i == n - 1)

    with ExitStack() as G:
        cst = A(G, "cst", [128, NCONST], F32); Bcst = gbuf("cst")
        S.dma("sp", cst, const_d, writes=[Bcst])
        ident = cst[:, C_ID:C_ID + 128]; ones = cst[:, C_ONE:C_ONE + 128]
        tri = [cst[:, C_TRF:C_TRF + 128], cst[:, C_TRB:C_TRB + 128]]
        mneg = [cst[:, C_MNF:C_MNF + 128], cst[:, C_MNB:C_MNB + 128]]
        pm = cst[:, C_PM:C_PM + 128]
        mneg4 = [cst[:, C_MN4F:C_MN4F + 512], cst[:, C_MN4B:C_MN4B + 512]]
        sel = cst[:, C_SEL:C_SEL + 1024].rearrange("p (e m) -> p e m", e=8)
        identb = A(G, "identb", [128, 128], BF16); Bidb = Buf("idb")
        S.op("dve", lambda: V.tensor_copy(out=identb, in_=ident), reads=[Bcst], writes=[Bidb])
        modT = A(G, "modT", [128, 48, 3], F32); Bmod = Buf("mod")
        prm = A(G, "prm", [128, NP_], F32); Bprm = gbuf("prm")
        negA = A(G, "negA", [128, 16], F32); BnegA = Buf("negA")
        sT = A(G, "sT", [128, 8, 3], F32); BsT = gbuf("sT")
        S.dma("sp", sT, cT_d, writes=[BsT])
        S.op("act", lambda: ACT.activation(out=sT, in_=sT, func=AF.Silu), reads=[BsT], writes=[BsT])

        with ExitStack() as st:
            xin = [A(st, "xin%d" % i, [128, 1024], F32) for i in range(2)]; Bxin = [gbuf("xin%d" % i) for i in range(2)]
            xo = [A(st, "xo%d" % i, [128, 8, 512], F32) for i in range(2)]; Bxo = [Buf("xo%d" % i) for i in range(2)]
            ti = 0
            for b in range(NB):
                for bi, (t0, n) in enumerate(BLKS):
                    o, Bo = xo[bi % 2], Bxo[bi % 2]
                    for tt in range(n // 128):
                        t = t0 + tt * 128
                        src = x_d[b, t:t + 128, :] if t < L else ctx_d[b, t - L:t - L + 128, :]
                        xi, Bxi = xin[ti % 2], Bxin[ti % 2]; ti += 1
                        S.dma("sp", xi, src, writes=[Bxi])
                        for hh in range(2):
                            pa, pb_ = P()
                            for k4 in range(4):
                                k = hh * 4 + k4
                                S.op("pe", lambda: PE.transpose(out=pa[:, k4 * 128:(k4 + 1) * 128], in_=xi[:, k * 128:(k + 1) * 128], identity=ident),
                                     reads=[Bxi, Bcst], writes=[pb_], partial=(k4 > 0))
                            S.op("act" if hh else "dve",
                                 (lambda: ACT.copy(out=o[:, hh * 4:hh * 4 + 4, tt * 128:(tt + 1) * 128], in_=pa.rearrange("p (k c) -> p k c", k=4))) if hh else
                                 (lambda: V.tensor_copy(out=o[:, hh * 4:hh * 4 + 4, tt * 128:(tt + 1) * 128], in_=pa.rearrange("p (k c) -> p k c", k=4))),
                                 reads=[pb_], writes=[Bo], partial=not (tt == 0 and hh == 0))
                    S.dma("pool", XT[b][:, :, t0:t0 + n], o[:, :, 0:n], reads=[Bo], writes=[BXT[b]], partial=True, semb=Bo)
            S.barrier()

        for l in range(n_layers):
            last = (l == n_layers - 1)
            moe = (l % 2 == 1)
            win_l = win_d[l].rearrange("(k p) n -> p k n", p=128)
            S.dma("sp", prm, prm_d[l], writes=[Bprm])
            S.op("act", lambda: ACT.activation(out=negA, in_=prm[:, P_ALOG:P_ALOG + 16], func=AF.Exp), reads=[Bprm], writes=[BnegA])
            S.op("dve", lambda: V.tensor_scalar(out=negA, in0=negA, scalar1=-1.0, scalar2=None, op0=ALU.mult), reads=[BnegA], writes=[BnegA])
            with ExitStack() as st:
                wm = [A(st, "wm%d" % i, [128, 8, 256], F32) for i in range(4)]; Bwm = [gbuf("wm%d" % i) for i in range(4)]
                wmv = wmod_d[l].rearrange("(k p) n -> p k n", p=128)
                for pc in range(24):
                    w_, Bw_ = wm[pc % 4], Bwm[pc % 4]
                    S.dma(("sp", "pool", "act", "pool")[pc % 4], w_, wmv[:, :, pc * 256:(pc + 1) * 256], writes=[Bw_])
                    for oc in range(2):
                        m = pc * 2 + oc
                        pa, pb_ = P()
                        mmacc(pa[:, 0:3], [(w_[:, k, oc * 128:(oc + 1) * 128], sT[:, k, :]) for k in range(8)], [Bw_, BsT], pb_)
                        S.op("dve", lambda: V.tensor_scalar(out=modT[:, m, :], in0=pa[:, 0:3], scalar1=prm[:, P_BMOD + m:P_BMOD + m + 1],
                                                            scalar2=(1.0 if (m // 8) in (1, 2, 4, 5) else 0.0), op0=ALU.add, op1=ALU.add),
                             reads=[pb_, Bprm], writes=[Bmod], partial=(m > 0))
                S.barrier()

            for b in range(NB):
                def mj(t0):
                    return b if t0 < L else 2
                with ExitStack() as pst:
                    XS = A(pst, "XS", [128, NT, 512], BF16); BXS = Buf("XS")
                    BT = A(pst, "BT", [128, 2, T], BF16); BBT = Buf("BT")
                    CT = A(pst, "CT", [128, 2, T], BF16); BCT = Buf("CT")
                    Btok = A(pst, "Btok", [128, NT, 2, 128], BF16); BBtok = Buf("Btok")
                    dt = A(pst, "dt", [128, NT, 16], F32); Bdt = Buf("dt")
                    da = A(pst, "da", [128, NT, 16], F32); Bda = Buf("da")
                    with ExitStack() as st:
                        h1T = A(st, "h1T", [128, 8, T], BF16); Bh1 = Buf("h1T")
                        wdt = A(st, "wdt", [128, 8, 16], F32); Bwdt = gbuf("wdt")
                        S.dma("sp", wdt, win_l[:, :, 3072:3088], writes=[Bwdt])
                        with ExitStack() as st1:
                            xt = [A(st1, "xt%d" % i, [128, 8, 256], F32) for i in range(2)]; Bxt = [gbuf("xt%d" % i) for i in range(2)]
                            h1f = [A(st1, "h1f%d" % i, [128, 8, 256], F32) for i in range(2)]; Bh1f = [Buf("h1f%d" % i) for i in range(2)]
                            dtr = A(st1, "dtr", [128, 16], F32); Bdtr = Buf("dtr")
                            for bi in range(T // 256):
                                t0 = bi * 256
                                j = mj(t0)
                                x_, Bx_ = xt[bi % 2], Bxt[bi % 2]
                                hf, Bhf = h1f[bi % 2], Bh1f[bi % 2]
                                S.dma("pool", x_, XT[b][:, :, t0:t0 + 256], reads=[BXT[b]], writes=[Bx_])
                                for k in range(8):
                                    S.op("dve", lambda: V.tensor_scalar(out=hf[:, k, :], in0=x_[:, k, :], scalar1=modT[:, 8 + k, j:j + 1], scalar2=modT[:, k, j:j + 1],
                                                                        op0=ALU.mult, op1=ALU.add), reads=[Bx_, Bmod], writes=[Bhf], partial=(k > 0))
                                S.op("act", lambda: ACT.copy(out=h1T[:, :, t0:t0 + 256], in_=hf), reads=[Bhf], writes=[Bh1], partial=True)
                                for tt in range(2):
                                    t = bi * 2 + tt
                                    pa, pb_ = P()
                                    mmacc(pa[:, 0:16], [(hf[:, k, tt * 128:(tt + 1) * 128], wdt[:, k, :]) for k in range(8)], [Bhf, Bwdt], pb_)
                                    S.op("dve", lambda: V.tensor_tensor(out=dtr, in0=pa[:, 0:16], in1=prm[:, P_DTB:P_DTB + 16], op=ALU.add), reads=[pb_, Bprm], writes=[Bdtr])
                                    S.op("act", lambda: ACT.activation(out=dtr, in_=dtr, func=AF.Exp), reads=[Bdtr], writes=[Bdtr])
                                    S.op("act", lambda: ACT.activation(out=dt[:, t, :], in_=dtr, func=AF.Ln, bias=1.0), reads=[Bdtr], writes=[Bdt], partial=True)
                                    S.op("dve", lambda: V.tensor_tensor(out=da[:, t, :], in0=dt[:, t, :], in1=negA, op=ALU.mult), reads=[Bdt, BnegA], writes=[Bda], partial=True)
                            S.barrier()
                        with ExitStack() as st2:
                            wst = [A(st2, "wst%d" % i, [128, 8, 256], F32) for i in range(2)]; Bwst = [gbuf("wst%d" % i) for i in range(2)]
                            wbf = [A(st2, "wbf%d" % i, [128, 8, 256], BF16) for i in range(2)]; Bwbf = [Buf("wbf%d" % i) for i in range(2)]
                            qks = [A(st2, "qks%d" % i, [128, T], BF16) for i in range(2)]; Bqks = [gbuf("qks%d" % i) for i in range(2)]
                            vs = [A(st2, "vs%d" % i, [128, 8, 65], BF16) for i in range(2)]; Bvs = [gbuf("vs%d" % i) for i in range(2)]
                            zs = [A(st2, "zs%d" % i, [128, 256], BF16) for i in range(2)]; Bzs = [gbuf("zs%d" % i) for i in range(2)]
                            U = A(st2, "U", [128, 2312], F32); BU = Buf("U")
                            acc = A(st2, "cacc", [128, T], F32); Bacc = Buf("cacc")
                            xsT = A(st2, "xsT", [128, T], BF16); BxsT = Buf("xsT")
                            rp = [A(st2, "rp%d" % i, [128, 2, 512], F32) for i in range(2)]; Brp = [gbuf("rp%d" % i) for i in range(2)]
                            dg = [A(st2, "dg%d" % i, [128, 5, 128], F32) for i in range(2)]; Bdg = [Buf("dg%d" % i) for i in range(2)]
                            tm1 = A(st2, "tm1", [128, 512], F32); Btm1 = Buf("tm1")
                            tm2 = A(st2, "tm2", [128, 512], F32); Btm2 = Buf("tm2")
                            S.op("dve", lambda: V.memset(U, 0.0), writes=[BU])
                            for i in range(2):
                                S.op("dve", lambda: V.memset(vs[i], 1.0), writes=[Bvs[i]])
                            cnt = [0, 0, 0, 0]

                            def getw(pi, slot):
                                i = slot % 2
                                S.dma("sp", wst[i], win_l[:, :, pi * 256:(pi + 1) * 256], writes=[Bwst[i]])
                                if pi % 2:
                                    S.op("act", lambda: ACT.copy(out=wbf[i], in_=wst[i]), reads=[Bwst[i]], writes=[Bwbf[i]])
                                else:
                                    S.op("pool", lambda: GP.tensor_copy(out=wbf[i], in_=wst[i]), reads=[Bwst[i]], writes=[Bwbf[i]])
                                return wbf[i], Bwbf[i]

                            porder = [8, 0, 9, 1, 10, 2, 11, 3, 4, 5, 6, 7]
                            nxt = getw(porder[0], 0)
                            for pidx, pi in enumerate(porder):
                                wb, Bwb = nxt
                                if pidx + 1 < 12:
                                    nxt = getw(porder[pidx + 1], pidx + 1)
                                grp, half = pi // 2, pi % 2
                                if grp in (0, 1):
                                    for oc2 in range(2):
                                        oc = half * 2 + oc2
                                        q_, Bq_ = qks[cnt[0] % 2], Bqks[cnt[0] % 2]; cnt[0] += 1
                                        for (t0, n) in BLKS:
                                            pa, pb_ = P()
                                            mmacc(pa[:, 0:n], [(wb[:, k, oc2 * 128:(oc2 + 1) * 128], h1T[:, k, t0:t0 + n]) for k in range(8)], [Bwb, Bh1], pb_)
                                            S.op("act", lambda: ACT.activation(out=q_[:, t0:t0 + n], in_=pa[:, 0:n], func=AF.Copy, scale=(0.125 if grp == 0 else 1.0)),
                                                 reads=[pb_], writes=[Bq_], partial=True)
                                        dst, Bdst = (QT[b], BQT[b]) if grp == 0 else (KT[b], BKT[b])
                                        S.dma("pool", dst[:, oc, :], q_, reads=[Bq_], writes=[Bdst], partial=True, semb=Bq_)
                                elif grp == 2:
                                    for t in range(NT):
                                        pa, pb_ = P()
                                        mmacc(pa[:, 0:256], [(h1T[:, k, t * 128:(t + 1) * 128], wb[:, k, :]) for k in range(8)], [Bwb, Bh1], pb_)
                                        v_, Bv_ = vs[cnt[1] % 2], Bvs[cnt[1] % 2]; cnt[1] += 1
                                        S.op("dve", lambda: V.tensor_copy(out=v_[:, 0:4, 0:64], in_=pa[:, 0:256].rearrange("p (h d) -> p h d", h=4)),
                                             reads=[pb_], writes=[Bv_])
                                        S.dma("pool", VV[b][:, t, half * 260:half * 260 + 260].rearrange("p (h d) -> p h d", h=4), v_[:, 0:4, :],
                                              reads=[Bv_], writes=[BVV[b]], partial=True, semb=Bv_)
                                elif grp == 3:
                                    for t in range(NT):
                                        pa, pb_ = P()
                                        mmacc(pa[:, 0:256], [(h1T[:, k, t * 128:(t + 1) * 128], wb[:, k, :]) for k in range(8)], [Bwb, Bh1], pb_)
                                        z_, Bz_ = zs[cnt[2] % 2], Bzs[cnt[2] % 2]; cnt[2] += 1
                                        S.op("act", lambda: ACT.activation(out=z_, in_=pa[:, 0:256], func=AF.Silu), reads=[pb_], writes=[Bz_])
                                        S.dma("pool", ZS[b][:, t, half * 256:(half + 1) * 256], z_, reads=[Bz_], writes=[BZS[b]], partial=True, semb=Bz_)
                                else:
                                    for oc2 in range(2):
                                        ch = (pi - 8) * 2 + oc2
                                        for (t0, n) in BLKS:
                                            pa, pb_ = P()
                                            mmacc(pa[:, 0:n], [(wb[:, k, oc2 * 128:(oc2 + 1) * 128], h1T[:, k, t0:t0 + n]) for k in range(8)], [Bwb, Bh1], pb_)
                                            off = 2 + t0 if t0 < L else 2054 + (t0 - L)
                                            S.op("act", lambda: ACT.copy(out=U[:, off:off + n], in_=pa[:, 0:n]), reads=[pb_], writes=[BU], partial=(t0 > 0))
                                        first = True
                                        for (o, t0, n) in [(2, 0, L), (2054, L, C)]:
                                            S.op("dve", lambda: V.tensor_scalar(out=acc[:, t0:t0 + n], in0=U[:, o - 2:o - 2 + n], scalar1=prm[:, P_CW + ch * 5:P_CW + ch * 5 + 1],
                                                                                scalar2=None, op0=ALU.mult), reads=[BU, Bprm], writes=[Bacc], partial=not first)
                                            first = False
                                            for jj in range(1, 5):
                                                S.op("dve", lambda: V.scalar_tensor_tensor(out=acc[:, t0:t0 + n], in0=U[:, o - 2 + jj:o - 2 + jj + n],
                                                                                           scalar=prm[:, P_CW + ch * 5 + jj:P_CW + ch * 5 + jj + 1],
                                                                                           in1=acc[:, t0:t0 + n], op0=ALU.mult, op1=ALU.add),
                                                     reads=[BU, Bprm, Bacc], writes=[Bacc])
                                        cbias = prm[:, P_CB + ch:P_CB + ch + 1]
                                        if ch < 4:
                                            S.op("act", lambda: ACT.activation(out=xsT, in_=acc, func=AF.Silu, bias=cbias), reads=[Bacc, Bprm], writes=[BxsT])
                                            for t4 in range(0, NT, 4):
                                                nt = min(4, NT - t4)
                                                pa, pb_ = P()
                                                pab = pa.bitcast(BF16)
                                                for i in range(nt):
                                                    S.op("pe", lambda: PE.transpose(out=pab[:, i * 128:(i + 1) * 128], in_=xsT[:, (t4 + i) * 128:(t4 + i + 1) * 128], identity=identb),
                                                         reads=[BxsT, Bidb], writes=[pb_], partial=(i > 0))
                                                S.op("dve", lambda: V.tensor_copy(out=XS[:, t4:t4 + nt, ch * 128:(ch + 1) * 128],
                                                                                  in_=pab[:, 0:nt * 128].rearrange("p (a c) -> p a c", a=nt)),
                                                     reads=[pb_], writes=[BXS], partial=True)
                                        else:
                                            g = ch % 2
                                            dstT, BdstT = (BT, BBT) if ch < 6 else (CT, BCT)
                                            S.op("act", lambda: ACT.activation(out=acc, in_=acc, func=AF.Silu, bias=cbias), reads=[Bacc, Bprm], writes=[Bacc])
                                            for bi in range(4):
                                                t0 = bi * 512
                                                r_, Br_ = rp[cnt[3] % 2], Brp[cnt[3] % 2]; cnt[3] += 1
                                                S.dma("pool", r_, rope_d[:, :, t0:t0 + 512], writes=[Br_])
                                                pa, pb_ = P()
                                                mm(pa, pm, acc[:, t0:t0 + 512], [Bcst, Bacc], pb_, True, True)
                                                S.op("pool", lambda: GP.tensor_tensor(out=tm1, in0=acc[:, t0:t0 + 512], in1=r_[:, 0, :], op=ALU.mult), reads=[Bacc, Br_], writes=[Btm1])
                                                S.op("dve", lambda: V.tensor_tensor(out=tm2, in0=pa, in1=r_[:, 1, :], op=ALU.mult), reads=[pb_, Br_], writes=[Btm2])
                                                S.op("dve", lambda: V.tensor_tensor(out=dstT[:, g, t0:t0 + 512], in0=tm1, in1=tm2, op=ALU.add), reads=[Btm1, Btm2], writes=[BdstT], partial=True)
                                            S.op("act", lambda: ACT.copy(out=dstT[:, g, L:T], in_=acc[:, L:T]), reads=[Bacc], writes=[BdstT], partial=True)
                                            if ch < 6:
                                                for t4 in range(0, NT, 4):
                                                    nt = min(4, NT - t4)
                                                    pa, pb_ = P()
                                                    pab = pa.bitcast(BF16)
                                                    for i in range(nt):
                                                        S.op("pe", lambda: PE.transpose(out=pab[:, i * 128:(i + 1) * 128], in_=BT[:, g, (t4 + i) * 128:(t4 + i + 1) * 128], identity=identb),
                                                             reads=[BBT, Bidb], writes=[pb_], partial=(i > 0))
                                                    S.op("dve", lambda: V.tensor_copy(out=Btok[:, t4:t4 + nt, g, :], in_=pab[:, 0:nt * 128].rearrange("p (a c) -> p a c", a=nt)),
                                                         reads=[pb_], writes=[BBtok], partial=True)
                            S.barrier()

                    if l == 0 and b == 0:
                        dump("XS", XS, BXS, BF16); dump("dt", dt, Bdt); dump("da", da, Bda); dump("BT", BT, BBT, BF16); dump("CT", CT, BCT, BF16); dump("Btok", Btok, BBtok, BF16)
                        S.barrier()
                    with ExitStack() as st:
                        Yacc = A(st, "Yacc", [128, NT, 512], F32); BY = [Buf("Y%d" % t) for t in range(NT)]
                        car = [A(st, "car%d" % d, [128, 512], F32) for d in range(2)]; Bcar = [Buf("car%d" % d) for d in range(2)]
                        carb = [A(st, "carb%d" % d, [128, 512], BF16) for d in range(2)]; Bcarb = [Buf("carb%d" % d) for d in range(2)]
                        cats = A(st, "cats", [128, 4, T], BF16); Bcats = gbuf("cats")
                        cs = A(st, "cs", [128, 16], F32); Bcs = Buf("cs")
                        sc = A(st, "scl", [128, 6, 8], F32); Bsc = Buf("scl")
                        XDT = A(st, "XDT", [128, 512], BF16); BXDT = Buf("XDT")
                        XE = A(st, "XE", [128, 512], BF16); BXE = Buf("XE")
                        R = [A(st, "R%d" % i, [128, 128], F32) for i in range(2)]; BR = [Buf("R%d" % i) for i in range(2)]
                        LT = [A(st, "LT%d" % i, [128, 128], F32) for i in range(2)]; BLT = [Buf("LT%d" % i) for i in range(2)]
                        MT = [A(st, "MT%d" % i, [128, 128], BF16) for i in range(2)]; BMT = [Buf("MT%d" % i) for i in range(2)]
                        tY = A(st, "tY", [128, 512], F32); BtY = Buf("tY")
                        dsk = prm[:, P_DSK:P_DSK + 8]

                        def bc8(ap8):
                            return ap8.unsqueeze(2).to_broadcast([128, 8, 64])

                        def v3(ap512):
                            return ap512.rearrange("p (h d) -> p h d", h=8)

                        for t in range(NT):
                            S.op("dve", lambda: V.tensor_tensor(out=v3(Yacc[:, t, :]), in0=v3(XS[:, t, :]), in1=bc8(dsk), op=ALU.mult), reads=[BXS, Bprm], writes=[BY[t]])
                        cs2 = [A(st, "cs2%d" % i, [128, 16], F32) for i in range(2)]; Bcs2 = [Buf("cs2%d" % i) for i in range(2)]
                        sc2 = [A(st, "sc2%d" % i, [128, 6, 8], F32) for i in range(2)]; Bsc2 = [Buf("sc2%d" % i) for i in range(2)]
                        XDT2 = [A(st, "XDT2%d" % i, [128, 512], BF16) for i in range(2)]; BXDT2 = [Buf("XDT2%d" % i) for i in range(2)]
                        XE2 = [A(st, "XE2%d" % i, [128, 512], BF16) for i in range(2)]; BXE2 = [Buf("XE2%d" % i) for i in range(2)]
                        LTa = [A(st, "LTa%d" % i, [128, 1024], F32) for i in range(2)]; BLTa = [Buf("LTa%d" % i) for i in range(2)]
                        MTa = [A(st, "MTa%d" % i, [128, 1024], BF16) for i in range(2)]; BMTa = [Buf("MTa%d" % i) for i in range(2)]
                        tY2 = [A(st, "tY2%d" % i, [128, 512], F32) for i in range(2)]; BtY2 = [Buf("tY2%d" % i) for i in range(2)]
                        cc = 0
                        for d in range(2):
                            S.op("dve", lambda: V.memset(car[d], 0.0), writes=[Bcar[d]])
                            S.op("dve", lambda: V.memset(carb[d], 0.0), writes=[Bcarb[d]])
                            order = [16, 17] + list(range(16)) if d == 0 else [17, 16] + list(range(15, -1, -1))
                            for t in order:
                                ci = cc % 2; cc += 1
                                cs, Bcs = cs2[ci], Bcs2[ci]
                                sc, Bsc = sc2[ci], Bsc2[ci]
                                XDT, BXDT = XDT2[ci], BXDT2[ci]
                                XE, BXE = XE2[ci], BXE2[ci]
                                LTl, BLTl = LTa[ci], BLTa[ci]
                                MTl, BMTl = MTa[ci], BMTa[ci]
                                tY, BtY = tY2[ci], BtY2[ci]
                                tsl = slice(t * 128, (t + 1) * 128)
                                dav = da[:, t, d * 8:(d + 1) * 8]
                                dtv = dt[:, t, d * 8:(d + 1) * 8]
                                pcb, Bpcb = ps[0], Bps[0]
                                for g in range(2):
                                    mm(pcb[:, g * 128:(g + 1) * 128], BT[:, g, tsl], CT[:, g, tsl], [BBT, BCT], Bpcb, g == 0, True, start=True)
                                pcs, Bpcs = ps[1], Bps[1]
                                mm(pcs[:, 0:8], tri[d], dav, [Bcst, Bda], Bpcs, True, True)
                                mm(pcs[:, 8:16], ones, dav, [Bcst, Bda], Bpcs, False, True, start=True)
                                pl = [ps[5], ps[6]]; Bpl = [Bps[5], Bps[6]]
                                for h in range(8):
                                    hb, hq = h // 4, h % 4
                                    mm(pl[hb][:, hq * 128:(hq + 1) * 128], dav[:, h:h + 1].to_broadcast([128, 128]), tri[d], [Bcst, Bda], Bpl[hb], hq == 0, False, start=(hq == 0))
                                for hb in range(2):
                                    mm(pl[hb], ident, mneg4[d], [Bcst], Bpl[hb], False, True, start=False)
                                S.op("act", lambda: ACT.copy(out=cs, in_=pcs[:, 0:16]), reads=[Bpcs], writes=[Bcs])
                                S.op("dve", lambda: V.tensor_scalar(out=sc[:, 0, :], in0=cs[:, 0:8], scalar1=-1.0, scalar2=None, op0=ALU.mult), reads=[Bcs], writes=[Bsc])
                                S.op("dve", lambda: V.tensor_tensor(out=sc[:, 5, :], in0=cs[:, 8:16], in1=cs[:, 0:8], op=ALU.subtract), reads=[Bcs], writes=[Bsc], partial=True)
                                S.op("act", lambda: ACT.activation(out=sc[:, 1, :], in_=cs[:, 0:8], func=AF.Exp), reads=[Bcs], writes=[Bsc], partial=True)
                                S.op("act", lambda: ACT.activation(out=sc[:, 3, :], in_=cs[:, 8:16], func=AF.Exp), reads=[Bcs], writes=[Bsc], partial=True)
                                S.op("act", lambda: ACT.activation(out=sc[:, 2, :], in_=sc[:, 5, :], func=AF.Exp), reads=[Bsc], writes=[Bsc])
                                S.op("dve", lambda: V.tensor_tensor(out=sc[:, 4, :], in0=sc[:, 2, :], in1=dtv, op=ALU.mult), reads=[Bsc, Bdt], writes=[Bsc])
                                S.op("dve", lambda: V.tensor_tensor(out=v3(XDT), in0=v3(XS[:, t, :]), in1=bc8(dtv), op=ALU.mult), reads=[BXS, Bdt], writes=[BXDT])
                                for h in range(8):
                                    hs = slice(h * 64, (h + 1) * 64)
                                    S.op("act", lambda: ACT.activation(out=XE[:, hs], in_=XS[:, t, hs], func=AF.Identity, scale=sc[:, 4, h:h + 1]), reads=[BXS, Bsc], writes=[BXE], partial=(h > 0))
                                for h in range(8):
                                    hb, hq = h // 4, h % 4
                                    S.op("act", lambda: ACT.activation(out=LTl[:, h * 128:(h + 1) * 128], in_=pl[hb][:, hq * 128:(hq + 1) * 128], func=AF.Exp, bias=sc[:, 0, h:h + 1]),
                                         reads=[Bpl[hb], Bsc], writes=[BLTl], partial=(h > 0))
                                for g in range(2):
                                    S.op("dve", lambda: V.tensor_tensor(out=MTl[:, g * 512:(g + 1) * 512].rearrange("p (a c) -> p a c", a=4),
                                                                        in0=pcb[:, g * 128:(g + 1) * 128].unsqueeze(1).to_broadcast([128, 4, 128]),
                                                                        in1=LTl[:, g * 512:(g + 1) * 512].rearrange("p (a c) -> p a c", a=4), op=ALU.mult),
                                         reads=[Bpcb, BLTl], writes=[BMTl], partial=(g > 0))
                                py, Bpy = ps[2], Bps[2]; po, Bpo = ps[3], Bps[3]; pst_, Bpst = ps[4], Bps[4]
                                for h in range(8):
                                    g = h // 4
                                    hs = slice(h * 64, (h + 1) * 64)
                                    mm(po[:, hs], CT[:, g, tsl], carb[d][:, hs], [BCT, Bcarb[d]], Bpo, h == 0, True, start=True)
                                for h in range(8):
                                    g = h // 4
                                    hs = slice(h * 64, (h + 1) * 64)
                                    mm(pst_[:, hs], Btok[:, t, g, :], XE[:, hs], [BBtok, BXE], Bpst, h == 0, True, start=True)
                                for h in range(8):
                                    hs = slice(h * 64, (h + 1) * 64)
                                    mm(py[:, hs], MTl[:, h * 128:(h + 1) * 128], XDT[:, hs], [BMTl, BXDT], Bpy, h == 0, True, start=True)
                                S.op("dve", lambda: V.tensor_tensor(out=v3(car[d]), in0=v3(car[d]), in1=bc8(sc[:, 3, :]), op=ALU.mult), reads=[Bcar[d], Bsc], writes=[Bcar[d]])
                                S.op("dve", lambda: V.tensor_tensor(out=car[d], in0=pst_, in1=car[d], op=ALU.add), reads=[Bpst, Bcar[d]], writes=[Bcar[d]])
                                S.op("act", lambda: ACT.copy(out=carb[d], in_=car[d]), reads=[Bcar[d]], writes=[Bcarb[d]])
                                S.op("dve", lambda: V.tensor_tensor(out=v3(tY), in0=v3(po), in1=bc8(sc[:, 1, :]), op=ALU.mult), reads=[Bpo, Bsc], writes=[BtY])
                                S.op("pool", lambda: GP.tensor_tensor(out=Yacc[:, t, :], in0=Yacc[:, t, :], in1=tY, op=ALU.add), reads=[BtY, BY[t]], writes=[BY[t]])
                                S.op("dve", lambda: V.tensor_tensor(out=Yacc[:, t, :], in0=py, in1=Yacc[:, t, :], op=ALU.add), reads=[Bpy, BY[t]], writes=[BY[t]])
                        if l == 0 and b == 0:
                            dump("Yacc", Yacc, BY[0])
                            S.barrier()
                        zt = [A(st, "zt%d" % i, [128, 512], BF16) for i in range(2)]; Bzt = [gbuf("zt%d" % i) for i in range(2)]
                        Gt = A(st, "Gt", [128, 512], F32); BGt = Buf("Gt")
                        Gq = A(st, "Gq", [128, 512], F32); BGq = Buf("Gq")
                        ss = A(st, "ss", [128, 2], F32); Bss = Buf("ss")
                        stok = [A(st, "stok%d" % i, [128, 512], BF16) for i in range(2)]; Bstok = [Buf("stok%d" % i) for i in range(2)]
                        nw = prm[:, P_NW:P_NW + 512]
                        tlist = list(range(NT)) if not last else list(range(16))
                        for t in tlist:
                            z_, Bz_ = zt[t % 2], Bzt[t % 2]
                            S.dma("sp", z_, ZS[b][:, t, :], reads=[BZS[b]], writes=[Bz_])
                            S.op("dve", lambda: V.tensor_tensor(out=Gt, in0=Yacc[:, t, :], in1=z_, op=ALU.mult), reads=[BY[t], Bz_], writes=[BGt])
                            for g in range(2):
                                S.op("act", lambda: ACT.activation(out=Gq[:, g * 256:(g + 1) * 256], in_=Gt[:, g * 256:(g + 1) * 256], func=AF.Square, accum_out=ss[:, g:g + 1]),
                                     reads=[BGt], writes=[BGq, Bss], partial=(g > 0))
                            S.op("act", lambda: ACT.activation(out=ss, in_=ss, func=AF.Sqrt, bias=1e-5, scale=1.0 / 256), reads=[Bss], writes=[Bss])
                            S.op("dve", lambda: V.reciprocal(out=ss, in_=ss), reads=[Bss], writes=[Bss])
                            s_, Bs_ = stok[t % 2], Bstok[t % 2]
                            for g in range(2):
                                S.op("dve", lambda: V.scalar_tensor_tensor(out=s_[:, g * 256:(g + 1) * 256], in0=Gt[:, g * 256:(g + 1) * 256], scalar=ss[:, g:g + 1],
                                                                           in1=nw[:, g * 256:(g + 1) * 256], op0=ALU.mult, op1=ALU.mult),
                                     reads=[BGt, Bss, Bprm], writes=[Bs_], partial=(g > 0))
                            pa, pb_ = P()
                            pab = pa.bitcast(BF16)
                            for i in range(4):
                                S.op("pe", lambda: PE.transpose(out=pab[:, i * 128:(i + 1) * 128], in_=s_[:, i * 128:(i + 1) * 128], identity=identb),
                                     reads=[Bs_, Bidb], writes=[pb_], partial=(i > 0))
                            S.op("act", lambda: ACT.copy(out=cats[:, :, t * 128:(t + 1) * 128], in_=pab[:, 0:512].rearrange("p (a c) -> p a c", a=4)),
                                 reads=[pb_], writes=[Bcats], partial=True)
                        S.dma("pool", CAT[b][:, 4:8, :], cats, reads=[Bcats], writes=[BCAT[b]], partial=True, semb=Bcats)
                        S.barrier()

                with ExitStack() as st:
                    tab = [A(st, "tab%d" % i, [128, 25, 128], F32) for i in range(2)]; Btab = [gbuf("tab%d" % i) for i in range(2)]
                    qh = [A(st, "qh%d" % i, [64, T], BF16) for i in range(2)]; Bqh = [gbuf("qh%d" % i) for i in range(2)]
                    kh = [A(st, "kh%d" % i, [64, T], BF16) for i in range(2)]; Bkh = [gbuf("kh%d" % i) for i in range(2)]
                    vh = [A(st, "vh%d" % i, [128, NT, 65], BF16) for i in range(2)]; Bvh = [gbuf("vh%d" % i) for i in range(2)]
                    atok = A(st, "atok", [128, NT, 512], BF16); Batok = [Buf("atok%d" % t) for t in range(NT)]
                    cata = A(st, "cata", [128, 4, T], BF16); Bcata = gbuf("cata")
                    e1 = [A(st, "e1%d" % i, [128, 640], F32) for i in range(3)]; Be1 = [Buf("e1%d" % i) for i in range(3)]
                    pT = [A(st, "pT%d" % i, [128, 896], BF16) for i in range(3)]; BpT = [Buf("pT%d" % i) for i in range(3)]
                    rinv = [A(st, "rinv%d" % i, [128, 1], F32) for i in range(3)]; Brinv = [Buf("rinv%d" % i) for i in range(3)]
                    def load_head(h):
                        hi = h % 2
                        c_, pb0 = h // 2, (h % 2) * 64
                        S.dma("sp", tab[hi], btab_d[l, h].rearrange("p (a c) -> p a c", a=25), writes=[Btab[hi]])
                        S.dma("sp", qh[hi], QT[b][pb0:pb0 + 64, c_, :], reads=[BQT[b]], writes=[Bqh[hi]])
                        S.dma("sp", kh[hi], KT[b][pb0:pb0 + 64, c_, :], reads=[BKT[b]], writes=[Bkh[hi]])
                        S.dma("sp", vh[hi], VV[b][:, :, h * 65:(h + 1) * 65], reads=[BVV[b]], writes=[Bvh[hi]])

                    jl = list(range(16)) + ([] if last else [16, 17])
                    aitems = [(h, j) for h in range(8) for j in jl]

                    def QKE(idx):
                        h, j = aitems[idx]
                        hi = h % 2
                        i = idx % 3
                        qs = qh[hi][:, j * 128:(j + 1) * 128]
                        p1, Bp1 = ps[(idx % 2) * 2], Bps[(idx % 2) * 2]
                        p2, Bp2 = ps[(idx % 2) * 2 + 1], Bps[(idx % 2) * 2 + 1]
                        if j < 16:
                            ty = 0 if j == 0 else 1 if j == 1 else 3 if j == 14 else 4 if j == 15 else 2
                            base = min(max(j - 2, 0), 11)
                            for s in range(4):
                                kt = base + s
                                mm(p1[:, s * 128:(s + 1) * 128], kh[hi][:, kt * 128:(kt + 1) * 128], qs, [Bkh[hi], Bqh[hi]], Bp1, s == 0, True, start=True)
                            for s, kt in enumerate([base + 4, 16, 17]):
                                mm(p2[:, s * 128:(s + 1) * 128], kh[hi][:, kt * 128:(kt + 1) * 128], qs, [Bkh[hi], Bqh[hi]], Bp2, s == 0, True, start=True)
                            S.op("dve", lambda: V.tensor_tensor(out=e1[i][:, 0:512], in0=p1, in1=tab[hi][:, ty * 5:ty * 5 + 4, :].rearrange("p a c -> p (a c)"), op=ALU.add),
                                 reads=[Bp1, Btab[hi]], writes=[Be1[i]])
                            S.op("dve", lambda: V.tensor_tensor(out=e1[i][:, 512:640], in0=p2[:, 0:128], in1=tab[hi][:, ty * 5 + 4, :], op=ALU.add),
                                 reads=[Bp2, Btab[hi]], writes=[Be1[i]], partial=True)
                            S.op("act", lambda: ACT.activation(out=pT[i][:, 0:640], in_=e1[i], func=AF.Exp), reads=[Be1[i]], writes=[BpT[i]])
                            S.op("act", lambda: ACT.activation(out=pT[i][:, 640:896], in_=p2[:, 128:384], func=AF.Exp), reads=[Bp2], writes=[BpT[i]], partial=True)
                        else:
                            for s, kt in enumerate([16, 17]):
                                mm(p2[:, s * 128:(s + 1) * 128], kh[hi][:, kt * 128:(kt + 1) * 128], qs, [Bkh[hi], Bqh[hi]], Bp2, s == 0, True, start=True)
                            S.op("act", lambda: ACT.activation(out=pT[i][:, 0:256], in_=p2[:, 0:256], func=AF.Exp), reads=[Bp2], writes=[BpT[i]])

                    def PVN(idx):
                        h, j = aitems[idx]
                        hi = h % 2
                        i = idx % 3
                        po, Bpo = ps[4 + idx % 4], Bps[4 + idx % 4]
                        if j < 16:
                            base = min(max(j - 2, 0), 11)
                            kts = [base + s for s in range(5)] + [16, 17]
                        else:
                            kts = [16, 17]
                        mmacc(po[:, 0:65], [(pT[i][:, s * 128:(s + 1) * 128], vh[hi][:, kt, :]) for s, kt in enumerate(kts)], [BpT[i], Bvh[hi]], Bpo)
                        S.op("dve", lambda: V.reciprocal(out=rinv[i], in_=po[:, 64:65]), reads=[Bpo], writes=[Brinv[i]])
                        S.op("dve", lambda: V.tensor_scalar(out=atok[:, j, h * 64:(h + 1) * 64], in0=po[:, 0:64], scalar1=rinv[i], scalar2=None, op0=ALU.mult),
                             reads=[Bpo, Brinv[i]], writes=[Batok[j]], partial=True)

                    load_head(0)
                    load_head(1)
                    QKE(0)
                    for idx in range(len(aitems)):
                        if idx + 1 < len(aitems):
                            QKE(idx + 1)
                        PVN(idx)
                        h, j = aitems[idx]
                        if j == jl[0] and 1 <= h <= 6:
                            load_head(h + 1)
                    tl = list(range(NT)) if not last else list(range(16))
                    for t in tl:
                        pa, pb_ = P()
                        pab = pa.bitcast(BF16)
                        for i in range(4):
                            S.op("pe", lambda: PE.transpose(out=pab[:, i * 128:(i + 1) * 128], in_=atok[:, t, i * 128:(i + 1) * 128], identity=identb),
                                 reads=[Batok[t], Bidb], writes=[pb_], partial=(i > 0))
                        S.op("act", lambda: ACT.copy(out=cata[:, :, t * 128:(t + 1) * 128], in_=pab[:, 0:512].rearrange("p (a c) -> p a c", a=4)),
                             reads=[pb_], writes=[Bcata], partial=True)
                    S.dma("pool", CAT[b][:, 0:4, :], cata, reads=[Bcata], writes=[BCAT[b]], partial=True, semb=Bcata)
                    S.barrier()

                blks = BLKS if not last else BLKS[:4]
                Teff = T if not last else L

                def layernorm(st_tiles, Y, BY_, n, which, outs):
                    Ysq, BYsq, mean, Bmean, msq, Bmsq, rstd, Brstd, tmp, Btmp = st_tiles
                    p1, Bp1 = P()
                    mmacc(p1[:, 0:n], [(ones, Y[:, k, 0:n]) for k in range(8)], [Bcst, BY_], Bp1)
                    S.op("act", lambda: ACT.activation(out=Ysq[:, :, 0:n], in_=Y[:, :, 0:n], func=AF.Square), reads=[BY_], writes=[BYsq])
                    p2, Bp2 = P()
                    mmacc(p2[:, 0:n], [(ones, Ysq[:, k, 0:n]) for k in range(8)], [Bcst, BYsq], Bp2)
                    S.op("act", lambda: ACT.activation(out=mean[:, 0:n], in_=p1[:, 0:n], func=AF.Copy, scale=1.0 / 1024), reads=[Bp1], writes=[Bmean])
                    S.op("act", lambda: ACT.activation(out=msq[:, 0:n], in_=p1[:, 0:n], func=AF.Square, scale=1.0 / 1024), reads=[Bp1], writes=[Bmsq])
                    S.op("dve", lambda: V.scalar_tensor_tensor(out=rstd[:, 0:n], in0=p2[:, 0:n], scalar=1.0 / 1024, in1=msq[:, 0:n], op0=ALU.mult, op1=ALU.subtract),
                         reads=[Bp2, Bmsq], writes=[Brstd])
                    S.op("act", lambda: ACT.activation(out=rstd[:, 0:n], in_=rstd[:, 0:n], func=AF.Sqrt, bias=1e-5), reads=[Brstd], writes=[Brstd])
                    S.op("dve", lambda: V.reciprocal(out=rstd[:, 0:n], in_=rstd[:, 0:n]), reads=[Brstd], writes=[Brstd])
                    for k in range(8):
                        S.op("pool", lambda: GP.tensor_tensor(out=tmp[:, k, 0:n], in0=Y[:, k, 0:n], in1=mean[:, 0:n], op=ALU.subtract), reads=[BY_, Bmean], writes=[Btmp], partial=(k > 0))
                    for k in range(8):
                        S.op("dve", lambda: V.tensor_tensor(out=tmp[:, k, 0:n], in0=tmp[:, k, 0:n], in1=rstd[:, 0:n], op=ALU.mult), reads=[Btmp, Brstd], writes=[Btmp], partial=(k > 0))
                    gcol = P_LNG + which * 8
                    bcol = P_LNB + which * 8
                    for k in range(8):
                        S.op("act", lambda: ACT.activation(out=tmp[:, k, 0:n], in_=tmp[:, k, 0:n], func=AF.Identity, scale=prm[:, gcol + k:gcol + k + 1], bias=prm[:, bcol + k:bcol + k + 1]),
                             reads=[Btmp, Bprm], writes=[Btmp], partial=(k > 0))
                    return tmp, Btmp

                with ExitStack() as pd:
                    h2T = A(pd, "h2T", [128, 8, T], BF16); Bh2 = Buf("h2T")
                    combT = A(pd, "combT", [8, T], F32); BcombT = Buf("combT")
                    with ExitStack() as st:
                        catb = [A(st, "catb%d" % i, [128, 8, 512], BF16) for i in range(2)]; Bcatb = [gbuf("catb%d" % i) for i in range(2)]
                        xtb = [A(st, "xtb0", [128, 8, 512], F32)] * 2; Bxtb = [gbuf("xtb0")] * 2
                        woutb = A(st, "woutb", [128, 8, 1024], BF16); Bwout = Buf("wout")
                        wo_s = [A(st, "wos%d" % i, [128, 8, 128], F32) for i in range(2)]; Bwo_s = [gbuf("wos%d" % i) for i in range(2)]
                        wov = wout_d[l].rearrange("(k p) n -> p k n", p=128)
                        for pc in range(8):
                            w_, Bw_ = wo_s[pc % 2], Bwo_s[pc % 2]
                            S.dma("sp" if pc % 2 == 0 else "pool", w_, wov[:, :, pc * 128:(pc + 1) * 128], writes=[Bw_])
                            S.op("act", lambda: ACT.copy(out=woutb[:, :, pc * 128:(pc + 1) * 128], in_=w_), reads=[Bw_], writes=[Bwout], partial=(pc > 0))
                        Y = A(st, "Y", [128, 8, 512], F32); BY_ = Buf("Y")
                        lnt = (A(st, "Ysq", [128, 8, 512], F32), Buf("Ysq"), A(st, "mean", [128, 512], F32), Buf("mean"), A(st, "msq", [128, 512], F32), Buf("msq"),
                               A(st, "rstd", [128, 512], F32), Buf("rstd"), A(st, "lntmp", [128, 8, 512], F32), gbuf("lntmp"))
                        h2f, Bh2f = lnt[0], lnt[1]
                        rtw = A(st, "rtw", [128, 8, 8], F32); Brtw = gbuf("rtw")
                        lg = A(st, "lg", [128, 8], F32); Blg = Buf("lg")
                        sm = A(st, "sm", [128, 8, 8], F32); Bsm = Buf("sm")
                        if moe:
                            S.dma("sp", rtw, rt_d[l // 2], writes=[Brtw])
                        for bi, (t0, n) in enumerate(blks):
                            j = mj(t0)
                            cb_, Bcb_ = catb[bi % 2], Bcatb[bi % 2]
                            xb_, Bxb_ = xtb[bi % 2], Bxtb[bi % 2]
                            S.dma("sp", cb_[:, :, 0:n], CAT[b][:, :, t0:t0 + n], reads=[BCAT[b]], writes=[Bcb_])
                            S.dma("pool", xb_[:, :, 0:n], XT[b][:, :, t0:t0 + n], reads=[BXT[b]], writes=[Bxb_])
                            for oc in range(8):
                                pa, pb_ = P()
                                mmacc(pa[:, 0:n], [(woutb[:, k, oc * 128:(oc + 1) * 128], cb_[:, k, 0:n]) for k in range(8)], [Bwout, Bcb_], pb_)
                                S.op("act", lambda: ACT.activation(out=Y[:, oc, 0:n], in_=pa[:, 0:n], func=AF.Identity, scale=modT[:, 16 + oc, j:j + 1]),
                                     reads=[pb_, Bmod], writes=[BY_], partial=(oc > 0))
                                S.op("dve", lambda: V.scalar_tensor_tensor(out=Y[:, oc, 0:n], in0=xb_[:, oc, 0:n], scalar=ALPHA, in1=Y[:, oc, 0:n], op0=ALU.mult, op1=ALU.add),
                                     reads=[Bxb_, BY_], writes=[BY_], partial=True)
                            x1, Bx1 = layernorm(lnt, Y, BY_, n, 0, None)
                            S.dma("pool", X1T[b][:, :, t0:t0 + n], x1[:, :, 0:n], reads=[Bx1], writes=[BX1T[b]], partial=True, semb=Bx1)
                            for k in range(8):
                                S.op("dve", lambda: V.tensor_scalar(out=h2f[:, k, 0:n], in0=x1[:, k, 0:n], scalar1=modT[:, 32 + k, j:j + 1], scalar2=modT[:, 24 + k, j:j + 1],
                                                                    op0=ALU.mult, op1=ALU.add), reads=[Bx1, Bmod], writes=[Bh2f], partial=(k > 0))
                            S.op("act", lambda: ACT.copy(out=h2T[:, :, t0:t0 + n], in_=h2f[:, :, 0:n]), reads=[Bh2f], writes=[Bh2], partial=True)
                            if moe:
                                for tt in range(n // 128):
                                    tsl = slice(tt * 128, (tt + 1) * 128)
                                    pa, pb_ = P()
                                    mmacc(pa[:, 0:8], [(h2f[:, k, tsl], rtw[:, k, :]) for k in range(8)], [Bh2f, Brtw], pb_)
                                    S.op("dve", lambda: V.tensor_copy(out=lg, in_=pa[:, 0:8]), reads=[pb_], writes=[Blg])
                                    m1, eq1, l2, m2, eq2, dd, g1, cmb = (sm[:, 0, 0:1], sm[:, 1, :], sm[:, 2, :], sm[:, 0, 1:2], sm[:, 3, :], sm[:, 0, 2:3], sm[:, 0, 3:4], sm[:, 4, :])
                                    g2 = sm[:, 0, 4:5]
                                    S.op("dve", lambda: V.reduce_max(out=m1, in_=lg, axis=AX.X), reads=[Blg], writes=[Bsm])
                                    S.op("dve", lambda: V.tensor_scalar(out=eq1, in0=lg, scalar1=m1, scalar2=None, op0=ALU.is_equal), reads=[Blg, Bsm], writes=[Bsm])
                                    S.op("dve", lambda: V.scalar_tensor_tensor(out=l2, in0=eq1, scalar=-1e30, in1=lg, op0=ALU.mult, op1=ALU.add), reads=[Blg, Bsm], writes=[Bsm])
                                    S.op("dve", lambda: V.reduce_max(out=m2, in_=l2, axis=AX.X), reads=[Bsm], writes=[Bsm])
                                    S.op("dve", lambda: V.tensor_scalar(out=eq2, in0=l2, scalar1=m2, scalar2=None, op0=ALU.is_equal), reads=[Bsm], writes=[Bsm])
                                    S.op("dve", lambda: V.tensor_tensor(out=dd, in0=m2, in1=m1, op=ALU.subtract), reads=[Bsm], writes=[Bsm])
                                    S.op("act", lambda: ACT.activation(out=dd, in_=dd, func=AF.Exp), reads=[Bsm], writes=[Bsm])
                                    S.op("dve", lambda: V.tensor_scalar(out=g1, in0=dd, scalar1=1.0, scalar2=None, op0=ALU.add), reads=[Bsm], writes=[Bsm])
                                    S.op("dve", lambda: V.reciprocal(out=g1, in_=g1), reads=[Bsm], writes=[Bsm])
                                    S.op("dve", lambda: V.tensor_tensor(out=g2, in0=dd, in1=g1, op=ALU.mult), reads=[Bsm], writes=[Bsm])
                                    S.op("dve", lambda: V.tensor_scalar(out=cmb, in0=eq1, scalar1=g1, scalar2=None, op0=ALU.mult), reads=[Bsm], writes=[Bsm])
                                    S.op("dve", lambda: V.scalar_tensor_tensor(out=cmb, in0=eq2, scalar=g2, in1=cmb, op0=ALU.mult, op1=ALU.add), reads=[Bsm], writes=[Bsm])
                                    pa2, pb2 = P()
                                    S.op("pe", lambda: PE.transpose(out=pa2[0:8, 0:128], in_=cmb, identity=ident), reads=[Bsm, Bcst], writes=[pb2])
                                    S.op("act", lambda: ACT.copy(out=combT[:, t0 + tt * 128:t0 + (tt + 1) * 128], in_=pa2[0:8, 0:128]), reads=[pb2], writes=[BcombT], partial=True)
                        S.barrier()
                    with ExitStack() as pd2:
                        facc = A(pd2, "facc", [128, 8, T], F32)
                        Bfacc = [[Buf("facc%d_%d" % (oc, bi)) for bi in range(5)] for oc in range(8)]
                        with ExitStack() as st:
                            wst = [A(st, "fst%d" % i, [128, 2048], F32) for i in range(2)]; Bwst = [gbuf("fst%d" % i) for i in range(2)]
                            wbf = [A(st, "fbf%d" % i, [128, 2048], BF16) for i in range(6)]; Bwbf = [Buf("fbf%d" % i) for i in range(6)]
                            actT = [A(st, "actT%d" % i, [128, 2, 512], BF16) for i in range(2)]; BactT = [Buf("actT%d" % i) for i in range(2)]
                            sg = [A(st, "sg%d" % i, [128, 512], F32) for i in range(2)]; Bsg = [Buf("sg%d" % i) for i in range(2)]
                            a32, Ba32 = sg, Bsg
                            cbc = [A(st, "cbc0", [128, T], F32)] * 2; Bcbc = [Buf("cbc0")] * 2
                            wc = [0, 0]
                            F = 3584 if moe else 2816
                            nfg = F // 256
                            li = l // 2
                            experts = list(range(8)) if moe else [0]

                            def wsrc(e, fg):
                                if moe:
                                    g_ = mg_d[li, e].rearrange("(k p) n -> p k n", p=128)[:, :, fg * 256:(fg + 1) * 256]
                                    u_ = mu_d[li, e].rearrange("(k p) n -> p k n", p=128)[:, :, fg * 256:(fg + 1) * 256]
                                    d_ = md_d[li, e, fg * 256:(fg + 1) * 256, :].rearrange("(k p) n -> p k n", p=128)
                                else:
                                    g_ = fg_d[li].rearrange("(k p) n -> p k n", p=128)[:, :, fg * 256:(fg + 1) * 256]
                                    u_ = fu_d[li].rearrange("(k p) n -> p k n", p=128)[:, :, fg * 256:(fg + 1) * 256]
                                    d_ = fd_d[li, fg * 256:(fg + 1) * 256, :].rearrange("(k p) n -> p k n", p=128)
                                return g_, u_, d_

                            def getw3(e, fg):
                                res = []
                                for wi, src in enumerate(wsrc(e, fg)):
                                    si = wc[0] % 2; wc[0] += 1
                                    bi_ = wc[1] % 6; wc[1] += 1
                                    kk = 8 if wi < 2 else 2
                                    sv = wst[si].rearrange("p (k n) -> p k n", k=kk)
                                    bv = wbf[bi_].rearrange("p (k n) -> p k n", k=kk)
                                    S.dma("sp", sv, src, writes=[Bwst[si]])
                                    if wi == 1:
                                        S.op("act", lambda: ACT.copy(out=wbf[bi_], in_=wst[si]), reads=[Bwst[si]], writes=[Bwbf[bi_]])
                                    else:
                                        S.op("pool", lambda: GP.tensor_copy(out=wbf[bi_], in_=wst[si]), reads=[Bwst[si]], writes=[Bwbf[bi_]])
                                    res.append((bv, Bwbf[bi_]))
                                return res

                            pend = {}

                            def conv(wi, si, bi_):
                                if wi == 1:
                                    S.op("act", lambda: ACT.copy(out=wbf[bi_], in_=wst[si]), reads=[Bwst[si]], writes=[Bwbf[bi_]])
                                else:
                                    S.op("pool", lambda: GP.tensor_copy(out=wbf[bi_], in_=wst[si]), reads=[Bwst[si]], writes=[Bwbf[bi_]])

                            def stageA(w2):
                                srcs = wsrc(*work[w2])
                                plan = []
                                for wi in range(3):
                                    si = wc[0] % 2; wc[0] += 1
                                    bi_ = wc[1] % 6; wc[1] += 1
                                    plan.append((si, bi_, 8 if wi < 2 else 2, srcs[wi]))
                                pend[w2] = plan
                                for wi in range(2):
                                    si, bi_, kk, src = plan[wi]
                                    S.dma("sp", wst[si].rearrange("p (k n) -> p k n", k=kk), src, writes=[Bwst[si]])
                                wts[w2] = [(wbf[p_[1]].rearrange("p (k n) -> p k n", k=p_[2]), Bwbf[p_[1]]) for p_ in plan]

                            def stageB(w2):
                                plan = pend[w2]
                                for wi in range(2):
                                    conv(wi, plan[wi][0], plan[wi][1])
                                si, bi_, kk, src = plan[2]
                                S.dma("sp", wst[si].rearrange("p (k n) -> p k n", k=kk), src, writes=[Bwst[si]])

                            def stageC(w2):
                                si, bi_, kk, src = pend.pop(w2)[2]
                                conv(2, si, bi_)

                            work = [(e, fg) for e in experts for fg in range(nfg)]
                            items = [(wi_, bi) for wi_ in range(len(work)) for bi in range(len(blks))]
                            wts = {0: getw3(*work[0])}
                            if len(work) > 1:
                                wts[1] = getw3(*work[1])
                            dctr = [0]

                            def GU(idx):
                                wi_, bi = items[idx]
                                e, fg = work[wi_]
                                t0, n = blks[bi]
                                (wg, Bwg), (wu, Bwu), _ = wts[wi_]
                                cb_e, Bcb_e = cbc[0], Bcbc[0]
                                if moe and fg == 0 and bi == 0:
                                    for (t0c, nc_) in blks:
                                        pa, pb_ = ps[4 + dctr[0] % 4], Bps[4 + dctr[0] % 4]; dctr[0] += 1
                                        mm(pa[:, 0:nc_], sel[0:8, e, :], combT[:, t0c:t0c + nc_], [Bcst, BcombT], pb_, True, True)
                                        S.op("act", lambda: ACT.copy(out=cb_e[:, t0c:t0c + nc_], in_=pa[:, 0:nc_]), reads=[pb_], writes=[Bcb_e], partial=(t0c > 0))
                                a_, Ba_ = actT[idx % 2], BactT[idx % 2]
                                for fc in range(2):
                                    pg, Bpg = ps[2 * fc], Bps[2 * fc]
                                    pu, Bpu = ps[2 * fc + 1], Bps[2 * fc + 1]
                                    mmacc(pg[:, 0:n], [(wg[:, k, fc * 128:(fc + 1) * 128], h2T[:, k, t0:t0 + n]) for k in range(8)], [Bwg, Bh2], Bpg)
                                    mmacc(pu[:, 0:n], [(wu[:, k, fc * 128:(fc + 1) * 128], h2T[:, k, t0:t0 + n]) for k in range(8)], [Bwu, Bh2], Bpu)
                                    s_, Bs_ = sg[fc], Bsg[fc]
                                    S.op("act", lambda: ACT.activation(out=s_[:, 0:n], in_=pg[:, 0:n], func=AF.Silu), reads=[Bpg], writes=[Bs_])
                                    if moe:
                                        S.op("dve", lambda: V.tensor_tensor(out=s_[:, 0:n], in0=pu[:, 0:n], in1=s_[:, 0:n], op=ALU.mult), reads=[Bpu, Bs_], writes=[Bs_])
                                        S.op("pool", lambda: GP.tensor_tensor(out=a_[:, fc, 0:n], in0=s_[:, 0:n], in1=cb_e[:, t0:t0 + n], op=ALU.mult),
                                             reads=[Bs_, Bcb_e], writes=[Ba_], partial=(fc > 0))
                                    else:
                                        S.op("dve", lambda: V.tensor_tensor(out=a_[:, fc, 0:n], in0=pu[:, 0:n], in1=s_[:, 0:n], op=ALU.mult), reads=[Bpu, Bs_], writes=[Ba_], partial=(fc > 0))

                            def DN(idx):
                                wi_, bi = items[idx]
                                t0, n = blks[bi]
                                _, _, (wd, Bwd) = wts[wi_]
                                a_, Ba_ = actT[idx % 2], BactT[idx % 2]
                                for oc in range(8):
                                    pd_, Bpd_ = ps[4 + dctr[0] % 4], Bps[4 + dctr[0] % 4]; dctr[0] += 1
                                    mmacc(pd_[:, 0:n], [(wd[:, fc, oc * 128:(oc + 1) * 128], a_[:, fc, 0:n]) for fc in range(2)], [Bwd, Ba_], Bpd_)
                                    if wi_ == 0:
                                        S.op("act", lambda: ACT.copy(out=facc[:, oc, t0:t0 + n], in_=pd_[:, 0:n]), reads=[Bpd_], writes=[Bfacc[oc][bi]])
                                    else:
                                        S.op("dve", lambda: V.tensor_tensor(out=facc[:, oc, t0:t0 + n], in0=pd_[:, 0:n], in1=facc[:, oc, t0:t0 + n], op=ALU.add),
                                             reads=[Bpd_, Bfacc[oc][bi]], writes=[Bfacc[oc][bi]])

                            GU(0)
                            for idx in range(len(items)):
                                wi_, bi = items[idx]
                                if idx + 1 < len(items):
                                    GU(idx + 1)
                                DN(idx)
                                if bi == 1 and (wi_ + 1) in pend:
                                    stageB(wi_ + 1)
                                if bi == 3 and (wi_ + 1) in pend:
                                    stageC(wi_ + 1)
                                if bi == len(blks) - 1:
                                    wts.pop(wi_)
                                    if wi_ + 2 < len(work):
                                        stageA(wi_ + 2)
                            S.barrier()
                        with ExitStack() as st:
                            x1b = [A(st, "x1b%d" % i, [128, 8, 256], F32) for i in range(2)]; Bx1b = [gbuf("x1b%d" % i) for i in range(2)]
                            Y = A(st, "Y2", [128, 8, 256], F32); BY_ = Buf("Y2")
                            lnt = (A(st, "Ysq2", [128, 8, 256], F32), Buf("Ysq2"), A(st, "mean2", [128, 256], F32), Buf("mean2"), A(st, "msq2", [128, 256], F32), Buf("msq2"),
                                   A(st, "rstd2", [128, 256], F32), Buf("rstd2"), A(st, "lntmp2", [128, 8, 256], F32), gbuf("lntmp2"))
                            ot = [A(st, "ot%d" % i, [128, 1024], F32) for i in range(2)]; Bot = [gbuf("ot%d" % i) for i in range(2)]
                            oi = 0
                            for bi3 in range(Teff // 256):
                                t0, n = bi3 * 256, 256
                                bi = t0 // 512
                                j = mj(t0)
                                xb_, Bxb_ = x1b[bi3 % 2], Bx1b[bi3 % 2]
                                S.dma("pool", xb_[:, :, 0:n], X1T[b][:, :, t0:t0 + n], reads=[BX1T[b]], writes=[Bxb_])
                                for oc in range(8):
                                    S.op("act", lambda: ACT.activation(out=Y[:, oc, 0:n], in_=facc[:, oc, t0:t0 + n], func=AF.Identity, scale=modT[:, 40 + oc, j:j + 1]),
                                         reads=[Bfacc[oc][bi], Bmod], writes=[BY_], partial=(oc > 0))
                                    S.op("dve", lambda: V.scalar_tensor_tensor(out=Y[:, oc, 0:n], in0=xb_[:, oc, 0:n], scalar=ALPHA, in1=Y[:, oc, 0:n], op0=ALU.mult, op1=ALU.add),
                                         reads=[Bxb_, BY_], writes=[BY_], partial=True)
                                xn, Bxn = layernorm(lnt, Y, BY_, n, 1, None)
                                if not last:
                                    S.dma("pool", XT[b][:, :, t0:t0 + n], xn[:, :, 0:n], reads=[Bxn], writes=[BXT[b]], partial=True, semb=Bxn)
                                else:
                                    for tt in range(n // 128):
                                        o_, Bo_ = ot[oi % 2], Bot[oi % 2]; oi += 1
                                        for hh in range(2):
                                            pa, pb_ = P()
                                            for k4 in range(4):
                                                k = hh * 4 + k4
                                                S.op("pe", lambda: PE.transpose(out=pa[:, k4 * 128:(k4 + 1) * 128], in_=xn[:, k, tt * 128:(tt + 1) * 128], identity=ident),
                                                     reads=[Bxn, Bcst], writes=[pb_], partial=(k4 > 0))
                                            if hh:
                                                S.op("act", lambda: ACT.copy(out=o_[:, hh * 512:(hh + 1) * 512], in_=pa), reads=[pb_], writes=[Bo_], partial=True)
                                            else:
                                                S.op("dve", lambda: V.tensor_copy(out=o_[:, hh * 512:(hh + 1) * 512], in_=pa), reads=[pb_], writes=[Bo_])
                                        S.dma("sp", out_d[b, t0 + tt * 128:t0 + (tt + 1) * 128, :], o_, reads=[Bo_], writes=[Bout], partial=True, semb=Bo_)
                            S.barrier()
        S.barrier()
    assert S.nsem < 100, S.nsem
    return nc, S


def _consts():
    c = np.zeros((128, NCONST), np.float32)
    s = np.arange(128)[:, None]; l_ = np.arange(128)[None, :]
    c[:, C_ID:C_ID + 128] = np.eye(128)
    c[:, C_ONE:C_ONE + 128] = 1.0
    c[:, C_TRF:C_TRF + 128] = (s <= l_)
    c[:, C_TRB:C_TRB + 128] = (s >= l_)
    c[:, C_MNF:C_MNF + 128] = np.where(s <= l_, 0.0, NEG)
    c[:, C_MNB:C_MNB + 128] = np.where(s >= l_, 0.0, NEG)
    pmat = np.zeros((128, 128), np.float32)
    for base in (0, 64):
        for i in range(32):
            pmat[base + 32 + i, base + i] = -1.0
            pmat[base + i, base + 32 + i] = 1.0
    c[:, C_PM:C_PM + 128] = pmat
    selm = np.zeros((128, 8, 128), np.float32)
    for e in range(8):
        selm[e, e, :] = 1.0
    c[:, C_SEL:C_SEL + 1024] = selm.reshape(128, 1024)
    c[:, C_MN4F:C_MN4F + 512] = np.tile(c[:, C_MNF:C_MNF + 128], (1, 4))
    c[:, C_MN4B:C_MN4B + 512] = np.tile(c[:, C_MNB:C_MNB + 128], (1, 4))
    return c


def _rope():
    t = np.arange(L)
    inv = (10000.0 ** (-np.arange(32, dtype=np.float32) / 32)).astype(np.float32)
    ang_r = (t // 64).astype(np.float32)[:, None] * inv
    ang_c = (t % 64).astype(np.float32)[:, None] * inv
    cos = np.zeros((128, L), np.float32); sin = np.zeros((128, L), np.float32)
    for base, ang in ((0, ang_r), (64, ang_c)):
        cos[base:base + 32] = np.cos(ang).T; cos[base + 32:base + 64] = np.cos(ang).T
        sin[base:base + 32] = np.sin(ang).T; sin[base + 32:base + 64] = np.sin(ang).T
    return np.ascontiguousarray(np.stack([cos, sin], axis=1))


def _bias_gather_index():
    types_j = [0, 1, 2, 14, 15]
    valid = np.zeros((5, 5, 128, 128), bool)
    ridx = np.zeros((5, 5, 128, 128), np.int64)
    cidx = np.zeros((5, 5, 128, 128), np.int64)
    key = np.arange(128); q = np.arange(128)
    kr_l, kc = key // 64, key % 64
    qr_l, qc = q // 64, q % 64
    for ti, j in enumerate(types_j):
        base = min(max(j - 2, 0), 11)
        for s in range(5):
            kr = 2 * (base + s) + kr_l
            qi = 2 * j + qr_l
            r0 = np.clip(qi - 4, 0, 24)
            cs = np.clip(qc - 8, 0, 48)
            vr = (kr[:, None] >= r0[None, :]) & (kr[:, None] < r0[None, :] + 8)
            vc = (kc[:, None] >= cs[None, :]) & (kc[:, None] < cs[None, :] + 16)
            valid[ti, s] = vr & vc
            ridx[ti, s] = np.clip(kr[:, None] - qi[None, :] + 7, 0, 14)
            cidx[ti, s] = np.clip(kc[:, None] - qc[None, :] + 15, 0, 30)
    return valid, ridx, cidx


def _prep_shared(inp):
    f = lambda a: np.ascontiguousarray(np.asarray(a, dtype=np.float32))
    valid, ridx, cidx = _bias_gather_index()
    rpb = f(inp["na_rpb"])
    g = rpb[:, :, ridx, cidx]
    g = np.where(valid[None, None], g, np.float32(NEG)).astype(np.float32)
    btab = np.ascontiguousarray(g.transpose(0, 1, 4, 2, 3, 5)).reshape(4, 8, 128, 25 * 128)
    prm = np.zeros((4, 128, NP_), np.float32)
    bm = f(inp["b_mod"]).reshape(4, 48, 128).transpose(0, 2, 1)
    prm[:, :, P_BMOD:P_BMOD + 48] = bm
    cw = f(inp["conv_w"]).reshape(4, 5, 8, 128).transpose(0, 3, 2, 1)
    prm[:, :, P_CW:P_CW + 40] = cw.reshape(4, 128, 40)
    prm[:, :, P_CB:P_CB + 8] = f(inp["conv_b"]).reshape(4, 8, 128).transpose(0, 2, 1)
    prm[:, :, P_DTB:P_DTB + 16] = f(inp["dt_bias"]).reshape(4, 1, 16)
    prm[:, :, P_ALOG:P_ALOG + 16] = f(inp["a_log"]).reshape(4, 1, 16)
    prm[:, :, P_DSK:P_DSK + 8] = f(inp["d_skip"]).reshape(4, 1, 8)
    prm[:, :, P_NW:P_NW + 512] = f(inp["ssd_norm_w"]).reshape(4, 1, 512)
    prm[:, :, P_LNG:P_LNG + 16] = f(inp["ln_g"]).reshape(4, 2, 8, 128).transpose(0, 3, 1, 2).reshape(4, 128, 16)
    prm[:, :, P_LNB:P_LNB + 16] = f(inp["ln_b"]).reshape(4, 2, 8, 128).transpose(0, 3, 1, 2).reshape(4, 128, 16)
    router = np.ascontiguousarray(f(inp["router_w"]).reshape(2, 8, 128, 8).transpose(0, 2, 1, 3))
    shared = {"consts": _consts(), "rope": _rope(), "prm": prm, "btab": btab, "router": router}
    for k in ("w_mod", "w_in", "w_out", "ffn_w_gate", "ffn_w_up", "ffn_w_down", "moe_w_gate", "moe_w_up", "moe_w_down"):
        shared[k] = f(inp[k])
    return shared


def _core_inputs(inp, shared, core):
    f = lambda a: np.ascontiguousarray(np.asarray(a, dtype=np.float32))
    b0 = core * NB
    c2 = f(inp["c"])[b0:b0 + NB]
    cc = np.concatenate([c2, f(inp["c_ctx"])[None]], axis=0)
    cT = np.ascontiguousarray(cc.reshape(3, 8, 128).transpose(2, 1, 0))
    m = dict(shared)
    m["x"] = f(inp["x"])[b0:b0 + NB]
    m["ctx"] = f(inp["ctx"])[b0:b0 + NB]
    m["cT"] = cT
    return m


_CACHE = {}


def kernel(**inputs):
    if "nc" not in _CACHE:
        _CACHE["nc"] = build(4)[0]
    nc = _CACHE["nc"]
    shared = _prep_shared(inputs)
    n_cores = 8
    in_maps = [_core_inputs(inputs, shared, c) for c in range(n_cores)]
    res = run_bass_kernel_spmd(nc, in_maps, core_ids=list(range(n_cores)))
    out = np.concatenate([np.asarray(r["out"]) for r in res.results], axis=0)
    return out.astype(np.float32)
```

```python
from contextlib import ExitStack
import numpy as np
import concourse.bass as bass
import concourse.mybir as mybir
from concourse.bass_utils import run_bass_kernel_spmd

F32 = mybir.dt.float32
BF16 = mybir.dt.bfloat16
ALU = mybir.AluOpType
AF = mybir.ActivationFunctionType
AX = mybir.AxisListType

EPOCH = 30000
NB, L, C, T, NT = 2, 2048, 256, 2304, 18
BLKS = [(0, 512), (512, 512), (1024, 512), (1536, 512), (2048, 256)]
ALPHA = (2.0 * 4) ** 0.25
NEG = -30000.0
C_ID, C_ONE, C_TRF, C_TRB, C_MNF, C_MNB, C_PM, C_SEL = 0, 128, 256, 384, 512, 640, 768, 896
C_MN4F, C_MN4B = 1920, 2432
NCONST = 896 + 1024 + 1024
P_BMOD, P_CW, P_CB, P_DTB, P_ALOG, P_DSK, P_NW, P_LNG, P_LNB = 0, 48, 88, 96, 112, 128, 136, 648, 664
NP_ = 680


class Buf:
    __slots__ = ("name", "w", "r", "dsem", "dcount")

    def __init__(self, name="b"):
        self.name = name
        self.w = {}
        self.r = {}
        self.dsem = None
        self.dcount = 0


class Sched:
    def __init__(self, nc):
        self.nc = nc
        self.engs = {"pe": nc.tensor, "act": nc.scalar, "dve": nc.vector, "pool": nc.gpsimd, "sp": nc.sync}
        self.sem, self.cnt = {}, {}
        self.seq = {e: 0 for e in self.engs}
        self.seen = {e: {} for e in self.engs}
        self.last = {}
        self.dtoks = {}
        self.nsem = self.nwait = self.ninst = 0
        for e in self.engs:
            self._new_epoch(e)

    def _alloc_sem(self, name):
        self.nsem += 1
        return self.nc.alloc_semaphore("%s_%d" % (name, self.nsem))

    def _new_epoch(self, e):
        self.sem[e] = self._alloc_sem("s_" + e)
        self.cnt[e] = 0

    def _wait(self, eng, tok):
        key, seq, sem, val = tok
        if eng == "pe" and key == "pe":
            return
        if self.seen[eng].get(key, -1) >= seq:
            return
        self.engs[eng].wait_ge(sem, val)
        self.nwait += 1
        self.seen[eng][key] = seq

    def _deps(self, eng, reads, writes, partial):
        for b in reads:
            for t in b.w.values():
                self._wait(eng, t)
        for b in writes:
            for t in b.r.values():
                self._wait(eng, t)
            if not partial:
                for t in b.w.values():
                    self._wait(eng, t)

    def _commit(self, tok, reads, writes, partial):
        key = tok[0]
        for b in reads:
            b.r[key] = tok
        for b in writes:
            if partial and not b.r:
                b.w[key] = tok
            else:
                b.w = {key: tok}
            b.r = {}

    def op(self, eng, fn, reads=(), writes=(), partial=False):
        self._deps(eng, reads, writes, partial)
        ins = fn()
        if self.cnt[eng] >= EPOCH:
            self._new_epoch(eng)
        self.cnt[eng] += 1
        self.seq[eng] += 1
        ins.then_inc(self.sem[eng], 1)
        tok = (eng, self.seq[eng], self.sem[eng], self.cnt[eng])
        self.last[eng] = tok
        self._commit(tok, reads, writes, partial)
        self.ninst += 1
        return tok

    def dma(self, q, out, in_, reads=(), writes=(), partial=False, semb=None, **kw):
        self._deps(q, reads, writes, partial)
        ins = self.engs[q].dma_start(out=out, in_=in_, **kw)
        b = semb if semb is not None else writes[0]
        if b.dsem is None:
            b.dsem = self._alloc_sem("d_" + b.name)
        b.dcount += 16
        ins.then_inc(b.dsem, 16)
        tok = (("d", b.dsem.num), b.dcount, b.dsem, b.dcount)
        self.dtoks[tok[0]] = tok
        self._commit(tok, reads, writes, partial)
        self.ninst += 1
        return tok

    def barrier(self):
        toks = list(self.last.values()) + list(self.dtoks.values())
        for e in self.engs:
            for t in toks:
                self._wait(e, t)
        self.dtoks = {}


def build(n_layers=4, debug=False):
    nc = bass.Bass("TRN2", target_bir_lowering=False)
    S = Sched(nc)
    uid = [0]

    def D(name, shape, dt=F32, kind="ExternalInput"):
        if kind == "Internal" and debug:
            kind = "ExternalOutput"
        return nc.dram_tensor(name, shape, dt, kind=kind).ap()

    def A(st, name, shape, dt):
        uid[0] += 1
        return st.enter_context(nc.sbuf_tensor("%s_%d" % (name, uid[0]), shape, dt))[:]

    gb = {}

    def gbuf(name):
        if name not in gb:
            gb[name] = Buf(name)
        return gb[name]

    def dump(name, ap, buf, dt_=F32):
        if not debug:
            return
        o = nc.dram_tensor("dbg_" + name, list(ap.shape), dt_, kind="ExternalOutput").ap()
        S.dma("sp", o, ap, reads=[buf], writes=[gbuf("dbg_" + name)])

    x_d = D("x", [NB, L, 1024]); ctx_d = D("ctx", [NB, C, 1024]); cT_d = D("cT", [128, 8, 3])
    const_d = D("consts", [128, NCONST]); rope_d = D("rope", [128, 2, L]); prm_d = D("prm", [4, 128, NP_])
    wmod_d = D("w_mod", [4, 1024, 6144]); win_d = D("w_in", [4, 1024, 3088]); wout_d = D("w_out", [4, 1024, 1024])
    btab_d = D("btab", [4, 8, 128, 25 * 128])
    fg_d = D("ffn_w_gate", [2, 1024, 2816]); fu_d = D("ffn_w_up", [2, 1024, 2816]); fd_d = D("ffn_w_down", [2, 2816, 1024])
    rt_d = D("router", [2, 128, 8, 8])
    mg_d = D("moe_w_gate", [2, 8, 1024, 3584]); mu_d = D("moe_w_up", [2, 8, 1024, 3584]); md_d = D("moe_w_down", [2, 8, 3584, 1024])
    out_d = D("out", [NB, L, 1024], kind="ExternalOutput")
    XT = [D("XT%d" % b, [128, 8, T], kind="Internal") for b in range(NB)]
    X1T = [D("X1T%d" % b, [128, 8, T], kind="Internal") for b in range(NB)]
    QT = [D("QT%d" % b, [128, 4, T], BF16, kind="Internal") for b in range(NB)]
    KT = [D("KT%d" % b, [128, 4, T], BF16, kind="Internal") for b in range(NB)]
    VV = [D("VV%d" % b, [128, NT, 520], BF16, kind="Internal") for b in range(NB)]
    ZS = [D("ZS%d" % b, [128, NT, 512], BF16, kind="Internal") for b in range(NB)]
    CAT = [D("CAT%d" % b, [128, 8, T], BF16, kind="Internal") for b in range(NB)]
    BXT = [gbuf("XT%d" % b) for b in range(NB)]; BX1T = [gbuf("X1T%d" % b) for b in range(NB)]
    BQT = [gbuf("QT%d" % b) for b in range(NB)]; BKT = [gbuf("KT%d" % b) for b in range(NB)]
    BVV = [gbuf("VV%d" % b) for b in range(NB)]; BZS = [gbuf("ZS%d" % b) for b in range(NB)]
    BCAT = [gbuf("CAT%d" % b) for b in range(NB)]
    Bout = gbuf("out")

    ps = [nc.alloc_psum_tensor("ps%d" % i, [128, 512], F32).ap() for i in range(8)]
    Bps = [Buf("ps%d" % i) for i in range(8)]
    pctr = [0]

    def P():
        i = pctr[0] % 8
        pctr[0] += 1
        return ps[i], Bps[i]

    V, ACT, PE, GP = nc.vector, nc.scalar, nc.tensor, nc.gpsimd

    def mm(out, lhsT, rhs, rd, wb, first, last, start=None):
        S.op("pe", lambda: PE.matmul(out, lhsT=lhsT, rhs=rhs, start=(first if start is None else start), stop=last), reads=rd, writes=[wb], partial=not first)

    def mmacc(out, pairs, rd, wb):
        n = len(pairs)
        for i, (l_, r_) in enumerate(pairs):
            mm(out, l_, r_, rd, wb, i == 0, i == n - 1)

    with ExitStack() as G:
        cst = A(G, "cst", [128, NCONST], F32); Bcst = gbuf("cst")
        S.dma("sp", cst, const_d, writes=[Bcst])
        ident = cst[:, C_ID:C_ID + 128]; ones = cst[:, C_ONE:C_ONE + 128]
        tri = [cst[:, C_TRF:C_TRF + 128], cst[:, C_TRB:C_TRB + 128]]
        mneg = [cst[:, C_MNF:C_MNF + 128], cst[:, C_MNB:C_MNB + 128]]
        pm = cst[:, C_PM:C_PM + 128]
        mneg4 = [cst[:, C_MN4F:C_MN4F + 512], cst[:, C_MN4B:C_MN4B + 512]]
        sel = cst[:, C_SEL:C_SEL + 1024].rearrange("p (e m) -> p e m", e=8)
        identb = A(G, "identb", [128, 128], BF16); Bidb = Buf("idb")
        S.op("dve", lambda: V.tensor_copy(out=identb, in_=ident), reads=[Bcst], writes=[Bidb])
        modT = A(G, "modT", [128, 48, 3], F32); Bmod = Buf("mod")
        prm = A(G, "prm", [128, NP_], F32); Bprm = gbuf("prm")
        negA = A(G, "negA", [128, 16], F32); BnegA = Buf("negA")
        sT = A(G, "sT", [128, 8, 3], F32); BsT = gbuf("sT")
        S.dma("sp", sT, cT_d, writes=[BsT])
        S.op("act", lambda: ACT.activation(out=sT, in_=sT, func=AF.Silu), reads=[BsT], writes=[BsT])

        with ExitStack() as st:
            xin = [A(st, "xin%d" % i, [128, 1024], F32) for i in range(2)]; Bxin = [gbuf("xin%d" % i) for i in range(2)]
            xo = [A(st, "xo%d" % i, [128, 8, 512], F32) for i in range(2)]; Bxo = [Buf("xo%d" % i) for i in range(2)]
            ti = 0
            for b in range(NB):
                for bi, (t0, n) in enumerate(BLKS):
                    o, Bo = xo[bi % 2], Bxo[bi % 2]
                    for tt in range(n // 128):
                        t = t0 + tt * 128
                        src = x_d[b, t:t + 128, :] if t < L else ctx_d[b, t - L:t - L + 128, :]
                        xi, Bxi = xin[ti % 2], Bxin[ti % 2]; ti += 1
                        S.dma("sp", xi, src, writes=[Bxi])
                        for hh in range(2):
                            pa, pb_ = P()
                            for k4 in range(4):
                                k = hh * 4 + k4
                                S.op("pe", lambda: PE.transpose(out=pa[:, k4 * 128:(k4 + 1) * 128], in_=xi[:, k * 128:(k + 1) * 128], identity=ident),
                                     reads=[Bxi, Bcst], writes=[pb_], partial=(k4 > 0))
                            S.op("act" if hh else "dve",
                                 (lambda: ACT.copy(out=o[:, hh * 4:hh * 4 + 4, tt * 128:(tt + 1) * 128], in_=pa.rearrange("p (k c) -> p k c", k=4))) if hh else
                                 (lambda: V.tensor_copy(out=o[:, hh * 4:hh * 4 + 4, tt * 128:(tt + 1) * 128], in_=pa.rearrange("p (k c) -> p k c", k=4))),
                                 reads=[pb_], writes=[Bo], partial=not (tt == 0 and hh == 0))
                    S.dma("pool", XT[b][:, :, t0:t0 + n], o[:, :, 0:n], reads=[Bo], writes=[BXT[b]], partial=True, semb=Bo)
            S.barrier()

        for l in range(n_layers):
            last = (l == n_layers - 1)
            moe = (l % 2 == 1)
            win_l = win_d[l].rearrange("(k p) n -> p k n", p=128)
            S.dma("sp", prm, prm_d[l], writes=[Bprm])
            S.op("act", lambda: ACT.activation(out=negA, in_=prm[:, P_ALOG:P_ALOG + 16], func=AF.Exp), reads=[Bprm], writes=[BnegA])
            S.op("dve", lambda: V.tensor_scalar(out=negA, in0=negA, scalar1=-1.0, scalar2=None, op0=ALU.mult), reads=[BnegA], writes=[BnegA])
            with ExitStack() as st:
                wm = [A(st, "wm%d" % i, [128, 8, 256], F32) for i in range(4)]; Bwm = [gbuf("wm%d" % i) for i in range(4)]
                wmv = wmod_d[l].rearrange("(k p) n -> p k n", p=128)
                for pc in range(24):
                    w_, Bw_ = wm[pc % 4], Bwm[pc % 4]
                    S.dma(("sp", "pool", "act", "pool")[pc % 4], w_, wmv[:, :, pc * 256:(pc + 1) * 256], writes=[Bw_])
                    for oc in range(2):
                        m = pc * 2 + oc
                        pa, pb_ = P()
                        mmacc(pa[:, 0:3], [(w_[:, k, oc * 128:(oc + 1) * 128], sT[:, k, :]) for k in range(8)], [Bw_, BsT], pb_)
                        S.op("dve", lambda: V.tensor_scalar(out=modT[:, m, :], in0=pa[:, 0:3], scalar1=prm[:, P_BMOD + m:P_BMOD + m + 1],
                                                            scalar2=(1.0 if (m // 8) in (1, 2, 4, 5) else 0.0), op0=ALU.add, op1=ALU.add),
                             reads=[pb_, Bprm], writes=[Bmod], partial=(m > 0))
                S.barrier()

            for b in range(NB):
                def mj(t0):
                    return b if t0 < L else 2
                with ExitStack() as pst:
                    XS = A(pst, "XS", [128, NT, 512], BF16); BXS = Buf("XS")
                    BT = A(pst, "BT", [128, 2, T], BF16); BBT = Buf("BT")
                    CT = A(pst, "CT", [128, 2, T], BF16); BCT = Buf("CT")
                    Btok = A(pst, "Btok", [128, NT, 2, 128], BF16); BBtok = Buf("Btok")
                    dt = A(pst, "dt", [128, NT, 16], F32); Bdt = Buf("dt")
                    da = A(pst, "da", [128, NT, 16], F32); Bda = Buf("da")
                    with ExitStack() as st:
                        h1T = A(st, "h1T", [128, 8, T], BF16); Bh1 = Buf("h1T")
                        wdt = A(st, "wdt", [128, 8, 16], F32); Bwdt = gbuf("wdt")
                        S.dma("sp", wdt, win_l[:, :, 3072:3088], writes=[Bwdt])
                        with ExitStack() as st1:
                            xt = [A(st1, "xt%d" % i, [128, 8, 256], F32) for i in range(2)]; Bxt = [gbuf("xt%d" % i) for i in range(2)]
                            h1f = [A(st1, "h1f%d" % i, [128, 8, 256], F32) for i in range(2)]; Bh1f = [Buf("h1f%d" % i) for i in range(2)]
                            dtr = A(st1, "dtr", [128, 16], F32); Bdtr = Buf("dtr")
                            for bi in range(T // 256):
                                t0 = bi * 256
                                j = mj(t0)
                                x_, Bx_ = xt[bi % 2], Bxt[bi % 2]
                                hf, Bhf = h1f[bi % 2], Bh1f[bi % 2]
                                S.dma("pool", x_, XT[b][:, :, t0:t0 + 256], reads=[BXT[b]], writes=[Bx_])
                                for k in range(8):
                                    S.op("dve", lambda: V.tensor_scalar(out=hf[:, k, :], in0=x_[:, k, :], scalar1=modT[:, 8 + k, j:j + 1], scalar2=modT[:, k, j:j + 1],
                                                                        op0=ALU.mult, op1=ALU.add), reads=[Bx_, Bmod], writes=[Bhf], partial=(k > 0))
                                S.op("act", lambda: ACT.copy(out=h1T[:, :, t0:t0 + 256], in_=hf), reads=[Bhf], writes=[Bh1], partial=True)
                                for tt in range(2):
                                    t = bi * 2 + tt
                                    pa, pb_ = P()
                                    mmacc(pa[:, 0:16], [(hf[:, k, tt * 128:(tt + 1) * 128], wdt[:, k, :]) for k in range(8)], [Bhf, Bwdt], pb_)
                                    S.op("dve", lambda: V.tensor_tensor(out=dtr, in0=pa[:, 0:16], in1=prm[:, P_DTB:P_DTB + 16], op=ALU.add), reads=[pb_, Bprm], writes=[Bdtr])
                                    S.op("act", lambda: ACT.activation(out=dtr, in_=dtr, func=AF.Exp), reads=[Bdtr], writes=[Bdtr])
                                    S.op("act", lambda: ACT.activation(out=dt[:, t, :], in_=dtr, func=AF.Ln, bias=1.0), reads=[Bdtr], writes=[Bdt], partial=True)
                                    S.op("dve", lambda: V.tensor_tensor(out=da[:, t, :], in0=dt[:, t, :], in1=negA, op=ALU.mult), reads=[Bdt, BnegA], writes=[Bda], partial=True)
                            S.barrier()
                        with ExitStack() as st2:
                            wst = [A(st2, "wst%d" % i, [128, 8, 256], F32) for i in range(2)]; Bwst = [gbuf("wst%d" % i) for i in range(2)]
                            wbf = [A(st2, "wbf%d" % i, [128, 8, 256], BF16) for i in range(2)]; Bwbf = [Buf("wbf%d" % i) for i in range(2)]
                            qks = [A(st2, "qks%d" % i, [128, T], BF16) for i in range(2)]; Bqks = [gbuf("qks%d" % i) for i in range(2)]
                            vs = [A(st2, "vs%d" % i, [128, 8, 65], BF16) for i in range(2)]; Bvs = [gbuf("vs%d" % i) for i in range(2)]
                            zs = [A(st2, "zs%d" % i, [128, 256], BF16) for i in range(2)]; Bzs = [gbuf("zs%d" % i) for i in range(2)]
                            U = A(st2, "U", [128, 2312], F32); BU = Buf("U")
                            acc = A(st2, "cacc", [128, T], F32); Bacc = Buf("cacc")
                            xsT = A(st2, "xsT", [128, T], BF16); BxsT = Buf("xsT")
                            rp = [A(st2, "rp%d" % i, [128, 2, 512], F32) for i in range(2)]; Brp = [gbuf("rp%d" % i) for i in range(2)]
                            dg = [A(st2, "dg%d" % i, [128, 5, 128], F32) for i in range(2)]; Bdg = [Buf("dg%d" % i) for i in range(2)]
                            tm1 = A(st2, "tm1", [128, 512], F32); Btm1 = Buf("tm1")
                            tm2 = A(st2, "tm2", [128, 512], F32); Btm2 = Buf("tm2")
                            S.op("dve", lambda: V.memset(U, 0.0), writes=[BU])
                            for i in range(2):
                                S.op("dve", lambda: V.memset(vs[i], 1.0), writes=[Bvs[i]])
                            cnt = [0, 0, 0, 0]

                            def getw(pi, slot):
                                i = slot % 2
                                S.dma("sp", wst[i], win_l[:, :, pi * 256:(pi + 1) * 256], writes=[Bwst[i]])
                                if pi % 2:
                                    S.op("act", lambda: ACT.copy(out=wbf[i], in_=wst[i]), reads=[Bwst[i]], writes=[Bwbf[i]])
                                else:
                                    S.op("pool", lambda: GP.tensor_copy(out=wbf[i], in_=wst[i]), reads=[Bwst[i]], writes=[Bwbf[i]])
                                return wbf[i], Bwbf[i]

                            porder = [8, 0, 9, 1, 10, 2, 11, 3, 4, 5, 6, 7]
                            nxt = getw(porder[0], 0)
                            for pidx, pi in enumerate(porder):
                                wb, Bwb = nxt
                                if pidx + 1 < 12:
                                    nxt = getw(porder[pidx + 1], pidx + 1)
                                grp, half = pi // 2, pi % 2
                                if grp in (0, 1):
                                    for oc2 in range(2):
                                        oc = half * 2 + oc2
                                        q_, Bq_ = qks[cnt[0] % 2], Bqks[cnt[0] % 2]; cnt[0] += 1
                                        for (t0, n) in BLKS:
                                            pa, pb_ = P()
                                            mmacc(pa[:, 0:n], [(wb[:, k, oc2 * 128:(oc2 + 1) * 128], h1T[:, k, t0:t0 + n]) for k in range(8)], [Bwb, Bh1], pb_)
                                            S.op("act", lambda: ACT.activation(out=q_[:, t0:t0 + n], in_=pa[:, 0:n], func=AF.Copy, scale=(0.125 if grp == 0 else 1.0)),
                                                 reads=[pb_], writes=[Bq_], partial=True)
                                        dst, Bdst = (QT[b], BQT[b]) if grp == 0 else (KT[b], BKT[b])
                                        S.dma("pool", dst[:, oc, :], q_, reads=[Bq_], writes=[Bdst], partial=True, semb=Bq_)
                                elif grp == 2:
                                    for t in range(NT):
                                        pa, pb_ = P()
                                        mmacc(pa[:, 0:256], [(h1T[:, k, t * 128:(t + 1) * 128], wb[:, k, :]) for k in range(8)], [Bwb, Bh1], pb_)
                                        v_, Bv_ = vs[cnt[1] % 2], Bvs[cnt[1] % 2]; cnt[1] += 1
                                        S.op("dve", lambda: V.tensor_copy(out=v_[:, 0:4, 0:64], in_=pa[:, 0:256].rearrange("p (h d) -> p h d", h=4)),
                                             reads=[pb_], writes=[Bv_])
                                        S.dma("pool", VV[b][:, t, half * 260:half * 260 + 260].rearrange("p (h d) -> p h d", h=4), v_[:, 0:4, :],
                                              reads=[Bv_], writes=[BVV[b]], partial=True, semb=Bv_)
                                elif grp == 3:
                                    for t in range(NT):
                                        pa, pb_ = P()
                                        mmacc(pa[:, 0:256], [(h1T[:, k, t * 128:(t + 1) * 128], wb[:, k, :]) for k in range(8)], [Bwb, Bh1], pb_)
                                        z_, Bz_ = zs[cnt[2] % 2], Bzs[cnt[2] % 2]; cnt[2] += 1
                                        S.op("act", lambda: ACT.activation(out=z_, in_=pa[:, 0:256], func=AF.Silu), reads=[pb_], writes=[Bz_])
                                        S.dma("pool", ZS[b][:, t, half * 256:(half + 1) * 256], z_, reads=[Bz_], writes=[BZS[b]], partial=True, semb=Bz_)
                                else:
                                    for oc2 in range(2):
                                        ch = (pi - 8) * 2 + oc2
                                        for (t0, n) in BLKS:
                                            pa, pb_ = P()
                                            mmacc(pa[:, 0:n], [(wb[:, k, oc2 * 128:(oc2 + 1) * 128], h1T[:, k, t0:t0 + n]) for k in range(8)], [Bwb, Bh1], pb_)
                                            off = 2 + t0 if t0 < L else 2054 + (t0 - L)
                                            S.op("act", lambda: ACT.copy(out=U[:, off:off + n], in_=pa[:, 0:n]), reads=[pb_], writes=[BU], partial=(t0 > 0))
                                        first = True
                                        for (o, t0, n) in [(2, 0, L), (2054, L, C)]:
                                            S.op("dve", lambda: V.tensor_scalar(out=acc[:, t0:t0 + n], in0=U[:, o - 2:o - 2 + n], scalar1=prm[:, P_CW + ch * 5:P_CW + ch * 5 + 1],
                                                                                scalar2=None, op0=ALU.mult), reads=[BU, Bprm], writes=[Bacc], partial=not first)
                                            first = False
                                            for jj in range(1, 5):
                                                S.op("dve", lambda: V.scalar_tensor_tensor(out=acc[:, t0:t0 + n], in0=U[:, o - 2 + jj:o - 2 + jj + n],
                                                                                           scalar=prm[:, P_CW + ch * 5 + jj:P_CW + ch * 5 + jj + 1],
                                                                                           in1=acc[:, t0:t0 + n], op0=ALU.mult, op1=ALU.add),
                                                     reads=[BU, Bprm, Bacc], writes=[Bacc])
                                        cbias = prm[:, P_CB + ch:P_CB + ch + 1]
                                        if ch < 4:
                                            S.op("act", lambda: ACT.activation(out=xsT, in_=acc, func=AF.Silu, bias=cbias), reads=[Bacc, Bprm], writes=[BxsT])
                                            for t4 in range(0, NT, 4):
                                                nt = min(4, NT - t4)
                                                pa, pb_ = P()
                                                pab = pa.bitcast(BF16)
                                                for i in range(nt):
                                                    S.op("pe", lambda: PE.transpose(out=pab[:, i * 128:(i + 1) * 128], in_=xsT[:, (t4 + i) * 128:(t4 + i + 1) * 128], identity=identb),
                                                         reads=[BxsT, Bidb], writes=[pb_], partial=(i > 0))
                                                S.op("dve", lambda: V.tensor_copy(out=XS[:, t4:t4 + nt, ch * 128:(ch + 1) * 128],
                                                                                  in_=pab[:, 0:nt * 128].rearrange("p (a c) -> p a c", a=nt)),
                                                     reads=[pb_], writes=[BXS], partial=True)
                                        else:
                                            g = ch % 2
                                            dstT, BdstT = (BT, BBT) if ch < 6 else (CT, BCT)
                                            S.op("act", lambda: ACT.activation(out=acc, in_=acc, func=AF.Silu, bias=cbias), reads=[Bacc, Bprm], writes=[Bacc])
                                            for bi in range(4):
                                                t0 = bi * 512
                                                r_, Br_ = rp[cnt[3] % 2], Brp[cnt[3] % 2]; cnt[3] += 1
                                                S.dma("pool", r_, rope_d[:, :, t0:t0 + 512], writes=[Br_])
                                                pa, pb_ = P()
                                                mm(pa, pm, acc[:, t0:t0 + 512], [Bcst, Bacc], pb_, True, True)
                                                S.op("pool", lambda: GP.tensor_tensor(out=tm1, in0=acc[:, t0:t0 + 512], in1=r_[:, 0, :], op=ALU.mult), reads=[Bacc, Br_], writes=[Btm1])
                                                S.op("dve", lambda: V.tensor_tensor(out=tm2, in0=pa, in1=r_[:, 1, :], op=ALU.mult), reads=[pb_, Br_], writes=[Btm2])
                                                S.op("dve", lambda: V.tensor_tensor(out=dstT[:, g, t0:t0 + 512], in0=tm1, in1=tm2, op=ALU.add), reads=[Btm1, Btm2], writes=[BdstT], partial=True)
                                            S.op("act", lambda: ACT.copy(out=dstT[:, g, L:T], in_=acc[:, L:T]), reads=[Bacc], writes=[BdstT], partial=True)
                                            if ch < 6:
                                                for t4 in range(0, NT, 4):
                                                    nt = min(4, NT - t4)
                                                    pa, pb_ = P()
                                                    pab = pa.bitcast(BF16)
                                                    for i in range(nt):
                                                        S.op("pe", lambda: PE.transpose(out=pab[:, i * 128:(i + 1) * 128], in_=BT[:, g, (t4 + i) * 128:(t4 + i + 1) * 128], identity=identb),
                                                             reads=[BBT, Bidb], writes=[pb_], partial=(i > 0))
                                                    S.op("dve", lambda: V.tensor_copy(out=Btok[:, t4:t4 + nt, g, :], in_=pab[:, 0:nt * 128].rearrange("p (a c) -> p a c", a=nt)),
                                                         reads=[pb_], writes=[BBtok], partial=True)
                            S.barrier()

                    if l == 0 and b == 0:
                        dump("XS", XS, BXS, BF16); dump("dt", dt, Bdt); dump("da", da, Bda); dump("BT", BT, BBT, BF16); dump("CT", CT, BCT, BF16); dump("Btok", Btok, BBtok, BF16)
                        S.barrier()
                    with ExitStack() as st:
                        Yacc = A(st, "Yacc", [128, NT, 512], F32); BY = [Buf("Y%d" % t) for t in range(NT)]
                        car = [A(st, "car%d" % d, [128, 512], F32) for d in range(2)]; Bcar = [Buf("car%d" % d) for d in range(2)]
                        carb = [A(st, "carb%d" % d, [128, 512], BF16) for d in range(2)]; Bcarb = [Buf("carb%d" % d) for d in range(2)]
                        cats = A(st, "cats", [128, 4, T], BF16); Bcats = gbuf("cats")
                        cs = A(st, "cs", [128, 16], F32); Bcs = Buf("cs")
                        sc = A(st, "scl", [128, 6, 8], F32); Bsc = Buf("scl")
                        XDT = A(st, "XDT", [128, 512], BF16); BXDT = Buf("XDT")
                        XE = A(st, "XE", [128, 512], BF16); BXE = Buf("XE")
                        R = [A(st, "R%d" % i, [128, 128], F32) for i in range(2)]; BR = [Buf("R%d" % i) for i in range(2)]
                        LT = [A(st, "LT%d" % i, [128, 128], F32) for i in range(2)]; BLT = [Buf("LT%d" % i) for i in range(2)]
                        MT = [A(st, "MT%d" % i, [128, 128], BF16) for i in range(2)]; BMT = [Buf("MT%d" % i) for i in range(2)]
                        tY = A(st, "tY", [128, 512], F32); BtY = Buf("tY")
                        dsk = prm[:, P_DSK:P_DSK + 8]

                        def bc8(ap8):
                            return ap8.unsqueeze(2).to_broadcast([128, 8, 64])

                        def v3(ap512):
                            return ap512.rearrange("p (h d) -> p h d", h=8)

                        for t in range(NT):
                            S.op("dve", lambda: V.tensor_tensor(out=v3(Yacc[:, t, :]), in0=v3(XS[:, t, :]), in1=bc8(dsk), op=ALU.mult), reads=[BXS, Bprm], writes=[BY[t]])
                        cs2 = [A(st, "cs2%d" % i, [128, 16], F32) for i in range(2)]; Bcs2 = [Buf("cs2%d" % i) for i in range(2)]
                        sc2 = [A(st, "sc2%d" % i, [128, 6, 8], F32) for i in range(2)]; Bsc2 = [Buf("sc2%d" % i) for i in range(2)]
                        XDT2 = [A(st, "XDT2%d" % i, [128, 512], BF16) for i in range(2)]; BXDT2 = [Buf("XDT2%d" % i) for i in range(2)]
                        XE2 = [A(st, "XE2%d" % i, [128, 512], BF16) for i in range(2)]; BXE2 = [Buf("XE2%d" % i) for i in range(2)]
                        LTa = [A(st, "LTa%d" % i, [128, 1024], F32) for i in range(2)]; BLTa = [Buf("LTa%d" % i) for i in range(2)]
                        MTa = [A(st, "MTa%d" % i, [128, 1024], BF16) for i in range(2)]; BMTa = [Buf("MTa%d" % i) for i in range(2)]
                        tY2 = [A(st, "tY2%d" % i, [128, 512], F32) for i in range(2)]; BtY2 = [Buf("tY2%d" % i) for i in range(2)]
                        cc = 0
                        for d in range(2):
                            S.op("dve", lambda: V.memset(car[d], 0.0), writes=[Bcar[d]])
                            S.op("dve", lambda: V.memset(carb[d], 0.0), writes=[Bcarb[d]])
                            order = [16, 17] + list(range(16)) if d == 0 else [17, 16] + list(range(15, -1, -1))
                            for t in order:
                                ci = cc % 2; cc += 1
                                cs, Bcs = cs2[ci], Bcs2[ci]
                                sc, Bsc = sc2[ci], Bsc2[ci]
                                XDT, BXDT = XDT2[ci], BXDT2[ci]
                                XE, BXE = XE2[ci], BXE2[ci]
                                LTl, BLTl = LTa[ci], BLTa[ci]
                                MTl, BMTl = MTa[ci], BMTa[ci]
                                tY, BtY = tY2[ci], BtY2[ci]
                                tsl = slice(t * 128, (t + 1) * 128)
                                dav = da[:, t, d * 8:(d + 1) * 8]
                                dtv = dt[:, t, d * 8:(d + 1) * 8]
                                pcb, Bpcb = ps[0], Bps[0]
                                for g in range(2):
                                    mm(pcb[:, g * 128:(g + 1) * 128], BT[:, g, tsl], CT[:, g, tsl], [BBT, BCT], Bpcb, g == 0, True, start=True)
                                pcs, Bpcs = ps[1], Bps[1]
                                mm(pcs[:, 0:8], tri[d], dav, [Bcst, Bda], Bpcs, True, True)
                                mm(pcs[:, 8:16], ones, dav, [Bcst, Bda], Bpcs, False, True, start=True)
                                pl = [ps[5], ps[6]]; Bpl = [Bps[5], Bps[6]]
                                for h in range(8):
                                    hb, hq = h // 4, h % 4
                                    mm(pl[hb][:, hq * 128:(hq + 1) * 128], dav[:, h:h + 1].to_broadcast([128, 128]), tri[d], [Bcst, Bda], Bpl[hb], hq == 0, False, start=(hq == 0))
                                for hb in range(2):
                                    mm(pl[hb], ident, mneg4[d], [Bcst], Bpl[hb], False, True, start=False)
                                S.op("act", lambda: ACT.copy(out=cs, in_=pcs[:, 0:16]), reads=[Bpcs], writes=[Bcs])
                                S.op("dve", lambda: V.tensor_scalar(out=sc[:, 0, :], in0=cs[:, 0:8], scalar1=-1.0, scalar2=None, op0=ALU.mult), reads=[Bcs], writes=[Bsc])
                                S.op("dve", lambda: V.tensor_tensor(out=sc[:, 5, :], in0=cs[:, 8:16], in1=cs[:, 0:8], op=ALU.subtract), reads=[Bcs], writes=[Bsc], partial=True)
                                S.op("act", lambda: ACT.activation(out=sc[:, 1, :], in_=cs[:, 0:8], func=AF.Exp), reads=[Bcs], writes=[Bsc], partial=True)
                                S.op("act", lambda: ACT.activation(out=sc[:, 3, :], in_=cs[:, 8:16], func=AF.Exp), reads=[Bcs], writes=[Bsc], partial=True)
                                S.op("act", lambda: ACT.activation(out=sc[:, 2, :], in_=sc[:, 5, :], func=AF.Exp), reads=[Bsc], writes=[Bsc])
                                S.op("dve", lambda: V.tensor_tensor(out=sc[:, 4, :], in0=sc[:, 2, :], in1=dtv, op=ALU.mult), reads=[Bsc, Bdt], writes=[Bsc])
                                S.op("dve", lambda: V.tensor_tensor(out=v3(XDT), in0=v3(XS[:, t, :]), in1=bc8(dtv), op=ALU.mult), reads=[BXS, Bdt], writes=[BXDT])
                                for h in range(8):
                                    hs = slice(h * 64, (h + 1) * 64)
                                    S.op("act", lambda: ACT.activation(out=XE[:, hs], in_=XS[:, t, hs], func=AF.Identity, scale=sc[:, 4, h:h + 1]), reads=[BXS, Bsc], writes=[BXE], partial=(h > 0))
                                for h in range(8):
                                    hb, hq = h // 4, h % 4
                                    S.op("act", lambda: ACT.activation(out=LTl[:, h * 128:(h + 1) * 128], in_=pl[hb][:, hq * 128:(hq + 1) * 128], func=AF.Exp, bias=sc[:, 0, h:h + 1]),
                                         reads=[Bpl[hb], Bsc], writes=[BLTl], partial=(h > 0))
                                for g in range(2):
                                    S.op("dve", lambda: V.tensor_tensor(out=MTl[:, g * 512:(g + 1) * 512].rearrange("p (a c) -> p a c", a=4),
                                                                        in0=pcb[:, g * 128:(g + 1) * 128].unsqueeze(1).to_broadcast([128, 4, 128]),
                                                                        in1=LTl[:, g * 512:(g + 1) * 512].rearrange("p (a c) -> p a c", a=4), op=ALU.mult),
                                         reads=[Bpcb, BLTl], writes=[BMTl], partial=(g > 0))
                                py, Bpy = ps[2], Bps[2]; po, Bpo = ps[3], Bps[3]; pst_, Bpst = ps[4], Bps[4]
                                for h in range(8):
                                    g = h // 4
                                    hs = slice(h * 64, (h + 1) * 64)
                                    mm(po[:, hs], CT[:, g, tsl], carb[d][:, hs], [BCT, Bcarb[d]], Bpo, h == 0, True, start=True)
                                for h in range(8):
                                    g = h // 4
                                    hs = slice(h * 64, (h + 1) * 64)
                                    mm(pst_[:, hs], Btok[:, t, g, :], XE[:, hs], [BBtok, BXE], Bpst, h == 0, True, start=True)
                                for h in range(8):
                                    hs = slice(h * 64, (h + 1) * 64)
                                    mm(py[:, hs], MTl[:, h * 128:(h + 1) * 128], XDT[:, hs], [BMTl, BXDT], Bpy, h == 0, True, start=True)
                                S.op("dve", lambda: V.tensor_tensor(out=v3(car[d]), in0=v3(car[d]), in1=bc8(sc[:, 3, :]), op=ALU.mult), reads=[Bcar[d], Bsc], writes=[Bcar[d]])
                                S.op("dve", lambda: V.tensor_tensor(out=car[d], in0=pst_, in1=car[d], op=ALU.add), reads=[Bpst, Bcar[d]], writes=[Bcar[d]])
                                S.op("act", lambda: ACT.copy(out=carb[d], in_=car[d]), reads=[Bcar[d]], writes=[Bcarb[d]])
                                S.op("dve", lambda: V.tensor_tensor(out=v3(tY), in0=v3(po), in1=bc8(sc[:, 1, :]), op=ALU.mult), reads=[Bpo, Bsc], writes=[BtY])
                                S.op("pool", lambda: GP.tensor_tensor(out=Yacc[:, t, :], in0=Yacc[:, t, :], in1=tY, op=ALU.add), reads=[BtY, BY[t]], writes=[BY[t]])
                                S.op("dve", lambda: V.tensor_tensor(out=Yacc[:, t, :], in0=py, in1=Yacc[:, t, :], op=ALU.add), reads=[Bpy, BY[t]], writes=[BY[t]])
                        if l == 0 and b == 0:
                            dump("Yacc", Yacc, BY[0])
                            S.barrier()
                        zt = [A(st, "zt%d" % i, [128, 512], BF16) for i in range(2)]; Bzt = [gbuf("zt%d" % i) for i in range(2)]
                        Gt = A(st, "Gt", [128, 512], F32); BGt = Buf("Gt")
                        Gq = A(st, "Gq", [128, 512], F32); BGq = Buf("Gq")
                        ss = A(st, "ss", [128, 2], F32); Bss = Buf("ss")
                        stok = [A(st, "stok%d" % i, [128, 512], BF16) for i in range(2)]; Bstok = [Buf("stok%d" % i) for i in range(2)]
                        nw = prm[:, P_NW:P_NW + 512]
                        tlist = list(range(NT)) if not last else list(range(16))
                        for t in tlist:
                            z_, Bz_ = zt[t % 2], Bzt[t % 2]
                            S.dma("sp", z_, ZS[b][:, t, :], reads=[BZS[b]], writes=[Bz_])
                            S.op("dve", lambda: V.tensor_tensor(out=Gt, in0=Yacc[:, t, :], in1=z_, op=ALU.mult), reads=[BY[t], Bz_], writes=[BGt])
                            for g in range(2):
                                S.op("act", lambda: ACT.activation(out=Gq[:, g * 256:(g + 1) * 256], in_=Gt[:, g * 256:(g + 1) * 256], func=AF.Square, accum_out=ss[:, g:g + 1]),
                                     reads=[BGt], writes=[BGq, Bss], partial=(g > 0))
                            S.op("act", lambda: ACT.activation(out=ss, in_=ss, func=AF.Sqrt, bias=1e-5, scale=1.0 / 256), reads=[Bss], writes=[Bss])
                            S.op("dve", lambda: V.reciprocal(out=ss, in_=ss), reads=[Bss], writes=[Bss])
                            s_, Bs_ = stok[t % 2], Bstok[t % 2]
                            for g in range(2):
                                S.op("dve", lambda: V.scalar_tensor_tensor(out=s_[:, g * 256:(g + 1) * 256], in0=Gt[:, g * 256:(g + 1) * 256], scalar=ss[:, g:g + 1],
                                                                           in1=nw[:, g * 256:(g + 1) * 256], op0=ALU.mult, op1=ALU.mult),
                                     reads=[BGt, Bss, Bprm], writes=[Bs_], partial=(g > 0))
                            pa, pb_ = P()
                            pab = pa.bitcast(BF16)
                            for i in range(4):
                                S.op("pe", lambda: PE.transpose(out=pab[:, i * 128:(i + 1) * 128], in_=s_[:, i * 128:(i + 1) * 128], identity=identb),
                                     reads=[Bs_, Bidb], writes=[pb_], partial=(i > 0))
                            S.op("act", lambda: ACT.copy(out=cats[:, :, t * 128:(t + 1) * 128], in_=pab[:, 0:512].rearrange("p (a c) -> p a c", a=4)),
                                 reads=[pb_], writes=[Bcats], partial=True)
                        S.dma("pool", CAT[b][:, 4:8, :], cats, reads=[Bcats], writes=[BCAT[b]], partial=True, semb=Bcats)
                        S.barrier()

                with ExitStack() as st:
                    tab = [A(st, "tab%d" % i, [128, 25, 128], F32) for i in range(2)]; Btab = [gbuf("tab%d" % i) for i in range(2)]
                    qh = [A(st, "qh%d" % i, [64, T], BF16) for i in range(2)]; Bqh = [gbuf("qh%d" % i) for i in range(2)]
                    kh = [A(st, "kh%d" % i, [64, T], BF16) for i in range(2)]; Bkh = [gbuf("kh%d" % i) for i in range(2)]
                    vh = [A(st, "vh%d" % i, [128, NT, 65], BF16) for i in range(2)]; Bvh = [gbuf("vh%d" % i) for i in range(2)]
                    atok = A(st, "atok", [128, NT, 512], BF16); Batok = [Buf("atok%d" % t) for t in range(NT)]
                    cata = A(st, "cata", [128, 4, T], BF16); Bcata = gbuf("cata")
                    e1 = [A(st, "e1%d" % i, [128, 640], F32) for i in range(3)]; Be1 = [Buf("e1%d" % i) for i in range(3)]
                    pT = [A(st, "pT%d" % i, [128, 896], BF16) for i in range(3)]; BpT = [Buf("pT%d" % i) for i in range(3)]
                    rinv = [A(st, "rinv%d" % i, [128, 1], F32) for i in range(3)]; Brinv = [Buf("rinv%d" % i) for i in range(3)]
                    def load_head(h):
                        hi = h % 2
                        c_, pb0 = h // 2, (h % 2) * 64
                        S.dma("sp", tab[hi], btab_d[l, h].rearrange("p (a c) -> p a c", a=25), writes=[Btab[hi]])
                        S.dma("sp", qh[hi], QT[b][pb0:pb0 + 64, c_, :], reads=[BQT[b]], writes=[Bqh[hi]])
                        S.dma("sp", kh[hi], KT[b][pb0:pb0 + 64, c_, :], reads=[BKT[b]], writes=[Bkh[hi]])
                        S.dma("sp", vh[hi], VV[b][:, :, h * 65:(h + 1) * 65], reads=[BVV[b]], writes=[Bvh[hi]])

                    jl = list(range(16)) + ([] if last else [16, 17])
                    aitems = [(h, j) for h in range(8) for j in jl]

                    def QKE(idx):
                        h, j = aitems[idx]
                        hi = h % 2
                        i = idx % 3
                        qs = qh[hi][:, j * 128:(j + 1) * 128]
                        p1, Bp1 = ps[(idx % 2) * 2], Bps[(idx % 2) * 2]
                        p2, Bp2 = ps[(idx % 2) * 2 + 1], Bps[(idx % 2) * 2 + 1]
                        if j < 16:
                            ty = 0 if j == 0 else 1 if j == 1 else 3 if j == 14 else 4 if j == 15 else 2
                            base = min(max(j - 2, 0), 11)
                            for s in range(4):
                                kt = base + s
                                mm(p1[:, s * 128:(s + 1) * 128], kh[hi][:, kt * 128:(kt + 1) * 128], qs, [Bkh[hi], Bqh[hi]], Bp1, s == 0, True, start=True)
                            for s, kt in enumerate([base + 4, 16, 17]):
                                mm(p2[:, s * 128:(s + 1) * 128], kh[hi][:, kt * 128:(kt + 1) * 128], qs, [Bkh[hi], Bqh[hi]], Bp2, s == 0, True, start=True)
                            S.op("dve", lambda: V.tensor_tensor(out=e1[i][:, 0:512], in0=p1, in1=tab[hi][:, ty * 5:ty * 5 + 4, :].rearrange("p a c -> p (a c)"), op=ALU.add),
                                 reads=[Bp1, Btab[hi]], writes=[Be1[i]])
                            S.op("dve", lambda: V.tensor_tensor(out=e1[i][:, 512:640], in0=p2[:, 0:128], in1=tab[hi][:, ty * 5 + 4, :], op=ALU.add),
                                 reads=[Bp2, Btab[hi]], writes=[Be1[i]], partial=True)
                            S.op("act", lambda: ACT.activation(out=pT[i][:, 0:640], in_=e1[i], func=AF.Exp), reads=[Be1[i]], writes=[BpT[i]])
                            S.op("act", lambda: ACT.activation(out=pT[i][:, 640:896], in_=p2[:, 128:384], func=AF.Exp), reads=[Bp2], writes=[BpT[i]], partial=True)
                        else:
                            for s, kt in enumerate([16, 17]):
                                mm(p2[:, s * 128:(s + 1) * 128], kh[hi][:, kt * 128:(kt + 1) * 128], qs, [Bkh[hi], Bqh[hi]], Bp2, s == 0, True, start=True)
                            S.op("act", lambda: ACT.activation(out=pT[i][:, 0:256], in_=p2[:, 0:256], func=AF.Exp), reads=[Bp2], writes=[BpT[i]])

                    def PVN(idx):
                        h, j = aitems[idx]
                        hi = h % 2
                        i = idx % 3
                        po, Bpo = ps[4 + idx % 4], Bps[4 + idx % 4]
                        if j < 16:
                            base = min(max(j - 2, 0), 11)
                            kts = [base + s for s in range(5)] + [16, 17]
                        else:
                            kts = [16, 17]
                        mmacc(po[:, 0:65], [(pT[i][:, s * 128:(s + 1) * 128], vh[hi][:, kt, :]) for s, kt in enumerate(kts)], [BpT[i], Bvh[hi]], Bpo)
                        S.op("dve", lambda: V.reciprocal(out=rinv[i], in_=po[:, 64:65]), reads=[Bpo], writes=[Brinv[i]])
                        S.op("dve", lambda: V.tensor_scalar(out=atok[:, j, h * 64:(h + 1) * 64], in0=po[:, 0:64], scalar1=rinv[i], scalar2=None, op0=ALU.mult),
                             reads=[Bpo, Brinv[i]], writes=[Batok[j]], partial=True)

                    load_head(0)
                    load_head(1)
                    QKE(0)
                    for idx in range(len(aitems)):
                        if idx + 1 < len(aitems):
                            QKE(idx + 1)
                        PVN(idx)
                        h, j = aitems[idx]
                        if j == jl[0] and 1 <= h <= 6:
                            load_head(h + 1)
                    tl = list(range(NT)) if not last else list(range(16))
                    for t in tl:
                        pa, pb_ = P()
                        pab = pa.bitcast(BF16)
                        for i in range(4):
                            S.op("pe", lambda: PE.transpose(out=pab[:, i * 128:(i + 1) * 128], in_=atok[:, t, i * 128:(i + 1) * 128], identity=identb),
                                 reads=[Batok[t], Bidb], writes=[pb_], partial=(i > 0))
                        S.op("act", lambda: ACT.copy(out=cata[:, :, t * 128:(t + 1) * 128], in_=pab[:, 0:512].rearrange("p (a c) -> p a c", a=4)),
                             reads=[pb_], writes=[Bcata], partial=True)
                    S.dma("pool", CAT[b][:, 0:4, :], cata, reads=[Bcata], writes=[BCAT[b]], partial=True, semb=Bcata)
                    S.barrier()

                blks = BLKS if not last else BLKS[:4]
                Teff = T if not last else L

                def layernorm(st_tiles, Y, BY_, n, which, outs):
                    Ysq, BYsq, mean, Bmean, msq, Bmsq, rstd, Brstd, tmp, Btmp = st_tiles
                    p1, Bp1 = P()
                    mmacc(p1[:, 0:n], [(ones, Y[:, k, 0:n]) for k in range(8)], [Bcst, BY_], Bp1)
                    S.op("act", lambda: ACT.activation(out=Ysq[:, :, 0:n], in_=Y[:, :, 0:n], func=AF.Square), reads=[BY_], writes=[BYsq])
                    p2, Bp2 = P()
                    mmacc(p2[:, 0:n], [(ones, Ysq[:, k, 0:n]) for k in range(8)], [Bcst, BYsq], Bp2)
                    S.op("act", lambda: ACT.activation(out=mean[:, 0:n], in_=p1[:, 0:n], func=AF.Copy, scale=1.0 / 1024), reads=[Bp1], writes=[Bmean])
                    S.op("act", lambda: ACT.activation(out=msq[:, 0:n], in_=p1[:, 0:n], func=AF.Square, scale=1.0 / 1024), reads=[Bp1], writes=[Bmsq])
                    S.op("dve", lambda: V.scalar_tensor_tensor(out=rstd[:, 0:n], in0=p2[:, 0:n], scalar=1.0 / 1024, in1=msq[:, 0:n], op0=ALU.mult, op1=ALU.subtract),
                         reads=[Bp2, Bmsq], writes=[Brstd])
                    S.op("act", lambda: ACT.activation(out=rstd[:, 0:n], in_=rstd[:, 0:n], func=AF.Sqrt, bias=1e-5), reads=[Brstd], writes=[Brstd])
                    S.op("dve", lambda: V.reciprocal(out=rstd[:, 0:n], in_=rstd[:, 0:n]), reads=[Brstd], writes=[Brstd])
                    for k in range(8):
                        S.op("pool", lambda: GP.tensor_tensor(out=tmp[:, k, 0:n], in0=Y[:, k, 0:n], in1=mean[:, 0:n], op=ALU.subtract), reads=[BY_, Bmean], writes=[Btmp], partial=(k > 0))
                    for k in range(8):
                        S.op("dve", lambda: V.tensor_tensor(out=tmp[:, k, 0:n], in0=tmp[:, k, 0:n], in1=rstd[:, 0:n], op=ALU.mult), reads=[Btmp, Brstd], writes=[Btmp], partial=(k > 0))
                    gcol = P_LNG + which * 8
                    bcol = P_LNB + which * 8
                    for k in range(8):
                        S.op("act", lambda: ACT.activation(out=tmp[:, k, 0:n], in_=tmp[:, k, 0:n], func=AF.Identity, scale=prm[:, gcol + k:gcol + k + 1], bias=prm[:, bcol + k:bcol + k + 1]),
                             reads=[Btmp, Bprm], writes=[Btmp], partial=(k > 0))
                    return tmp, Btmp

                with ExitStack() as pd:
                    h2T = A(pd, "h2T", [128, 8, T], BF16); Bh2 = Buf("h2T")
                    combT = A(pd, "combT", [8, T], F32); BcombT = Buf("combT")
                    with ExitStack() as st:
                        catb = [A(st, "catb%d" % i, [128, 8, 512], BF16) for i in range(2)]; Bcatb = [gbuf("catb%d" % i) for i in range(2)]
                        xtb = [A(st, "xtb0", [128, 8, 512], F32)] * 2; Bxtb = [gbuf("xtb0")] * 2
                        woutb = A(st, "woutb", [128, 8, 1024], BF16); Bwout = Buf("wout")
                        wo_s = [A(st, "wos%d" % i, [128, 8, 128], F32) for i in range(2)]; Bwo_s = [gbuf("wos%d" % i) for i in range(2)]
                        wov = wout_d[l].rearrange("(k p) n -> p k n", p=128)
                        for pc in range(8):
                            w_, Bw_ = wo_s[pc % 2], Bwo_s[pc % 2]
                            S.dma("sp" if pc % 2 == 0 else "pool", w_, wov[:, :, pc * 128:(pc + 1) * 128], writes=[Bw_])
                            S.op("act", lambda: ACT.copy(out=woutb[:, :, pc * 128:(pc + 1) * 128], in_=w_), reads=[Bw_], writes=[Bwout], partial=(pc > 0))
                        Y = A(st, "Y", [128, 8, 512], F32); BY_ = Buf("Y")
                        lnt = (A(st, "Ysq", [128, 8, 512], F32), Buf("Ysq"), A(st, "mean", [128, 512], F32), Buf("mean"), A(st, "msq", [128, 512], F32), Buf("msq"),
                               A(st, "rstd", [128, 512], F32), Buf("rstd"), A(st, "lntmp", [128, 8, 512], F32), gbuf("lntmp"))
                        h2f, Bh2f = lnt[0], lnt[1]
                        rtw = A(st, "rtw", [128, 8, 8], F32); Brtw = gbuf("rtw")
                        lg = A(st, "lg", [128, 8], F32); Blg = Buf("lg")
                        sm = A(st, "sm", [128, 8, 8], F32); Bsm = Buf("sm")
                        if moe:
                            S.dma("sp", rtw, rt_d[l // 2], writes=[Brtw])
                        for bi, (t0, n) in enumerate(blks):
                            j = mj(t0)
                            cb_, Bcb_ = catb[bi % 2], Bcatb[bi % 2]
                            xb_, Bxb_ = xtb[bi % 2], Bxtb[bi % 2]
                            S.dma("sp", cb_[:, :, 0:n], CAT[b][:, :, t0:t0 + n], reads=[BCAT[b]], writes=[Bcb_])
                            S.dma("pool", xb_[:, :, 0:n], XT[b][:, :, t0:t0 + n], reads=[BXT[b]], writes=[Bxb_])
                            for oc in range(8):
                                pa, pb_ = P()
                                mmacc(pa[:, 0:n], [(woutb[:, k, oc * 128:(oc + 1) * 128], cb_[:, k, 0:n]) for k in range(8)], [Bwout, Bcb_], pb_)
                                S.op("act", lambda: ACT.activation(out=Y[:, oc, 0:n], in_=pa[:, 0:n], func=AF.Identity, scale=modT[:, 16 + oc, j:j + 1]),
                                     reads=[pb_, Bmod], writes=[BY_], partial=(oc > 0))
                                S.op("dve", lambda: V.scalar_tensor_tensor(out=Y[:, oc, 0:n], in0=xb_[:, oc, 0:n], scalar=ALPHA, in1=Y[:, oc, 0:n], op0=ALU.mult, op1=ALU.add),
                                     reads=[Bxb_, BY_], writes=[BY_], partial=True)
                            x1, Bx1 = layernorm(lnt, Y, BY_, n, 0, None)
                            S.dma("pool", X1T[b][:, :, t0:t0 + n], x1[:, :, 0:n], reads=[Bx1], writes=[BX1T[b]], partial=True, semb=Bx1)
                            for k in range(8):
                                S.op("dve", lambda: V.tensor_scalar(out=h2f[:, k, 0:n], in0=x1[:, k, 0:n], scalar1=modT[:, 32 + k, j:j + 1], scalar2=modT[:, 24 + k, j:j + 1],
                                                                    op0=ALU.mult, op1=ALU.add), reads=[Bx1, Bmod], writes=[Bh2f], partial=(k > 0))
                            S.op("act", lambda: ACT.copy(out=h2T[:, :, t0:t0 + n], in_=h2f[:, :, 0:n]), reads=[Bh2f], writes=[Bh2], partial=True)
                            if moe:
                                for tt in range(n // 128):
                                    tsl = slice(tt * 128, (tt + 1) * 128)
                                    pa, pb_ = P()
                                    mmacc(pa[:, 0:8], [(h2f[:, k, tsl], rtw[:, k, :]) for k in range(8)], [Bh2f, Brtw], pb_)
                                    S.op("dve", lambda: V.tensor_copy(out=lg, in_=pa[:, 0:8]), reads=[pb_], writes=[Blg])
                                    m1, eq1, l2, m2, eq2, dd, g1, cmb = (sm[:, 0, 0:1], sm[:, 1, :], sm[:, 2, :], sm[:, 0, 1:2], sm[:, 3, :], sm[:, 0, 2:3], sm[:, 0, 3:4], sm[:, 4, :])
                                    g2 = sm[:, 0, 4:5]
                                    S.op("dve", lambda: V.reduce_max(out=m1, in_=lg, axis=AX.X), reads=[Blg], writes=[Bsm])
                                    S.op("dve", lambda: V.tensor_scalar(out=eq1, in0=lg, scalar1=m1, scalar2=None, op0=ALU.is_equal), reads=[Blg, Bsm], writes=[Bsm])
                                    S.op("dve", lambda: V.scalar_tensor_tensor(out=l2, in0=eq1, scalar=-1e30, in1=lg, op0=ALU.mult, op1=ALU.add), reads=[Blg, Bsm], writes=[Bsm])
                                    S.op("dve", lambda: V.reduce_max(out=m2, in_=l2, axis=AX.X), reads=[Bsm], writes=[Bsm])
                                    S.op("dve", lambda: V.tensor_scalar(out=eq2, in0=l2, scalar1=m2, scalar2=None, op0=ALU.is_equal), reads=[Bsm], writes=[Bsm])
                                    S.op("dve", lambda: V.tensor_tensor(out=dd, in0=m2, in1=m1, op=ALU.subtract), reads=[Bsm], writes=[Bsm])
                                    S.op("act", lambda: ACT.activation(out=dd, in_=dd, func=AF.Exp), reads=[Bsm], writes=[Bsm])
                                    S.op("dve", lambda: V.tensor_scalar(out=g1, in0=dd, scalar1=1.0, scalar2=None, op0=ALU.add), reads=[Bsm], writes=[Bsm])
                                    S.op("dve", lambda: V.reciprocal(out=g1, in_=g1), reads=[Bsm], writes=[Bsm])
                                    S.op("dve", lambda: V.tensor_tensor(out=g2, in0=dd, in1=g1, op=ALU.mult), reads=[Bsm], writes=[Bsm])
                                    S.op("dve", lambda: V.tensor_scalar(out=cmb, in0=eq1, scalar1=g1, scalar2=None, op0=ALU.mult), reads=[Bsm], writes=[Bsm])
                                    S.op("dve", lambda: V.scalar_tensor_tensor(out=cmb, in0=eq2, scalar=g2, in1=cmb, op0=ALU.mult, op1=ALU.add), reads=[Bsm], writes=[Bsm])
                                    pa2, pb2 = P()
                                    S.op("pe", lambda: PE.transpose(out=pa2[0:8, 0:128], in_=cmb, identity=ident), reads=[Bsm, Bcst], writes=[pb2])
                                    S.op("act", lambda: ACT.copy(out=combT[:, t0 + tt * 128:t0 + (tt + 1) * 128], in_=pa2[0:8, 0:128]), reads=[pb2], writes=[BcombT], partial=True)
                        S.barrier()
                    with ExitStack() as pd2:
                        facc = A(pd2, "facc", [128, 8, T], F32)
                        Bfacc = [[Buf("facc%d_%d" % (oc, bi)) for bi in range(5)] for oc in range(8)]
                        with ExitStack() as st:
                            wst = [A(st, "fst%d" % i, [128, 2048], F32) for i in range(2)]; Bwst = [gbuf("fst%d" % i) for i in range(2)]
                            wbf = [A(st, "fbf%d" % i, [128, 2048], BF16) for i in range(6)]; Bwbf = [Buf("fbf%d" % i) for i in range(6)]
                            actT = [A(st, "actT%d" % i, [128, 2, 512], BF16) for i in range(2)]; BactT = [Buf("actT%d" % i) for i in range(2)]
                            sg = [A(st, "sg%d" % i, [128, 512], F32) for i in range(2)]; Bsg = [Buf("sg%d" % i) for i in range(2)]
                            a32, Ba32 = sg, Bsg
                            cbc = [A(st, "cbc0", [128, T], F32)] * 2; Bcbc = [Buf("cbc0")] * 2
                            wc = [0, 0]
                            F = 3584 if moe else 2816
                            nfg = F // 256
                            li = l // 2
                            experts = list(range(8)) if moe else [0]

                            def wsrc(e, fg):
                                if moe:
                                    g_ = mg_d[li, e].rearrange("(k p) n -> p k n", p=128)[:, :, fg * 256:(fg + 1) * 256]
                                    u_ = mu_d[li, e].rearrange("(k p) n -> p k n", p=128)[:, :, fg * 256:(fg + 1) * 256]
                                    d_ = md_d[li, e, fg * 256:(fg + 1) * 256, :].rearrange("(k p) n -> p k n", p=128)
                                else:
                                    g_ = fg_d[li].rearrange("(k p) n -> p k n", p=128)[:, :, fg * 256:(fg + 1) * 256]
                                    u_ = fu_d[li].rearrange("(k p) n -> p k n", p=128)[:, :, fg * 256:(fg + 1) * 256]
                                    d_ = fd_d[li, fg * 256:(fg + 1) * 256, :].rearrange("(k p) n -> p k n", p=128)
                                return g_, u_, d_

                            def getw3(e, fg):
                                res = []
                                for wi, src in enumerate(wsrc(e, fg)):
                                    si = wc[0] % 2; wc[0] += 1
                                    bi_ = wc[1] % 6; wc[1] += 1
                                    kk = 8 if wi < 2 else 2
                                    sv = wst[si].rearrange("p (k n) -> p k n", k=kk)
                                    bv = wbf[bi_].rearrange("p (k n) -> p k n", k=kk)
                                    S.dma("sp", sv, src, writes=[Bwst[si]])
                                    if wi == 1:
                                        S.op("act", lambda: ACT.copy(out=wbf[bi_], in_=wst[si]), reads=[Bwst[si]], writes=[Bwbf[bi_]])
                                    else:
                                        S.op("pool", lambda: GP.tensor_copy(out=wbf[bi_], in_=wst[si]), reads=[Bwst[si]], writes=[Bwbf[bi_]])
                                    res.append((bv, Bwbf[bi_]))
                                return res

                            pend = {}

                            def conv(wi, si, bi_):
                                if wi == 1:
                                    S.op("act", lambda: ACT.copy(out=wbf[bi_], in_=wst[si]), reads=[Bwst[si]], writes=[Bwbf[bi_]])
                                else:
                                    S.op("pool", lambda: GP.tensor_copy(out=wbf[bi_], in_=wst[si]), reads=[Bwst[si]], writes=[Bwbf[bi_]])

                            def stageA(w2):
                                srcs = wsrc(*work[w2])
                                plan = []
                                for wi in range(3):
                                    si = wc[0] % 2; wc[0] += 1
                                    bi_ = wc[1] % 6; wc[1] += 1
                                    plan.append((si, bi_, 8 if wi < 2 else 2, srcs[wi]))
                                pend[w2] = plan
                                for wi in range(2):
                                    si, bi_, kk, src = plan[wi]
                                    S.dma("sp", wst[si].rearrange("p (k n) -> p k n", k=kk), src, writes=[Bwst[si]])
                                wts[w2] = [(wbf[p_[1]].rearrange("p (k n) -> p k n", k=p_[2]), Bwbf[p_[1]]) for p_ in plan]

                            def stageB(w2):
                                plan = pend[w2]
                                for wi in range(2):
                                    conv(wi, plan[wi][0], plan[wi][1])
                                si, bi_, kk, src = plan[2]
                                S.dma("sp", wst[si].rearrange("p (k n) -> p k n", k=kk), src, writes=[Bwst[si]])

                            def stageC(w2):
                                si, bi_, kk, src = pend.pop(w2)[2]
                                conv(2, si, bi_)

                            work = [(e, fg) for e in experts for fg in range(nfg)]
                            items = [(wi_, bi) for wi_ in range(len(work)) for bi in range(len(blks))]
                            wts = {0: getw3(*work[0])}
                            if len(work) > 1:
                                wts[1] = getw3(*work[1])
                            dctr = [0]
                            gctr = [0]

                            def GU(idx):
                                wi_, bi = items[idx]
                                e, fg = work[wi_]
                                t0, n = blks[bi]
                                (wg, Bwg), (wu, Bwu), _ = wts[wi_]
                                cb_e, Bcb_e = cbc[0], Bcbc[0]
                                if moe and fg == 0 and bi == 0:
                                    for (t0c, nc_) in blks:
                                        pa, pb_ = ps[3 + dctr[0] % 5], Bps[3 + dctr[0] % 5]; dctr[0] += 1
                                        mm(pa[:, 0:nc_], sel[0:8, e, :], combT[:, t0c:t0c + nc_], [Bcst, BcombT], pb_, True, True)
                                        S.op("act", lambda: ACT.copy(out=cb_e[:, t0c:t0c + nc_], in_=pa[:, 0:nc_]), reads=[pb_], writes=[Bcb_e], partial=(t0c > 0))
                                a_, Ba_ = actT[idx % 2], BactT[idx % 2]
                                for fc in range(2):
                                    pg, Bpg = ps[gctr[0] % 3], Bps[gctr[0] % 3]; gctr[0] += 1
                                    pu, Bpu = ps[gctr[0] % 3], Bps[gctr[0] % 3]; gctr[0] += 1
                                    mmacc(pg[:, 0:n], [(wg[:, k, fc * 128:(fc + 1) * 128], h2T[:, k, t0:t0 + n]) for k in range(8)], [Bwg, Bh2], Bpg)
                                    mmacc(pu[:, 0:n], [(wu[:, k, fc * 128:(fc + 1) * 128], h2T[:, k, t0:t0 + n]) for k in range(8)], [Bwu, Bh2], Bpu)
                                    s_, Bs_ = sg[fc], Bsg[fc]
                                    S.op("act", lambda: ACT.activation(out=s_[:, 0:n], in_=pg[:, 0:n], func=AF.Silu), reads=[Bpg], writes=[Bs_])
                                    if moe:
                                        S.op("dve", lambda: V.tensor_tensor(out=s_[:, 0:n], in0=pu[:, 0:n], in1=s_[:, 0:n], op=ALU.mult), reads=[Bpu, Bs_], writes=[Bs_])
                                        S.op("pool", lambda: GP.tensor_tensor(out=a_[:, fc, 0:n], in0=s_[:, 0:n], in1=cb_e[:, t0:t0 + n], op=ALU.mult),
                                             reads=[Bs_, Bcb_e], writes=[Ba_], partial=(fc > 0))
                                    else:
                                        S.op("dve", lambda: V.tensor_tensor(out=a_[:, fc, 0:n], in0=pu[:, 0:n], in1=s_[:, 0:n], op=ALU.mult), reads=[Bpu, Bs_], writes=[Ba_], partial=(fc > 0))

                            def DN(idx):
                                wi_, bi = items[idx]
                                t0, n = blks[bi]
                                _, _, (wd, Bwd) = wts[wi_]
                                a_, Ba_ = actT[idx % 2], BactT[idx % 2]
                                for oc in range(8):
                                    pd_, Bpd_ = ps[3 + dctr[0] % 5], Bps[3 + dctr[0] % 5]; dctr[0] += 1
                                    mmacc(pd_[:, 0:n], [(wd[:, fc, oc * 128:(oc + 1) * 128], a_[:, fc, 0:n]) for fc in range(2)], [Bwd, Ba_], Bpd_)
                                    if wi_ == 0:
                                        S.op("act", lambda: ACT.copy(out=facc[:, oc, t0:t0 + n], in_=pd_[:, 0:n]), reads=[Bpd_], writes=[Bfacc[oc][bi]])
                                    else:
                                        S.op("dve", lambda: V.tensor_tensor(out=facc[:, oc, t0:t0 + n], in0=pd_[:, 0:n], in1=facc[:, oc, t0:t0 + n], op=ALU.add),
                                             reads=[Bpd_, Bfacc[oc][bi]], writes=[Bfacc[oc][bi]])

                            GU(0)
                            for idx in range(len(items)):
                                wi_, bi = items[idx]
                                if idx + 1 < len(items):
                                    GU(idx + 1)
                                DN(idx)
                                if bi == 1 and (wi_ + 1) in pend:
                                    stageB(wi_ + 1)
                                if bi == 3 and (wi_ + 1) in pend:
                                    stageC(wi_ + 1)
                                if bi == len(blks) - 1:
                                    wts.pop(wi_)
                                    if wi_ + 2 < len(work):
                                        stageA(wi_ + 2)
                            S.barrier()
                        with ExitStack() as st:
                            x1b = [A(st, "x1b%d" % i, [128, 8, 256], F32) for i in range(2)]; Bx1b = [gbuf("x1b%d" % i) for i in range(2)]
                            Y = A(st, "Y2", [128, 8, 256], F32); BY_ = Buf("Y2")
                            lnt = (A(st, "Ysq2", [128, 8, 256], F32), Buf("Ysq2"), A(st, "mean2", [128, 256], F32), Buf("mean2"), A(st, "msq2", [128, 256], F32), Buf("msq2"),
                                   A(st, "rstd2", [128, 256], F32), Buf("rstd2"), A(st, "lntmp2", [128, 8, 256], F32), gbuf("lntmp2"))
                            ot = [A(st, "ot%d" % i, [128, 1024], F32) for i in range(2)]; Bot = [gbuf("ot%d" % i) for i in range(2)]
                            oi = 0
                            for bi3 in range(Teff // 256):
                                t0, n = bi3 * 256, 256
                                bi = t0 // 512
                                j = mj(t0)
                                xb_, Bxb_ = x1b[bi3 % 2], Bx1b[bi3 % 2]
                                S.dma("pool", xb_[:, :, 0:n], X1T[b][:, :, t0:t0 + n], reads=[BX1T[b]], writes=[Bxb_])
                                for oc in range(8):
                                    S.op("act", lambda: ACT.activation(out=Y[:, oc, 0:n], in_=facc[:, oc, t0:t0 + n], func=AF.Identity, scale=modT[:, 40 + oc, j:j + 1]),
                                         reads=[Bfacc[oc][bi], Bmod], writes=[BY_], partial=(oc > 0))
                                    S.op("dve", lambda: V.scalar_tensor_tensor(out=Y[:, oc, 0:n], in0=xb_[:, oc, 0:n], scalar=ALPHA, in1=Y[:, oc, 0:n], op0=ALU.mult, op1=ALU.add),
                                         reads=[Bxb_, BY_], writes=[BY_], partial=True)
                                xn, Bxn = layernorm(lnt, Y, BY_, n, 1, None)
                                if not last:
                                    S.dma("pool", XT[b][:, :, t0:t0 + n], xn[:, :, 0:n], reads=[Bxn], writes=[BXT[b]], partial=True, semb=Bxn)
                                else:
                                    for tt in range(n // 128):
                                        o_, Bo_ = ot[oi % 2], Bot[oi % 2]; oi += 1
                                        for hh in range(2):
                                            pa, pb_ = P()
                                            for k4 in range(4):
                                                k = hh * 4 + k4
                                                S.op("pe", lambda: PE.transpose(out=pa[:, k4 * 128:(k4 + 1) * 128], in_=xn[:, k, tt * 128:(tt + 1) * 128], identity=ident),
                                                     reads=[Bxn, Bcst], writes=[pb_], partial=(k4 > 0))
                                            if hh:
                                                S.op("act", lambda: ACT.copy(out=o_[:, hh * 512:(hh + 1) * 512], in_=pa), reads=[pb_], writes=[Bo_], partial=True)
                                            else:
                                                S.op("dve", lambda: V.tensor_copy(out=o_[:, hh * 512:(hh + 1) * 512], in_=pa), reads=[pb_], writes=[Bo_])
                                        S.dma("sp", out_d[b, t0 + tt * 128:t0 + (tt + 1) * 128, :], o_, reads=[Bo_], writes=[Bout], partial=True, semb=Bo_)
                            S.barrier()
        S.barrier()
    assert S.nsem < 100, S.nsem
    return nc, S


def _consts():
    c = np.zeros((128, NCONST), np.float32)
    s = np.arange(128)[:, None]; l_ = np.arange(128)[None, :]
    c[:, C_ID:C_ID + 128] = np.eye(128)
    c[:, C_ONE:C_ONE + 128] = 1.0
    c[:, C_TRF:C_TRF + 128] = (s <= l_)
    c[:, C_TRB:C_TRB + 128] = (s >= l_)
    c[:, C_MNF:C_MNF + 128] = np.where(s <= l_, 0.0, NEG)
    c[:, C_MNB:C_MNB + 128] = np.where(s >= l_, 0.0, NEG)
    pmat = np.zeros((128, 128), np.float32)
    for base in (0, 64):
        for i in range(32):
            pmat[base + 32 + i, base + i] = -1.0
            pmat[base + i, base + 32 + i] = 1.0
    c[:, C_PM:C_PM + 128] = pmat
    selm = np.zeros((128, 8, 128), np.float32)
    for e in range(8):
        selm[e, e, :] = 1.0
    c[:, C_SEL:C_SEL + 1024] = selm.reshape(128, 1024)
    c[:, C_MN4F:C_MN4F + 512] = np.tile(c[:, C_MNF:C_MNF + 128], (1, 4))
    c[:, C_MN4B:C_MN4B + 512] = np.tile(c[:, C_MNB:C_MNB + 128], (1, 4))
    return c


def _rope():
    t = np.arange(L)
    inv = (10000.0 ** (-np.arange(32, dtype=np.float32) / 32)).astype(np.float32)
    ang_r = (t // 64).astype(np.float32)[:, None] * inv
    ang_c = (t % 64).astype(np.float32)[:, None] * inv
    cos = np.zeros((128, L), np.float32); sin = np.zeros((128, L), np.float32)
    for base, ang in ((0, ang_r), (64, ang_c)):
        cos[base:base + 32] = np.cos(ang).T; cos[base + 32:base + 64] = np.cos(ang).T
        sin[base:base + 32] = np.sin(ang).T; sin[base + 32:base + 64] = np.sin(ang).T
    return np.ascontiguousarray(np.stack([cos, sin], axis=1))


def _bias_gather_index():
    types_j = [0, 1, 2, 14, 15]
    valid = np.zeros((5, 5, 128, 128), bool)
    ridx = np.zeros((5, 5, 128, 128), np.int64)
    cidx = np.zeros((5, 5, 128, 128), np.int64)
    key = np.arange(128); q = np.arange(128)
    kr_l, kc = key // 64, key % 64
    qr_l, qc = q // 64, q % 64
    for ti, j in enumerate(types_j):
        base = min(max(j - 2, 0), 11)
        for s in range(5):
            kr = 2 * (base + s) + kr_l
            qi = 2 * j + qr_l
            r0 = np.clip(qi - 4, 0, 24)
            cs = np.clip(qc - 8, 0, 48)
            vr = (kr[:, None] >= r0[None, :]) & (kr[:, None] < r0[None, :] + 8)
            vc = (kc[:, None] >= cs[None, :]) & (kc[:, None] < cs[None, :] + 16)
            valid[ti, s] = vr & vc
            ridx[ti, s] = np.clip(kr[:, None] - qi[None, :] + 7, 0, 14)
            cidx[ti, s] = np.clip(kc[:, None] - qc[None, :] + 15, 0, 30)
    return valid, ridx, cidx


def _prep_shared(inp):
    f = lambda a: np.ascontiguousarray(np.asarray(a, dtype=np.float32))
    valid, ridx, cidx = _bias_gather_index()
    rpb = f(inp["na_rpb"])
    g = rpb[:, :, ridx, cidx]
    g = np.where(valid[None, None], g, np.float32(NEG)).astype(np.float32)
    btab = np.ascontiguousarray(g.transpose(0, 1, 4, 2, 3, 5)).reshape(4, 8, 128, 25 * 128)
    prm = np.zeros((4, 128, NP_), np.float32)
    bm = f(inp["b_mod"]).reshape(4, 48, 128).transpose(0, 2, 1)
    prm[:, :, P_BMOD:P_BMOD + 48] = bm
    cw = f(inp["conv_w"]).reshape(4, 5, 8, 128).transpose(0, 3, 2, 1)
    prm[:, :, P_CW:P_CW + 40] = cw.reshape(4, 128, 40)
    prm[:, :, P_CB:P_CB + 8] = f(inp["conv_b"]).reshape(4, 8, 128).transpose(0, 2, 1)
    prm[:, :, P_DTB:P_DTB + 16] = f(inp["dt_bias"]).reshape(4, 1, 16)
    prm[:, :, P_ALOG:P_ALOG + 16] = f(inp["a_log"]).reshape(4, 1, 16)
    prm[:, :, P_DSK:P_DSK + 8] = f(inp["d_skip"]).reshape(4, 1, 8)
    prm[:, :, P_NW:P_NW + 512] = f(inp["ssd_norm_w"]).reshape(4, 1, 512)
    prm[:, :, P_LNG:P_LNG + 16] = f(inp["ln_g"]).reshape(4, 2, 8, 128).transpose(0, 3, 1, 2).reshape(4, 128, 16)
    prm[:, :, P_LNB:P_LNB + 16] = f(inp["ln_b"]).reshape(4, 2, 8, 128).transpose(0, 3, 1, 2).reshape(4, 128, 16)
    router = np.ascontiguousarray(f(inp["router_w"]).reshape(2, 8, 128, 8).transpose(0, 2, 1, 3))
    shared = {"consts": _consts(), "rope": _rope(), "prm": prm, "btab": btab, "router": router}
    for k in ("w_mod", "w_in", "w_out", "ffn_w_gate", "ffn_w_up", "ffn_w_down", "moe_w_gate", "moe_w_up", "moe_w_down"):
        shared[k] = f(inp[k])
    return shared


def _core_inputs(inp, shared, core):
    f = lambda a: np.ascontiguousarray(np.asarray(a, dtype=np.float32))
    b0 = core * NB
    c2 = f(inp["c"])[b0:b0 + NB]
    cc = np.concatenate([c2, f(inp["c_ctx"])[None]], axis=0)
    cT = np.ascontiguousarray(cc.reshape(3, 8, 128).transpose(2, 1, 0))
    m = dict(shared)
    m["x"] = f(inp["x"])[b0:b0 + NB]
    m["ctx"] = f(inp["ctx"])[b0:b0 + NB]
    m["cT"] = cT
    return m


_CACHE = {}


def kernel(**inputs):
    if "nc" not in _CACHE:
        _CACHE["nc"] = build(4)[0]
    nc = _CACHE["nc"]
    shared = _prep_shared(inputs)
    n_cores = 8
    in_maps = [_core_inputs(inputs, shared, c) for c in range(n_cores)]
    res = run_bass_kernel_spmd(nc, in_maps, core_ids=list(range(n_cores)))
    out = np.concatenate([np.asarray(r["out"]) for r in res.results], axis=0)
    return out.astype(np.float32)
```

```python
from contextlib import ExitStack
import numpy as np
import concourse.bass as bass
import concourse.mybir as mybir
from concourse.bass_utils import run_bass_kernel_spmd

F32 = mybir.dt.float32
BF16 = mybir.dt.bfloat16
ALU = mybir.AluOpType
AF = mybir.ActivationFunctionType
AX = mybir.AxisListType

EPOCH = 30000
NB, L, C, T, NT = 2, 2048, 256, 2304, 18
BLKS = [(0, 512), (512, 512), (1024, 512), (1536, 512), (2048, 256)]
ALPHA = (2.0 * 4) ** 0.25
NEG = -30000.0
C_ID, C_ONE, C_TRF, C_TRB, C_MNF, C_MNB, C_PM, C_SEL = 0, 128, 256, 384, 512, 640, 768, 896
C_MN4F, C_MN4B = 1920, 2432
NCONST = 896 + 1024 + 1024
P_BMOD, P_CW, P_CB, P_DTB, P_ALOG, P_DSK, P_NW, P_LNG, P_LNB = 0, 48, 88, 96, 112, 128, 136, 648, 664
NP_ = 680


class Buf:
    __slots__ = ("name", "w", "r", "dsem", "dcount")

    def __init__(self, name="b"):
        self.name = name
        self.w = {}
        self.r = {}
        self.dsem = None
        self.dcount = 0


class Sched:
    def __init__(self, nc):
        self.nc = nc
        self.engs = {"pe": nc.tensor, "act": nc.scalar, "dve": nc.vector, "pool": nc.gpsimd, "sp": nc.sync}
        self.sem, self.cnt = {}, {}
        self.seq = {e: 0 for e in self.engs}
        self.seen = {e: {} for e in self.engs}
        self.last = {}
        self.dtoks = {}
        self.nsem = self.nwait = self.ninst = 0
        for e in self.engs:
            self._new_epoch(e)

    def _alloc_sem(self, name):
        self.nsem += 1
        return self.nc.alloc_semaphore("%s_%d" % (name, self.nsem))

    def _new_epoch(self, e):
        self.sem[e] = self._alloc_sem("s_" + e)
        self.cnt[e] = 0

    def _wait(self, eng, tok):
        key, seq, sem, val = tok
        if eng == "pe" and key == "pe":
            return
        if self.seen[eng].get(key, -1) >= seq:
            return
        self.engs[eng].wait_ge(sem, val)
        self.nwait += 1
        self.seen[eng][key] = seq

    def _deps(self, eng, reads, writes, partial):
        for b in reads:
            for t in b.w.values():
                self._wait(eng, t)
        for b in writes:
            for t in b.r.values():
                self._wait(eng, t)
            if not partial:
                for t in b.w.values():
                    self._wait(eng, t)

    def _commit(self, tok, reads, writes, partial):
        key = tok[0]
        for b in reads:
            b.r[key] = tok
        for b in writes:
            if partial and not b.r:
                b.w[key] = tok
            else:
                b.w = {key: tok}
            b.r = {}

    def op(self, eng, fn, reads=(), writes=(), partial=False):
        self._deps(eng, reads, writes, partial)
        ins = fn()
        if self.cnt[eng] >= EPOCH:
            self._new_epoch(eng)
        self.cnt[eng] += 1
        self.seq[eng] += 1
        ins.then_inc(self.sem[eng], 1)
        tok = (eng, self.seq[eng], self.sem[eng], self.cnt[eng])
        self.last[eng] = tok
        self._commit(tok, reads, writes, partial)
        self.ninst += 1
        return tok

    def dma(self, q, out, in_, reads=(), writes=(), partial=False, semb=None, **kw):
        self._deps(q, reads, writes, partial)
        ins = self.engs[q].dma_start(out=out, in_=in_, **kw)
        b = semb if semb is not None else writes[0]
        if b.dsem is None:
            b.dsem = self._alloc_sem("d_" + b.name)
        b.dcount += 16
        ins.then_inc(b.dsem, 16)
        tok = (("d", b.dsem.num), b.dcount, b.dsem, b.dcount)
        self.dtoks[tok[0]] = tok
        self._commit(tok, reads, writes, partial)
        self.ninst += 1
        return tok

    def barrier(self):
        toks = list(self.last.values()) + list(self.dtoks.values())
        for e in self.engs:
            for t in toks:
                self._wait(e, t)
        self.dtoks = {}


def build(n_layers=4, debug=False):
    nc = bass.Bass("TRN2", target_bir_lowering=False)
    S = Sched(nc)
    uid = [0]

    def D(name, shape, dt=F32, kind="ExternalInput"):
        if kind == "Internal" and debug:
            kind = "ExternalOutput"
        return nc.dram_tensor(name, shape, dt, kind=kind).ap()

    def A(st, name, shape, dt):
        uid[0] += 1
        return st.enter_context(nc.sbuf_tensor("%s_%d" % (name, uid[0]), shape, dt))[:]

    gb = {}

    def gbuf(name):
        if name not in gb:
            gb[name] = Buf(name)
        return gb[name]

    def dump(name, ap, buf, dt_=F32):
        if not debug:
            return
        o = nc.dram_tensor("dbg_" + name, list(ap.shape), dt_, kind="ExternalOutput").ap()
        S.dma("sp", o, ap, reads=[buf], writes=[gbuf("dbg_" + name)])

    x_d = D("x", [NB, L, 1024]); ctx_d = D("ctx", [NB, C, 1024]); cT_d = D("cT", [128, 8, 3])
    const_d = D("consts", [128, NCONST]); rope_d = D("rope", [128, 2, L]); prm_d = D("prm", [4, 128, NP_])
    wmod_d = D("w_mod", [4, 1024, 6144]); win_d = D("w_in", [4, 1024, 3088]); wout_d = D("w_out", [4, 1024, 1024])
    btab_d = D("btab", [4, 8, 128, 25 * 128])
    fg_d = D("ffn_w_gate", [2, 1024, 2816]); fu_d = D("ffn_w_up", [2, 1024, 2816]); fd_d = D("ffn_w_down", [2, 2816, 1024])
    rt_d = D("router", [2, 128, 8, 8])
    mg_d = D("moe_w_gate", [2, 8, 1024, 3584]); mu_d = D("moe_w_up", [2, 8, 1024, 3584]); md_d = D("moe_w_down", [2, 8, 3584, 1024])
    out_d = D("out", [NB, L, 1024], kind="ExternalOutput")
    XT = [D("XT%d" % b, [128, 8, T], kind="Internal") for b in range(NB)]
    X1T = [D("X1T%d" % b, [128, 8, T], kind="Internal") for b in range(NB)]
    QT = [D("QT%d" % b, [128, 4, T], BF16, kind="Internal") for b in range(NB)]
    KT = [D("KT%d" % b, [128, 4, T], BF16, kind="Internal") for b in range(NB)]
    VV = [D("VV%d" % b, [128, NT, 520], BF16, kind="Internal") for b in range(NB)]
    ZS = [D("ZS%d" % b, [128, NT, 512], BF16, kind="Internal") for b in range(NB)]
    CAT = [D("CAT%d" % b, [128, 8, T], BF16, kind="Internal") for b in range(NB)]
    BXT = [gbuf("XT%d" % b) for b in range(NB)]; BX1T = [gbuf("X1T%d" % b) for b in range(NB)]
    BQT = [gbuf("QT%d" % b) for b in range(NB)]; BKT = [gbuf("KT%d" % b) for b in range(NB)]
    BVV = [gbuf("VV%d" % b) for b in range(NB)]; BZS = [gbuf("ZS%d" % b) for b in range(NB)]
    BCAT = [gbuf("CAT%d" % b) for b in range(NB)]
    Bout = gbuf("out")

    ps = [nc.alloc_psum_tensor("ps%d" % i, [128, 512], F32).ap() for i in range(8)]
    Bps = [Buf("ps%d" % i) for i in range(8)]
    pctr = [0]

    def P():
        i = pctr[0] % 8
        pctr[0] += 1
        return ps[i], Bps[i]

    V, ACT, PE, GP = nc.vector, nc.scalar, nc.tensor, nc.gpsimd

    def mm(out, lhsT, rhs, rd, wb, first, last, start=None):
        S.op("pe", lambda: PE.matmul(out, lhsT=lhsT, rhs=rhs, start=(first if start is None else start), stop=last), reads=rd, writes=[wb], partial=not first)

    def mmacc(out, pairs, rd, wb):
        n = len(pairs)
        for i, (l_, r_) in enumerate(pairs):
            mm(out, l_, r_, rd, wb, i == 0, i == n - 1)

    with ExitStack() as G:
        cst = A(G, "cst", [128, NCONST], F32); Bcst = gbuf("cst")
        S.dma("sp", cst, const_d, writes=[Bcst])
        ident = cst[:, C_ID:C_ID + 128]; ones = cst[:, C_ONE:C_ONE + 128]
        tri = [cst[:, C_TRF:C_TRF + 128], cst[:, C_TRB:C_TRB + 128]]
        mneg = [cst[:, C_MNF:C_MNF + 128], cst[:, C_MNB:C_MNB + 128]]
        pm = cst[:, C_PM:C_PM + 128]
        mneg4 = [cst[:, C_MN4F:C_MN4F + 512], cst[:, C_MN4B:C_MN4B + 512]]
        sel = cst[:, C_SEL:C_SEL + 1024].rearrange("p (e m) -> p e m", e=8)
        identb = A(G, "identb", [128, 128], BF16); Bidb = Buf("idb")
        S.op("dve", lambda: V.tensor_copy(out=identb, in_=ident), reads=[Bcst], writes=[Bidb])
        modT = A(G, "modT", [128, 48, 3], F32); Bmod = Buf("mod")
        prm = A(G, "prm", [128, NP_], F32); Bprm = gbuf("prm")
        negA = A(G, "negA", [128, 16], F32); BnegA = Buf("negA")
        sT = A(G, "sT", [128, 8, 3], F32); BsT = gbuf("sT")
        S.dma("sp", sT, cT_d, writes=[BsT])
        S.op("act", lambda: ACT.activation(out=sT, in_=sT, func=AF.Silu), reads=[BsT], writes=[BsT])

        with ExitStack() as st:
            xin = [A(st, "xin%d" % i, [128, 1024], F32) for i in range(2)]; Bxin = [gbuf("xin%d" % i) for i in range(2)]
            xo = [A(st, "xo%d" % i, [128, 8, 512], F32) for i in range(2)]; Bxo = [Buf("xo%d" % i) for i in range(2)]
            ti = 0
            for b in range(NB):
                for bi, (t0, n) in enumerate(BLKS):
                    o, Bo = xo[bi % 2], Bxo[bi % 2]
                    for tt in range(n // 128):
                        t = t0 + tt * 128
                        src = x_d[b, t:t + 128, :] if t < L else ctx_d[b, t - L:t - L + 128, :]
                        xi, Bxi = xin[ti % 2], Bxin[ti % 2]; ti += 1
                        S.dma("sp", xi, src, writes=[Bxi])
                        for hh in range(2):
                            pa, pb_ = P()
                            for k4 in range(4):
                                k = hh * 4 + k4
                                S.op("pe", lambda: PE.transpose(out=pa[:, k4 * 128:(k4 + 1) * 128], in_=xi[:, k * 128:(k + 1) * 128], identity=ident),
                                     reads=[Bxi, Bcst], writes=[pb_], partial=(k4 > 0))
                            S.op("act" if hh else "dve",
                                 (lambda: ACT.copy(out=o[:, hh * 4:hh * 4 + 4, tt * 128:(tt + 1) * 128], in_=pa.rearrange("p (k c) -> p k c", k=4))) if hh else
                                 (lambda: V.tensor_copy(out=o[:, hh * 4:hh * 4 + 4, tt * 128:(tt + 1) * 128], in_=pa.rearrange("p (k c) -> p k c", k=4))),
                                 reads=[pb_], writes=[Bo], partial=not (tt == 0 and hh == 0))
                    S.dma("pool", XT[b][:, :, t0:t0 + n], o[:, :, 0:n], reads=[Bo], writes=[BXT[b]], partial=True, semb=Bo)
            S.barrier()

        for l in range(n_layers):
            last = (l == n_layers - 1)
            moe = (l % 2 == 1)
            win_l = win_d[l].rearrange("(k p) n -> p k n", p=128)
            S.dma("sp", prm, prm_d[l], writes=[Bprm])
            S.op("act", lambda: ACT.activation(out=negA, in_=prm[:, P_ALOG:P_ALOG + 16], func=AF.Exp), reads=[Bprm], writes=[BnegA])
            S.op("dve", lambda: V.tensor_scalar(out=negA, in0=negA, scalar1=-1.0, scalar2=None, op0=ALU.mult), reads=[BnegA], writes=[BnegA])
            with ExitStack() as st:
                wm = [A(st, "wm%d" % i, [128, 8, 256], F32) for i in range(4)]; Bwm = [gbuf("wm%d" % i) for i in range(4)]
                wmv = wmod_d[l].rearrange("(k p) n -> p k n", p=128)
                for pc in range(24):
                    w_, Bw_ = wm[pc % 4], Bwm[pc % 4]
                    S.dma(("sp", "pool", "act", "pool")[pc % 4], w_, wmv[:, :, pc * 256:(pc + 1) * 256], writes=[Bw_])
                    for oc in range(2):
                        m = pc * 2 + oc
                        pa, pb_ = P()
                        mmacc(pa[:, 0:3], [(w_[:, k, oc * 128:(oc + 1) * 128], sT[:, k, :]) for k in range(8)], [Bw_, BsT], pb_)
                        S.op("dve", lambda: V.tensor_scalar(out=modT[:, m, :], in0=pa[:, 0:3], scalar1=prm[:, P_BMOD + m:P_BMOD + m + 1],
                                                            scalar2=(1.0 if (m // 8) in (1, 2, 4, 5) else 0.0), op0=ALU.add, op1=ALU.add),
                             reads=[pb_, Bprm], writes=[Bmod], partial=(m > 0))
                S.barrier()

            for b in range(NB):
                def mj(t0):
                    return b if t0 < L else 2
                with ExitStack() as pst:
                    XS = A(pst, "XS", [128, NT, 512], BF16); BXS = Buf("XS")
                    BT = A(pst, "BT", [128, 2, T], BF16); BBT = Buf("BT")
                    CT = A(pst, "CT", [128, 2, T], BF16); BCT = Buf("CT")
                    Btok = A(pst, "Btok", [128, NT, 2, 128], BF16); BBtok = Buf("Btok")
                    dt = A(pst, "dt", [128, NT, 16], F32); Bdt = Buf("dt")
                    da = A(pst, "da", [128, NT, 16], F32); Bda = Buf("da")
                    with ExitStack() as st:
                        h1T = A(st, "h1T", [128, 8, T], BF16); Bh1 = Buf("h1T")
                        wdt = A(st, "wdt", [128, 8, 16], F32); Bwdt = gbuf("wdt")
                        S.dma("sp", wdt, win_l[:, :, 3072:3088], writes=[Bwdt])
                        with ExitStack() as st1:
                            xt = [A(st1, "xt%d" % i, [128, 8, 256], F32) for i in range(2)]; Bxt = [gbuf("xt%d" % i) for i in range(2)]
                            h1f = [A(st1, "h1f%d" % i, [128, 8, 256], F32) for i in range(2)]; Bh1f = [Buf("h1f%d" % i) for i in range(2)]
                            dtr = A(st1, "dtr", [128, 16], F32); Bdtr = Buf("dtr")
                            for bi in range(T // 256):
                                t0 = bi * 256
                                j = mj(t0)
                                x_, Bx_ = xt[bi % 2], Bxt[bi % 2]
                                hf, Bhf = h1f[bi % 2], Bh1f[bi % 2]
                                S.dma("pool", x_, XT[b][:, :, t0:t0 + 256], reads=[BXT[b]], writes=[Bx_])
                                for k in range(8):
                                    S.op("dve", lambda: V.tensor_scalar(out=hf[:, k, :], in0=x_[:, k, :], scalar1=modT[:, 8 + k, j:j + 1], scalar2=modT[:, k, j:j + 1],
                                                                        op0=ALU.mult, op1=ALU.add), reads=[Bx_, Bmod], writes=[Bhf], partial=(k > 0))
                                S.op("act", lambda: ACT.copy(out=h1T[:, :, t0:t0 + 256], in_=hf), reads=[Bhf], writes=[Bh1], partial=True)
                                for tt in range(2):
                                    t = bi * 2 + tt
                                    pa, pb_ = P()
                                    mmacc(pa[:, 0:16], [(hf[:, k, tt * 128:(tt + 1) * 128], wdt[:, k, :]) for k in range(8)], [Bhf, Bwdt], pb_)
                                    S.op("dve", lambda: V.tensor_tensor(out=dtr, in0=pa[:, 0:16], in1=prm[:, P_DTB:P_DTB + 16], op=ALU.add), reads=[pb_, Bprm], writes=[Bdtr])
                                    S.op("act", lambda: ACT.activation(out=dtr, in_=dtr, func=AF.Exp), reads=[Bdtr], writes=[Bdtr])
                                    S.op("act", lambda: ACT.activation(out=dt[:, t, :], in_=dtr, func=AF.Ln, bias=1.0), reads=[Bdtr], writes=[Bdt], partial=True)
                                    S.op("dve", lambda: V.tensor_tensor(out=da[:, t, :], in0=dt[:, t, :], in1=negA, op=ALU.mult), reads=[Bdt, BnegA], writes=[Bda], partial=True)
                            S.barrier()
                        with ExitStack() as st2:
                            wst = [A(st2, "wst%d" % i, [128, 8, 256], F32) for i in range(2)]; Bwst = [gbuf("wst%d" % i) for i in range(2)]
                            wbf = [A(st2, "wbf%d" % i, [128, 8, 256], BF16) for i in range(2)]; Bwbf = [Buf("wbf%d" % i) for i in range(2)]
                            qks = [A(st2, "qks%d" % i, [128, T], BF16) for i in range(2)]; Bqks = [gbuf("qks%d" % i) for i in range(2)]
                            vs = [A(st2, "vs%d" % i, [128, 8, 65], BF16) for i in range(2)]; Bvs = [gbuf("vs%d" % i) for i in range(2)]
                            zs = [A(st2, "zs%d" % i, [128, 256], BF16) for i in range(2)]; Bzs = [gbuf("zs%d" % i) for i in range(2)]
                            U = A(st2, "U", [128, 2312], F32); BU = Buf("U")
                            acc = A(st2, "cacc", [128, T], F32); Bacc = Buf("cacc")
                            xsT = A(st2, "xsT", [128, T], BF16); BxsT = Buf("xsT")
                            rp = [A(st2, "rp%d" % i, [128, 2, 512], F32) for i in range(2)]; Brp = [gbuf("rp%d" % i) for i in range(2)]
                            dg = [A(st2, "dg%d" % i, [128, 5, 128], F32) for i in range(2)]; Bdg = [Buf("dg%d" % i) for i in range(2)]
                            tm1 = A(st2, "tm1", [128, 512], F32); Btm1 = Buf("tm1")
                            tm2 = A(st2, "tm2", [128, 512], F32); Btm2 = Buf("tm2")
                            S.op("dve", lambda: V.memset(U, 0.0), writes=[BU])
                            for i in range(2):
                                S.op("dve", lambda: V.memset(vs[i], 1.0), writes=[Bvs[i]])
                            cnt = [0, 0, 0, 0]

                            def getw(pi, slot):
                                i = slot % 2
                                S.dma("sp", wst[i], win_l[:, :, pi * 256:(pi + 1) * 256], writes=[Bwst[i]])
                                if pi % 2:
                                    S.op("act", lambda: ACT.copy(out=wbf[i], in_=wst[i]), reads=[Bwst[i]], writes=[Bwbf[i]])
                                else:
                                    S.op("pool", lambda: GP.tensor_copy(out=wbf[i], in_=wst[i]), reads=[Bwst[i]], writes=[Bwbf[i]])
                                return wbf[i], Bwbf[i]

                            porder = [8, 0, 9, 1, 10, 2, 11, 3, 4, 5, 6, 7]
                            nxt = getw(porder[0], 0)
                            for pidx, pi in enumerate(porder):
                                wb, Bwb = nxt
                                if pidx + 1 < 12:
                                    nxt = getw(porder[pidx + 1], pidx + 1)
                                grp, half = pi // 2, pi % 2
                                if grp in (0, 1):
                                    for oc2 in range(2):
                                        oc = half * 2 + oc2
                                        q_, Bq_ = qks[cnt[0] % 2], Bqks[cnt[0] % 2]; cnt[0] += 1
                                        for (t0, n) in BLKS:
                                            pa, pb_ = P()
                                            mmacc(pa[:, 0:n], [(wb[:, k, oc2 * 128:(oc2 + 1) * 128], h1T[:, k, t0:t0 + n]) for k in range(8)], [Bwb, Bh1], pb_)
                                            S.op("act", lambda: ACT.activation(out=q_[:, t0:t0 + n], in_=pa[:, 0:n], func=AF.Copy, scale=(0.125 if grp == 0 else 1.0)),
                                                 reads=[pb_], writes=[Bq_], partial=True)
                                        dst, Bdst = (QT[b], BQT[b]) if grp == 0 else (KT[b], BKT[b])
                                        S.dma("pool", dst[:, oc, :], q_, reads=[Bq_], writes=[Bdst], partial=True, semb=Bq_)
                                elif grp == 2:
                                    for t in range(NT):
                                        pa, pb_ = P()
                                        mmacc(pa[:, 0:256], [(h1T[:, k, t * 128:(t + 1) * 128], wb[:, k, :]) for k in range(8)], [Bwb, Bh1], pb_)
                                        v_, Bv_ = vs[cnt[1] % 2], Bvs[cnt[1] % 2]; cnt[1] += 1
                                        S.op("dve", lambda: V.tensor_copy(out=v_[:, 0:4, 0:64], in_=pa[:, 0:256].rearrange("p (h d) -> p h d", h=4)),
                                             reads=[pb_], writes=[Bv_])
                                        S.dma("pool", VV[b][:, t, half * 260:half * 260 + 260].rearrange("p (h d) -> p h d", h=4), v_[:, 0:4, :],
                                              reads=[Bv_], writes=[BVV[b]], partial=True, semb=Bv_)
                                elif grp == 3:
                                    for t in range(NT):
                                        pa, pb_ = P()
                                        mmacc(pa[:, 0:256], [(h1T[:, k, t * 128:(t + 1) * 128], wb[:, k, :]) for k in range(8)], [Bwb, Bh1], pb_)
                                        z_, Bz_ = zs[cnt[2] % 2], Bzs[cnt[2] % 2]; cnt[2] += 1
                                        S.op("act", lambda: ACT.activation(out=z_, in_=pa[:, 0:256], func=AF.Silu), reads=[pb_], writes=[Bz_])
                                        S.dma("pool", ZS[b][:, t, half * 256:(half + 1) * 256], z_, reads=[Bz_], writes=[BZS[b]], partial=True, semb=Bz_)
                                else:
                                    for oc2 in range(2):
                                        ch = (pi - 8) * 2 + oc2
                                        for (t0, n) in BLKS:
                                            pa, pb_ = P()
                                            mmacc(pa[:, 0:n], [(wb[:, k, oc2 * 128:(oc2 + 1) * 128], h1T[:, k, t0:t0 + n]) for k in range(8)], [Bwb, Bh1], pb_)
                                            off = 2 + t0 if t0 < L else 2054 + (t0 - L)
                                            S.op("act", lambda: ACT.copy(out=U[:, off:off + n], in_=pa[:, 0:n]), reads=[pb_], writes=[BU], partial=(t0 > 0))
                                        first = True
                                        for (o, t0, n) in [(2, 0, L), (2054, L, C)]:
                                            S.op("dve", lambda: V.tensor_scalar(out=acc[:, t0:t0 + n], in0=U[:, o - 2:o - 2 + n], scalar1=prm[:, P_CW + ch * 5:P_CW + ch * 5 + 1],
                                                                                scalar2=None, op0=ALU.mult), reads=[BU, Bprm], writes=[Bacc], partial=not first)
                                            first = False
                                            for jj in range(1, 5):
                                                S.op("dve", lambda: V.scalar_tensor_tensor(out=acc[:, t0:t0 + n], in0=U[:, o - 2 + jj:o - 2 + jj + n],
                                                                                           scalar=prm[:, P_CW + ch * 5 + jj:P_CW + ch * 5 + jj + 1],
                                                                                           in1=acc[:, t0:t0 + n], op0=ALU.mult, op1=ALU.add),
                                                     reads=[BU, Bprm, Bacc], writes=[Bacc])
                                        cbias = prm[:, P_CB + ch:P_CB + ch + 1]
                                        if ch < 4:
                                            S.op("act", lambda: ACT.activation(out=xsT, in_=acc, func=AF.Silu, bias=cbias), reads=[Bacc, Bprm], writes=[BxsT])
                                            for t4 in range(0, NT, 4):
                                                nt = min(4, NT - t4)
                                                pa, pb_ = P()
                                                pab = pa.bitcast(BF16)
                                                for i in range(nt):
                                                    S.op("pe", lambda: PE.transpose(out=pab[:, i * 128:(i + 1) * 128], in_=xsT[:, (t4 + i) * 128:(t4 + i + 1) * 128], identity=identb),
                                                         reads=[BxsT, Bidb], writes=[pb_], partial=(i > 0))
                                                S.op("dve", lambda: V.tensor_copy(out=XS[:, t4:t4 + nt, ch * 128:(ch + 1) * 128],
                                                                                  in_=pab[:, 0:nt * 128].rearrange("p (a c) -> p a c", a=nt)),
                                                     reads=[pb_], writes=[BXS], partial=True)
                                        else:
                                            g = ch % 2
                                            dstT, BdstT = (BT, BBT) if ch < 6 else (CT, BCT)
                                            S.op("act", lambda: ACT.activation(out=acc, in_=acc, func=AF.Silu, bias=cbias), reads=[Bacc, Bprm], writes=[Bacc])
                                            for bi in range(4):
                                                t0 = bi * 512
                                                r_, Br_ = rp[cnt[3] % 2], Brp[cnt[3] % 2]; cnt[3] += 1
                                                S.dma("pool", r_, rope_d[:, :, t0:t0 + 512], writes=[Br_])
                                                pa, pb_ = P()
                                                mm(pa, pm, acc[:, t0:t0 + 512], [Bcst, Bacc], pb_, True, True)
                                                S.op("pool", lambda: GP.tensor_tensor(out=tm1, in0=acc[:, t0:t0 + 512], in1=r_[:, 0, :], op=ALU.mult), reads=[Bacc, Br_], writes=[Btm1])
                                                S.op("dve", lambda: V.tensor_tensor(out=tm2, in0=pa, in1=r_[:, 1, :], op=ALU.mult), reads=[pb_, Br_], writes=[Btm2])
                                                S.op("dve", lambda: V.tensor_tensor(out=dstT[:, g, t0:t0 + 512], in0=tm1, in1=tm2, op=ALU.add), reads=[Btm1, Btm2], writes=[BdstT], partial=True)
                                            S.op("act", lambda: ACT.copy(out=dstT[:, g, L:T], in_=acc[:, L:T]), reads=[Bacc], writes=[BdstT], partial=True)
                                            if ch < 6:
                                                for t4 in range(0, NT, 4):
                                                    nt = min(4, NT - t4)
                                                    pa, pb_ = P()
                                                    pab = pa.bitcast(BF16)
                                                    for i in range(nt):
                                                        S.op("pe", lambda: PE.transpose(out=pab[:, i * 128:(i + 1) * 128], in_=BT[:, g, (t4 + i) * 128:(t4 + i + 1) * 128], identity=identb),
                                                             reads=[BBT, Bidb], writes=[pb_], partial=(i > 0))
                                                    S.op("dve", lambda: V.tensor_copy(out=Btok[:, t4:t4 + nt, g, :], in_=pab[:, 0:nt * 128].rearrange("p (a c) -> p a c", a=nt)),
                                                         reads=[pb_], writes=[BBtok], partial=True)
                            S.barrier()

                    if l == 0 and b == 0:
                        dump("XS", XS, BXS, BF16); dump("dt", dt, Bdt); dump("da", da, Bda); dump("BT", BT, BBT, BF16); dump("CT", CT, BCT, BF16); dump("Btok", Btok, BBtok, BF16)
                        S.barrier()
                    with ExitStack() as st:
                        Yacc = A(st, "Yacc", [128, NT, 512], F32); BY = [Buf("Y%d" % t) for t in range(NT)]
                        car = [A(st, "car%d" % d, [128, 512], F32) for d in range(2)]; Bcar = [Buf("car%d" % d) for d in range(2)]
                        carb = [A(st, "carb%d" % d, [128, 512], BF16) for d in range(2)]; Bcarb = [Buf("carb%d" % d) for d in range(2)]
                        cats = A(st, "cats", [128, 4, T], BF16); Bcats = gbuf("cats")
                        cs = A(st, "cs", [128, 16], F32); Bcs = Buf("cs")
                        sc = A(st, "scl", [128, 6, 8], F32); Bsc = Buf("scl")
                        XDT = A(st, "XDT", [128, 512], BF16); BXDT = Buf("XDT")
                        XE = A(st, "XE", [128, 512], BF16); BXE = Buf("XE")
                        R = [A(st, "R%d" % i, [128, 128], F32) for i in range(2)]; BR = [Buf("R%d" % i) for i in range(2)]
                        LT = [A(st, "LT%d" % i, [128, 128], F32) for i in range(2)]; BLT = [Buf("LT%d" % i) for i in range(2)]
                        MT = [A(st, "MT%d" % i, [128, 128], BF16) for i in range(2)]; BMT = [Buf("MT%d" % i) for i in range(2)]
                        tY = A(st, "tY", [128, 512], F32); BtY = Buf("tY")
                        dsk = prm[:, P_DSK:P_DSK + 8]

                        def bc8(ap8):
                            return ap8.unsqueeze(2).to_broadcast([128, 8, 64])

                        def v3(ap512):
                            return ap512.rearrange("p (h d) -> p h d", h=8)

                        for t in range(NT):
                            S.op("dve", lambda: V.tensor_tensor(out=v3(Yacc[:, t, :]), in0=v3(XS[:, t, :]), in1=bc8(dsk), op=ALU.mult), reads=[BXS, Bprm], writes=[BY[t]])
                        cs2 = [A(st, "cs2%d" % i, [128, 16], F32) for i in range(2)]; Bcs2 = [Buf("cs2%d" % i) for i in range(2)]
                        sc2 = [A(st, "sc2%d" % i, [128, 6, 8], F32) for i in range(2)]; Bsc2 = [Buf("sc2%d" % i) for i in range(2)]
                        XDT2 = [A(st, "XDT2%d" % i, [128, 512], BF16) for i in range(2)]; BXDT2 = [Buf("XDT2%d" % i) for i in range(2)]
                        XE2 = [A(st, "XE2%d" % i, [128, 512], BF16) for i in range(2)]; BXE2 = [Buf("XE2%d" % i) for i in range(2)]
                        LTa = [A(st, "LTa%d" % i, [128, 1024], F32) for i in range(2)]; BLTa = [Buf("LTa%d" % i) for i in range(2)]
                        MTa = [A(st, "MTa%d" % i, [128, 1024], BF16) for i in range(2)]; BMTa = [Buf("MTa%d" % i) for i in range(2)]
                        tY2 = [A(st, "tY2%d" % i, [128, 512], F32) for i in range(2)]; BtY2 = [Buf("tY2%d" % i) for i in range(2)]
                        cc = 0
                        for d in range(2):
                            S.op("dve", lambda: V.memset(car[d], 0.0), writes=[Bcar[d]])
                            S.op("dve", lambda: V.memset(carb[d], 0.0), writes=[Bcarb[d]])
                            order = [16, 17] + list(range(16)) if d == 0 else [17, 16] + list(range(15, -1, -1))
                            for t in order:
                                ci = cc % 2; cc += 1
                                cs, Bcs = cs2[ci], Bcs2[ci]
                                sc, Bsc = sc2[ci], Bsc2[ci]
                                XDT, BXDT = XDT2[ci], BXDT2[ci]
                                XE, BXE = XE2[ci], BXE2[ci]
                                LTl, BLTl = LTa[ci], BLTa[ci]
                                MTl, BMTl = MTa[ci], BMTa[ci]
                                tY, BtY = tY2[ci], BtY2[ci]
                                tsl = slice(t * 128, (t + 1) * 128)
                                dav = da[:, t, d * 8:(d + 1) * 8]
                                dtv = dt[:, t, d * 8:(d + 1) * 8]
                                pcb, Bpcb = ps[0], Bps[0]
                                for g in range(2):
                                    mm(pcb[:, g * 128:(g + 1) * 128], BT[:, g, tsl], CT[:, g, tsl], [BBT, BCT], Bpcb, g == 0, True, start=True)
                                pcs, Bpcs = ps[1], Bps[1]
                                mm(pcs[:, 0:8], tri[d], dav, [Bcst, Bda], Bpcs, True, True)
                                mm(pcs[:, 8:16], ones, dav, [Bcst, Bda], Bpcs, False, True, start=True)
                                pl = [ps[5], ps[6]]; Bpl = [Bps[5], Bps[6]]
                                for h in range(8):
                                    hb, hq = h // 4, h % 4
                                    mm(pl[hb][:, hq * 128:(hq + 1) * 128], dav[:, h:h + 1].to_broadcast([128, 128]), tri[d], [Bcst, Bda], Bpl[hb], hq == 0, False, start=(hq == 0))
                                for hb in range(2):
                                    mm(pl[hb], ident, mneg4[d], [Bcst], Bpl[hb], False, True, start=False)
                                S.op("act", lambda: ACT.copy(out=cs, in_=pcs[:, 0:16]), reads=[Bpcs], writes=[Bcs])
                                S.op("dve", lambda: V.tensor_scalar(out=sc[:, 0, :], in0=cs[:, 0:8], scalar1=-1.0, scalar2=None, op0=ALU.mult), reads=[Bcs], writes=[Bsc])
                                S.op("dve", lambda: V.tensor_tensor(out=sc[:, 5, :], in0=cs[:, 8:16], in1=cs[:, 0:8], op=ALU.subtract), reads=[Bcs], writes=[Bsc], partial=True)
                                S.op("act", lambda: ACT.activation(out=sc[:, 1, :], in_=cs[:, 0:8], func=AF.Exp), reads=[Bcs], writes=[Bsc], partial=True)
                                S.op("act", lambda: ACT.activation(out=sc[:, 3, :], in_=cs[:, 8:16], func=AF.Exp), reads=[Bcs], writes=[Bsc], partial=True)
                                S.op("act", lambda: ACT.activation(out=sc[:, 2, :], in_=sc[:, 5, :], func=AF.Exp), reads=[Bsc], writes=[Bsc])
                                S.op("dve", lambda: V.tensor_tensor(out=sc[:, 4, :], in0=sc[:, 2, :], in1=dtv, op=ALU.mult), reads=[Bsc, Bdt], writes=[Bsc])
                                S.op("dve", lambda: V.tensor_tensor(out=v3(XDT), in0=v3(XS[:, t, :]), in1=bc8(dtv), op=ALU.mult), reads=[BXS, Bdt], writes=[BXDT])
                                for h in range(8):
                                    hs = slice(h * 64, (h + 1) * 64)
                                    S.op("act", lambda: ACT.activation(out=XE[:, hs], in_=XS[:, t, hs], func=AF.Identity, scale=sc[:, 4, h:h + 1]), reads=[BXS, Bsc], writes=[BXE], partial=(h > 0))
                                for h in range(8):
                                    hb, hq = h // 4, h % 4
                                    S.op("act", lambda: ACT.activation(out=LTl[:, h * 128:(h + 1) * 128], in_=pl[hb][:, hq * 128:(hq + 1) * 128], func=AF.Exp, bias=sc[:, 0, h:h + 1]),
                                         reads=[Bpl[hb], Bsc], writes=[BLTl], partial=(h > 0))
                                for g in range(2):
                                    S.op("dve", lambda: V.tensor_tensor(out=MTl[:, g * 512:(g + 1) * 512].rearrange("p (a c) -> p a c", a=4),
                                                                        in0=pcb[:, g * 128:(g + 1) * 128].unsqueeze(1).to_broadcast([128, 4, 128]),
                                                                        in1=LTl[:, g * 512:(g + 1) * 512].rearrange("p (a c) -> p a c", a=4), op=ALU.mult),
                                         reads=[Bpcb, BLTl], writes=[BMTl], partial=(g > 0))
                                py, Bpy = ps[2], Bps[2]; po, Bpo = ps[3], Bps[3]; pst_, Bpst = ps[4], Bps[4]
                                for h in range(8):
                                    g = h // 4
                                    hs = slice(h * 64, (h + 1) * 64)
                                    mm(po[:, hs], CT[:, g, tsl], carb[d][:, hs], [BCT, Bcarb[d]], Bpo, h == 0, True, start=True)
                                for h in range(8):
                                    g = h // 4
                                    hs = slice(h * 64, (h + 1) * 64)
                                    mm(pst_[:, hs], Btok[:, t, g, :], XE[:, hs], [BBtok, BXE], Bpst, h == 0, True, start=True)
                                for h in range(8):
                                    hs = slice(h * 64, (h + 1) * 64)
                                    mm(py[:, hs], MTl[:, h * 128:(h + 1) * 128], XDT[:, hs], [BMTl, BXDT], Bpy, h == 0, True, start=True)
                                S.op("dve", lambda: V.tensor_tensor(out=v3(car[d]), in0=v3(car[d]), in1=bc8(sc[:, 3, :]), op=ALU.mult), reads=[Bcar[d], Bsc], writes=[Bcar[d]])
                                S.op("dve", lambda: V.tensor_tensor(out=car[d], in0=pst_, in1=car[d], op=ALU.add), reads=[Bpst, Bcar[d]], writes=[Bcar[d]])
                                S.op("act", lambda: ACT.copy(out=carb[d], in_=car[d]), reads=[Bcar[d]], writes=[Bcarb[d]])
                                S.op("dve", lambda: V.tensor_tensor(out=v3(tY), in0=v3(po), in1=bc8(sc[:, 1, :]), op=ALU.mult), reads=[Bpo, Bsc], writes=[BtY])
                                S.op("pool", lambda: GP.tensor_tensor(out=Yacc[:, t, :], in0=Yacc[:, t, :], in1=tY, op=ALU.add), reads=[BtY, BY[t]], writes=[BY[t]])
                                S.op("dve", lambda: V.tensor_tensor(out=Yacc[:, t, :], in0=py, in1=Yacc[:, t, :], op=ALU.add), reads=[Bpy, BY[t]], writes=[BY[t]])
                        if l == 0 and b == 0:
                            dump("Yacc", Yacc, BY[0])
                            S.barrier()
                        zt = [A(st, "zt%d" % i, [128, 512], BF16) for i in range(2)]; Bzt = [gbuf("zt%d" % i) for i in range(2)]
                        Gt = A(st, "Gt", [128, 512], F32); BGt = Buf("Gt")
                        Gq = A(st, "Gq", [128, 512], F32); BGq = Buf("Gq")
                        ss = A(st, "ss", [128, 2], F32); Bss = Buf("ss")
                        stok = [A(st, "stok%d" % i, [128, 512], BF16) for i in range(2)]; Bstok = [Buf("stok%d" % i) for i in range(2)]
                        nw = prm[:, P_NW:P_NW + 512]
                        tlist = list(range(NT)) if not last else list(range(16))
                        for t in tlist:
                            z_, Bz_ = zt[t % 2], Bzt[t % 2]
                            S.dma("sp", z_, ZS[b][:, t, :], reads=[BZS[b]], writes=[Bz_])
                            S.op("dve", lambda: V.tensor_tensor(out=Gt, in0=Yacc[:, t, :], in1=z_, op=ALU.mult), reads=[BY[t], Bz_], writes=[BGt])
                            for g in range(2):
                                S.op("act", lambda: ACT.activation(out=Gq[:, g * 256:(g + 1) * 256], in_=Gt[:, g * 256:(g + 1) * 256], func=AF.Square, accum_out=ss[:, g:g + 1]),
                                     reads=[BGt], writes=[BGq, Bss], partial=(g > 0))
                            S.op("act", lambda: ACT.activation(out=ss, in_=ss, func=AF.Sqrt, bias=1e-5, scale=1.0 / 256), reads=[Bss], writes=[Bss])
                            S.op("dve", lambda: V.reciprocal(out=ss, in_=ss), reads=[Bss], writes=[Bss])
                            s_, Bs_ = stok[t % 2], Bstok[t % 2]
                            for g in range(2):
                                S.op("dve", lambda: V.scalar_tensor_tensor(out=s_[:, g * 256:(g + 1) * 256], in0=Gt[:, g * 256:(g + 1) * 256], scalar=ss[:, g:g + 1],
                                                                           in1=nw[:, g * 256:(g + 1) * 256], op0=ALU.mult, op1=ALU.mult),
                                     reads=[BGt, Bss, Bprm], writes=[Bs_], partial=(g > 0))
                            pa, pb_ = P()
                            pab = pa.bitcast(BF16)
                            for i in range(4):
                                S.op("pe", lambda: PE.transpose(out=pab[:, i * 128:(i + 1) * 128], in_=s_[:, i * 128:(i + 1) * 128], identity=identb),
                                     reads=[Bs_, Bidb], writes=[pb_], partial=(i > 0))
                            S.op("act", lambda: ACT.copy(out=cats[:, :, t * 128:(t + 1) * 128], in_=pab[:, 0:512].rearrange("p (a c) -> p a c", a=4)),
                                 reads=[pb_], writes=[Bcats], partial=True)
                        S.dma("pool", CAT[b][:, 4:8, :], cats, reads=[Bcats], writes=[BCAT[b]], partial=True, semb=Bcats)
                        S.barrier()

                with ExitStack() as st:
                    tab = [A(st, "tab%d" % i, [128, 25, 128], F32) for i in range(2)]; Btab = [gbuf("tab%d" % i) for i in range(2)]
                    qh = [A(st, "qh%d" % i, [64, T], BF16) for i in range(2)]; Bqh = [gbuf("qh%d" % i) for i in range(2)]
                    kh = [A(st, "kh%d" % i, [64, T], BF16) for i in range(2)]; Bkh = [gbuf("kh%d" % i) for i in range(2)]
                    vh = [A(st, "vh%d" % i, [128, NT, 65], BF16) for i in range(2)]; Bvh = [gbuf("vh%d" % i) for i in range(2)]
                    atok = A(st, "atok", [128, NT, 512], BF16); Batok = [Buf("atok%d" % t) for t in range(NT)]
                    cata = A(st, "cata", [128, 4, T], BF16); Bcata = gbuf("cata")
                    e1 = [A(st, "e1%d" % i, [128, 640], F32) for i in range(3)]; Be1 = [Buf("e1%d" % i) for i in range(3)]
                    pT = [A(st, "pT%d" % i, [128, 896], BF16) for i in range(3)]; BpT = [Buf("pT%d" % i) for i in range(3)]
                    rinv = [A(st, "rinv%d" % i, [128, 1], F32) for i in range(3)]; Brinv = [Buf("rinv%d" % i) for i in range(3)]
                    def load_head(h):
                        hi = h % 2
                        c_, pb0 = h // 2, (h % 2) * 64
                        S.dma("sp", tab[hi], btab_d[l, h].rearrange("p (a c) -> p a c", a=25), writes=[Btab[hi]])
                        S.dma("sp", qh[hi], QT[b][pb0:pb0 + 64, c_, :], reads=[BQT[b]], writes=[Bqh[hi]])
                        S.dma("sp", kh[hi], KT[b][pb0:pb0 + 64, c_, :], reads=[BKT[b]], writes=[Bkh[hi]])
                        S.dma("sp", vh[hi], VV[b][:, :, h * 65:(h + 1) * 65], reads=[BVV[b]], writes=[Bvh[hi]])

                    jl = list(range(16)) + ([] if last else [16, 17])
                    aitems = [(h, j) for h in range(8) for j in jl]

                    def QKE(idx):
                        h, j = aitems[idx]
                        hi = h % 2
                        i = idx % 3
                        qs = qh[hi][:, j * 128:(j + 1) * 128]
                        p1, Bp1 = ps[(idx % 2) * 2], Bps[(idx % 2) * 2]
                        p2, Bp2 = ps[(idx % 2) * 2 + 1], Bps[(idx % 2) * 2 + 1]
                        if j < 16:
                            ty = 0 if j == 0 else 1 if j == 1 else 3 if j == 14 else 4 if j == 15 else 2
                            base = min(max(j - 2, 0), 11)
                            for s in range(4):
                                kt = base + s
                                mm(p1[:, s * 128:(s + 1) * 128], kh[hi][:, kt * 128:(kt + 1) * 128], qs, [Bkh[hi], Bqh[hi]], Bp1, s == 0, True, start=True)
                            for s, kt in enumerate([base + 4, 16, 17]):
                                mm(p2[:, s * 128:(s + 1) * 128], kh[hi][:, kt * 128:(kt + 1) * 128], qs, [Bkh[hi], Bqh[hi]], Bp2, s == 0, True, start=True)
                            S.op("dve", lambda: V.tensor_tensor(out=e1[i][:, 0:512], in0=p1, in1=tab[hi][:, ty * 5:ty * 5 + 4, :].rearrange("p a c -> p (a c)"), op=ALU.add),
                                 reads=[Bp1, Btab[hi]], writes=[Be1[i]])
                            S.op("dve", lambda: V.tensor_tensor(out=e1[i][:, 512:640], in0=p2[:, 0:128], in1=tab[hi][:, ty * 5 + 4, :], op=ALU.add),
                                 reads=[Bp2, Btab[hi]], writes=[Be1[i]], partial=True)
                            S.op("act", lambda: ACT.activation(out=pT[i][:, 0:640], in_=e1[i], func=AF.Exp), reads=[Be1[i]], writes=[BpT[i]])
                            S.op("act", lambda: ACT.activation(out=pT[i][:, 640:896], in_=p2[:, 128:384], func=AF.Exp), reads=[Bp2], writes=[BpT[i]], partial=True)
                        else:
                            for s, kt in enumerate([16, 17]):
                                mm(p2[:, s * 128:(s + 1) * 128], kh[hi][:, kt * 128:(kt + 1) * 128], qs, [Bkh[hi], Bqh[hi]], Bp2, s == 0, True, start=True)
                            S.op("act", lambda: ACT.activation(out=pT[i][:, 0:256], in_=p2[:, 0:256], func=AF.Exp), reads=[Bp2], writes=[BpT[i]])

                    def PVN(idx):
                        h, j = aitems[idx]
                        hi = h % 2
                        i = idx % 3
                        po, Bpo = ps[4 + idx % 4], Bps[4 + idx % 4]
                        if j < 16:
                            base = min(max(j - 2, 0), 11)
                            kts = [base + s for s in range(5)] + [16, 17]
                        else:
                            kts = [16, 17]
                        mmacc(po[:, 0:65], [(pT[i][:, s * 128:(s + 1) * 128], vh[hi][:, kt, :]) for s, kt in enumerate(kts)], [BpT[i], Bvh[hi]], Bpo)
                        S.op("dve", lambda: V.reciprocal(out=rinv[i], in_=po[:, 64:65]), reads=[Bpo], writes=[Brinv[i]])
                        S.op("dve", lambda: V.tensor_scalar(out=atok[:, j, h * 64:(h + 1) * 64], in0=po[:, 0:64], scalar1=rinv[i], scalar2=None, op0=ALU.mult),
                             reads=[Bpo, Brinv[i]], writes=[Batok[j]], partial=True)

                    load_head(0)
                    load_head(1)
                    QKE(0)
                    for idx in range(len(aitems)):
                        if idx + 1 < len(aitems):
                            QKE(idx + 1)
                        PVN(idx)
                        h, j = aitems[idx]
                        if j == jl[0] and 1 <= h <= 6:
                            load_head(h + 1)
                    tl = list(range(NT)) if not last else list(range(16))
                    for t in tl:
                        pa, pb_ = P()
                        pab = pa.bitcast(BF16)
                        for i in range(4):
                            S.op("pe", lambda: PE.transpose(out=pab[:, i * 128:(i + 1) * 128], in_=atok[:, t, i * 128:(i + 1) * 128], identity=identb),
                                 reads=[Batok[t], Bidb], writes=[pb_], partial=(i > 0))
                        S.op("act", lambda: ACT.copy(out=cata[:, :, t * 128:(t + 1) * 128], in_=pab[:, 0:512].rearrange("p (a c) -> p a c", a=4)),
                             reads=[pb_], writes=[Bcata], partial=True)
                    S.dma("pool", CAT[b][:, 0:4, :], cata, reads=[Bcata], writes=[BCAT[b]], partial=True, semb=Bcata)
                    S.barrier()

                blks = BLKS if not last else BLKS[:4]
                Teff = T if not last else L

                def layernorm(st_tiles, Y, BY_, n, which, outs):
                    Ysq, BYsq, mean, Bmean, msq, Bmsq, rstd, Brstd, tmp, Btmp = st_tiles
                    p1, Bp1 = P()
                    mmacc(p1[:, 0:n], [(ones, Y[:, k, 0:n]) for k in range(8)], [Bcst, BY_], Bp1)
                    S.op("act", lambda: ACT.activation(out=Ysq[:, :, 0:n], in_=Y[:, :, 0:n], func=AF.Square), reads=[BY_], writes=[BYsq])
                    p2, Bp2 = P()
                    mmacc(p2[:, 0:n], [(ones, Ysq[:, k, 0:n]) for k in range(8)], [Bcst, BYsq], Bp2)
                    S.op("act", lambda: ACT.activation(out=mean[:, 0:n], in_=p1[:, 0:n], func=AF.Copy, scale=1.0 / 1024), reads=[Bp1], writes=[Bmean])
                    S.op("act", lambda: ACT.activation(out=msq[:, 0:n], in_=p1[:, 0:n], func=AF.Square, scale=1.0 / 1024), reads=[Bp1], writes=[Bmsq])
                    S.op("dve", lambda: V.scalar_tensor_tensor(out=rstd[:, 0:n], in0=p2[:, 0:n], scalar=1.0 / 1024, in1=msq[:, 0:n], op0=ALU.mult, op1=ALU.subtract),
                         reads=[Bp2, Bmsq], writes=[Brstd])
                    S.op("act", lambda: ACT.activation(out=rstd[:, 0:n], in_=rstd[:, 0:n], func=AF.Sqrt, bias=1e-5), reads=[Brstd], writes=[Brstd])
                    S.op("dve", lambda: V.reciprocal(out=rstd[:, 0:n], in_=rstd[:, 0:n]), reads=[Brstd], writes=[Brstd])
                    for k in range(8):
                        S.op("pool", lambda: GP.tensor_tensor(out=tmp[:, k, 0:n], in0=Y[:, k, 0:n], in1=mean[:, 0:n], op=ALU.subtract), reads=[BY_, Bmean], writes=[Btmp], partial=(k > 0))
                    for k in range(8):
                        S.op("dve", lambda: V.tensor_tensor(out=tmp[:, k, 0:n], in0=tmp[:, k, 0:n], in1=rstd[:, 0:n], op=ALU.mult), reads=[Btmp, Brstd], writes=[Btmp], partial=(k > 0))
                    gcol = P_LNG + which * 8
                    bcol = P_LNB + which * 8
                    for k in range(8):
                        S.op("act", lambda: ACT.activation(out=tmp[:, k, 0:n], in_=tmp[:, k, 0:n], func=AF.Identity, scale=prm[:, gcol + k:gcol + k + 1], bias=prm[:, bcol + k:bcol + k + 1]),
                             reads=[Btmp, Bprm], writes=[Btmp], partial=(k > 0))
                    return tmp, Btmp

                with ExitStack() as pd:
                    h2T = A(pd, "h2T", [128, 8, T], BF16); Bh2 = Buf("h2T")
                    combT = A(pd, "combT", [8, T], F32); BcombT = Buf("combT")
                    with ExitStack() as st:
                        catb = [A(st, "catb%d" % i, [128, 8, 512], BF16) for i in range(2)]; Bcatb = [gbuf("catb%d" % i) for i in range(2)]
                        xtb = [A(st, "xtb0", [128, 8, 512], F32)] * 2; Bxtb = [gbuf("xtb0")] * 2
                        woutb = A(st, "woutb", [128, 8, 1024], BF16); Bwout = Buf("wout")
                        wo_s = [A(st, "wos%d" % i, [128, 8, 128], F32) for i in range(2)]; Bwo_s = [gbuf("wos%d" % i) for i in range(2)]
                        wov = wout_d[l].rearrange("(k p) n -> p k n", p=128)
                        for pc in range(8):
                            w_, Bw_ = wo_s[pc % 2], Bwo_s[pc % 2]
                            S.dma("sp" if pc % 2 == 0 else "pool", w_, wov[:, :, pc * 128:(pc + 1) * 128], writes=[Bw_])
                            S.op("act", lambda: ACT.copy(out=woutb[:, :, pc * 128:(pc + 1) * 128], in_=w_), reads=[Bw_], writes=[Bwout], partial=(pc > 0))
                        Y = A(st, "Y", [128, 8, 512], F32); BY_ = Buf("Y")
                        lnt = (A(st, "Ysq", [128, 8, 512], F32), Buf("Ysq"), A(st, "mean", [128, 512], F32), Buf("mean"), A(st, "msq", [128, 512], F32), Buf("msq"),
                               A(st, "rstd", [128, 512], F32), Buf("rstd"), A(st, "lntmp", [128, 8, 512], F32), gbuf("lntmp"))
                        h2f, Bh2f = lnt[0], lnt[1]
                        rtw = A(st, "rtw", [128, 8, 8], F32); Brtw = gbuf("rtw")
                        lg = A(st, "lg", [128, 8], F32); Blg = Buf("lg")
                        sm = A(st, "sm", [128, 8, 8], F32); Bsm = Buf("sm")
                        if moe:
                            S.dma("sp", rtw, rt_d[l // 2], writes=[Brtw])
                        for bi, (t0, n) in enumerate(blks):
                            j = mj(t0)
                            cb_, Bcb_ = catb[bi % 2], Bcatb[bi % 2]
                            xb_, Bxb_ = xtb[bi % 2], Bxtb[bi % 2]
                            S.dma("sp", cb_[:, :, 0:n], CAT[b][:, :, t0:t0 + n], reads=[BCAT[b]], writes=[Bcb_])
                            S.dma("pool", xb_[:, :, 0:n], XT[b][:, :, t0:t0 + n], reads=[BXT[b]], writes=[Bxb_])
                            for oc in range(8):
                                pa, pb_ = P()
                                mmacc(pa[:, 0:n], [(woutb[:, k, oc * 128:(oc + 1) * 128], cb_[:, k, 0:n]) for k in range(8)], [Bwout, Bcb_], pb_)
                                S.op("act", lambda: ACT.activation(out=Y[:, oc, 0:n], in_=pa[:, 0:n], func=AF.Identity, scale=modT[:, 16 + oc, j:j + 1]),
                                     reads=[pb_, Bmod], writes=[BY_], partial=(oc > 0))
                                S.op("dve", lambda: V.scalar_tensor_tensor(out=Y[:, oc, 0:n], in0=xb_[:, oc, 0:n], scalar=ALPHA, in1=Y[:, oc, 0:n], op0=ALU.mult, op1=ALU.add),
                                     reads=[Bxb_, BY_], writes=[BY_], partial=True)
                            x1, Bx1 = layernorm(lnt, Y, BY_, n, 0, None)
                            S.dma("pool", X1T[b][:, :, t0:t0 + n], x1[:, :, 0:n], reads=[Bx1], writes=[BX1T[b]], partial=True, semb=Bx1)
                            for k in range(8):
                                S.op("dve", lambda: V.tensor_scalar(out=h2f[:, k, 0:n], in0=x1[:, k, 0:n], scalar1=modT[:, 32 + k, j:j + 1], scalar2=modT[:, 24 + k, j:j + 1],
                                                                    op0=ALU.mult, op1=ALU.add), reads=[Bx1, Bmod], writes=[Bh2f], partial=(k > 0))
                            S.op("act", lambda: ACT.copy(out=h2T[:, :, t0:t0 + n], in_=h2f[:, :, 0:n]), reads=[Bh2f], writes=[Bh2], partial=True)
                            if moe:
                                for tt in range(n // 128):
                                    tsl = slice(tt * 128, (tt + 1) * 128)
                                    pa, pb_ = P()
                                    mmacc(pa[:, 0:8], [(h2f[:, k, tsl], rtw[:, k, :]) for k in range(8)], [Bh2f, Brtw], pb_)
                                    S.op("dve", lambda: V.tensor_copy(out=lg, in_=pa[:, 0:8]), reads=[pb_], writes=[Blg])
                                    m1, eq1, l2, m2, eq2, dd, g1, cmb = (sm[:, 0, 0:1], sm[:, 1, :], sm[:, 2, :], sm[:, 0, 1:2], sm[:, 3, :], sm[:, 0, 2:3], sm[:, 0, 3:4], sm[:, 4, :])
                                    g2 = sm[:, 0, 4:5]
                                    S.op("dve", lambda: V.reduce_max(out=m1, in_=lg, axis=AX.X), reads=[Blg], writes=[Bsm])
                                    S.op("dve", lambda: V.tensor_scalar(out=eq1, in0=lg, scalar1=m1, scalar2=None, op0=ALU.is_equal), reads=[Blg, Bsm], writes=[Bsm])
                                    S.op("dve", lambda: V.scalar_tensor_tensor(out=l2, in0=eq1, scalar=-1e30, in1=lg, op0=ALU.mult, op1=ALU.add), reads=[Blg, Bsm], writes=[Bsm])
                                    S.op("dve", lambda: V.reduce_max(out=m2, in_=l2, axis=AX.X), reads=[Bsm], writes=[Bsm])
                                    S.op("dve", lambda: V.tensor_scalar(out=eq2, in0=l2, scalar1=m2, scalar2=None, op0=ALU.is_equal), reads=[Bsm], writes=[Bsm])
                                    S.op("dve", lambda: V.tensor_tensor(out=dd, in0=m2, in1=m1, op=ALU.subtract), reads=[Bsm], writes=[Bsm])
                                    S.op("act", lambda: ACT.activation(out=dd, in_=dd, func=AF.Exp), reads=[Bsm], writes=[Bsm])
                                    S.op("dve", lambda: V.tensor_scalar(out=g1, in0=dd, scalar1=1.0, scalar2=None, op0=ALU.add), reads=[Bsm], writes=[Bsm])
                                    S.op("dve", lambda: V.reciprocal(out=g1, in_=g1), reads=[Bsm], writes=[Bsm])
                                    S.op("dve", lambda: V.tensor_tensor(out=g2, in0=dd, in1=g1, op=ALU.mult), reads=[Bsm], writes=[Bsm])
                                    S.op("dve", lambda: V.tensor_scalar(out=cmb, in0=eq1, scalar1=g1, scalar2=None, op0=ALU.mult), reads=[Bsm], writes=[Bsm])
                                    S.op("dve", lambda: V.scalar_tensor_tensor(out=cmb, in0=eq2, scalar=g2, in1=cmb, op0=ALU.mult, op1=ALU.add), reads=[Bsm], writes=[Bsm])
                                    pa2, pb2 = P()
                                    S.op("pe", lambda: PE.transpose(out=pa2[0:8, 0:128], in_=cmb, identity=ident), reads=[Bsm, Bcst], writes=[pb2])
                                    S.op("act", lambda: ACT.copy(out=combT[:, t0 + tt * 128:t0 + (tt + 1) * 128], in_=pa2[0:8, 0:128]), reads=[pb2], writes=[BcombT], partial=True)
                        S.barrier()
                    with ExitStack() as pd2:
                        facc = A(pd2, "facc", [128, 8, T], F32)
                        Bfacc = [[Buf("facc%d_%d" % (oc, bi)) for bi in range(5)] for oc in range(8)]
                        with ExitStack() as st:
                            wst = [A(st, "fst%d" % i, [128, 2048], F32) for i in range(2)]; Bwst = [gbuf("fst%d" % i) for i in range(2)]
                            wbf = [A(st, "fbf%d" % i, [128, 2048], BF16) for i in range(6)]; Bwbf = [Buf("fbf%d" % i) for i in range(6)]
                            actT = [A(st, "actT%d" % i, [128, 2, 512], BF16) for i in range(2)]; BactT = [Buf("actT%d" % i) for i in range(2)]
                            sg = [A(st, "sg%d" % i, [128, 512], F32) for i in range(2)]; Bsg = [Buf("sg%d" % i) for i in range(2)]
                            a32, Ba32 = sg, Bsg
                            cbc = [A(st, "cbc0", [128, T], F32)] * 2; Bcbc = [Buf("cbc0")] * 2
                            wc = [0, 0]
                            F = 3584 if moe else 2816
                            nfg = F // 256
                            li = l // 2
                            experts = list(range(8)) if moe else [0]

                            def wsrc(e, fg):
                                if moe:
                                    g_ = mg_d[li, e].rearrange("(k p) n -> p k n", p=128)[:, :, fg * 256:(fg + 1) * 256]
                                    u_ = mu_d[li, e].rearrange("(k p) n -> p k n", p=128)[:, :, fg * 256:(fg + 1) * 256]
                                    d_ = md_d[li, e, fg * 256:(fg + 1) * 256, :].rearrange("(k p) n -> p k n", p=128)
                                else:
                                    g_ = fg_d[li].rearrange("(k p) n -> p k n", p=128)[:, :, fg * 256:(fg + 1) * 256]
                                    u_ = fu_d[li].rearrange("(k p) n -> p k n", p=128)[:, :, fg * 256:(fg + 1) * 256]
                                    d_ = fd_d[li, fg * 256:(fg + 1) * 256, :].rearrange("(k p) n -> p k n", p=128)
                                return g_, u_, d_

                            def getw3(e, fg):
                                res = []
                                for wi, src in enumerate(wsrc(e, fg)):
                                    si = wc[0] % 2; wc[0] += 1
                                    bi_ = wc[1] % 6; wc[1] += 1
                                    kk = 8 if wi < 2 else 2
                                    sv = wst[si].rearrange("p (k n) -> p k n", k=kk)
                                    bv = wbf[bi_].rearrange("p (k n) -> p k n", k=kk)
                                    S.dma("sp", sv, src, writes=[Bwst[si]])
                                    if wi == 1:
                                        S.op("act", lambda: ACT.copy(out=wbf[bi_], in_=wst[si]), reads=[Bwst[si]], writes=[Bwbf[bi_]])
                                    else:
                                        S.op("pool", lambda: GP.tensor_copy(out=wbf[bi_], in_=wst[si]), reads=[Bwst[si]], writes=[Bwbf[bi_]])
                                    res.append((bv, Bwbf[bi_]))
                                return res

                            pend = {}

                            def conv(wi, si, bi_):
                                if wi == 1:
                                    S.op("act", lambda: ACT.copy(out=wbf[bi_], in_=wst[si]), reads=[Bwst[si]], writes=[Bwbf[bi_]])
                                else:
                                    S.op("pool", lambda: GP.tensor_copy(out=wbf[bi_], in_=wst[si]), reads=[Bwst[si]], writes=[Bwbf[bi_]])

                            def stageA(w2):
                                srcs = wsrc(*work[w2])
                                plan = []
                                for wi in range(3):
                                    si = wc[0] % 2; wc[0] += 1
                                    bi_ = wc[1] % 6; wc[1] += 1
                                    plan.append((si, bi_, 8 if wi < 2 else 2, srcs[wi]))
                                pend[w2] = plan
                                for wi in range(2):
                                    si, bi_, kk, src = plan[wi]
                                    S.dma("sp", wst[si].rearrange("p (k n) -> p k n", k=kk), src, writes=[Bwst[si]])
                                wts[w2] = [(wbf[p_[1]].rearrange("p (k n) -> p k n", k=p_[2]), Bwbf[p_[1]]) for p_ in plan]

                            def stageB(w2):
                                plan = pend[w2]
                                for wi in range(2):
                                    conv(wi, plan[wi][0], plan[wi][1])
                                si, bi_, kk, src = plan[2]
                                S.dma("sp", wst[si].rearrange("p (k n) -> p k n", k=kk), src, writes=[Bwst[si]])

                            def stageC(w2):
                                si, bi_, kk, src = pend.pop(w2)[2]
                                conv(2, si, bi_)

                            work = [(e, fg) for e in experts for fg in range(nfg)]
                            items = [(wi_, bi) for wi_ in range(len(work)) for bi in range(len(blks))]
                            wts = {0: getw3(*work[0])}
                            if len(work) > 1:
                                wts[1] = getw3(*work[1])
                            dctr = [0]
                            gctr = [0]

                            def GU(idx):
                                wi_, bi = items[idx]
                                e, fg = work[wi_]
                                t0, n = blks[bi]
                                (wg, Bwg), (wu, Bwu), _ = wts[wi_]
                                cb_e, Bcb_e = cbc[0], Bcbc[0]
                                if moe and fg == 0 and bi == 0:
                                    for (t0c, nc_) in blks:
                                        pa, pb_ = P()
                                        mm(pa[:, 0:nc_], sel[0:8, e, :], combT[:, t0c:t0c + nc_], [Bcst, BcombT], pb_, True, True)
                                        S.op("act", lambda: ACT.copy(out=cb_e[:, t0c:t0c + nc_], in_=pa[:, 0:nc_]), reads=[pb_], writes=[Bcb_e], partial=(t0c > 0))
                                a_, Ba_ = actT[idx % 2], BactT[idx % 2]
                                for fc in range(2):
                                    pg, Bpg = P()
                                    pu, Bpu = P()
                                    mmacc(pg[:, 0:n], [(wg[:, k, fc * 128:(fc + 1) * 128], h2T[:, k, t0:t0 + n]) for k in range(8)], [Bwg, Bh2], Bpg)
                                    mmacc(pu[:, 0:n], [(wu[:, k, fc * 128:(fc + 1) * 128], h2T[:, k, t0:t0 + n]) for k in range(8)], [Bwu, Bh2], Bpu)
                                    s_, Bs_ = sg[fc], Bsg[fc]
                                    S.op("act", lambda: ACT.activation(out=s_[:, 0:n], in_=pg[:, 0:n], func=AF.Silu), reads=[Bpg], writes=[Bs_])
                                    if moe:
                                        S.op("dve", lambda: V.tensor_tensor(out=s_[:, 0:n], in0=pu[:, 0:n], in1=s_[:, 0:n], op=ALU.mult), reads=[Bpu, Bs_], writes=[Bs_])
                                        S.op("pool", lambda: GP.tensor_tensor(out=a_[:, fc, 0:n], in0=s_[:, 0:n], in1=cb_e[:, t0:t0 + n], op=ALU.mult),
                                             reads=[Bs_, Bcb_e], writes=[Ba_], partial=(fc > 0))
                                    else:
                                        S.op("dve", lambda: V.tensor_tensor(out=a_[:, fc, 0:n], in0=pu[:, 0:n], in1=s_[:, 0:n], op=ALU.mult), reads=[Bpu, Bs_], writes=[Ba_], partial=(fc > 0))

                            def DN(idx):
                                wi_, bi = items[idx]
                                t0, n = blks[bi]
                                _, _, (wd, Bwd) = wts[wi_]
                                a_, Ba_ = actT[idx % 2], BactT[idx % 2]
                                for oc in range(8):
                                    pd_, Bpd_ = P()
                                    mmacc(pd_[:, 0:n], [(wd[:, fc, oc * 128:(oc + 1) * 128], a_[:, fc, 0:n]) for fc in range(2)], [Bwd, Ba_], Bpd_)
                                    if wi_ == 0:
                                        S.op("act", lambda: ACT.copy(out=facc[:, oc, t0:t0 + n], in_=pd_[:, 0:n]), reads=[Bpd_], writes=[Bfacc[oc][bi]])
                                    else:
                                        S.op("dve", lambda: V.tensor_tensor(out=facc[:, oc, t0:t0 + n], in0=pd_[:, 0:n], in1=facc[:, oc, t0:t0 + n], op=ALU.add),
                                             reads=[Bpd_, Bfacc[oc][bi]], writes=[Bfacc[oc][bi]])

                            GU(0)
                            for idx in range(len(items)):
                                wi_, bi = items[idx]
                                if idx + 1 < len(items):
                                    GU(idx + 1)
                                DN(idx)
                                if bi == 1 and (wi_ + 1) in pend:
                                    stageB(wi_ + 1)
                                if bi == 3 and (wi_ + 1) in pend:
                                    stageC(wi_ + 1)
                                if bi == len(blks) - 1:
                                    wts.pop(wi_)
                                    if wi_ + 2 < len(work):
                                        stageA(wi_ + 2)
                            S.barrier()
                        with ExitStack() as st:
                            x1b = [A(st, "x1b%d" % i, [128, 8, 256], F32) for i in range(2)]; Bx1b = [gbuf("x1b%d" % i) for i in range(2)]
                            Y = A(st, "Y2", [128, 8, 256], F32); BY_ = Buf("Y2")
                            lnt = (A(st, "Ysq2", [128, 8, 256], F32), Buf("Ysq2"), A(st, "mean2", [128, 256], F32), Buf("mean2"), A(st, "msq2", [128, 256], F32), Buf("msq2"),
                                   A(st, "rstd2", [128, 256], F32), Buf("rstd2"), A(st, "lntmp2", [128, 8, 256], F32), gbuf("lntmp2"))
                            ot = [A(st, "ot%d" % i, [128, 1024], F32) for i in range(2)]; Bot = [gbuf("ot%d" % i) for i in range(2)]
                            oi = 0
                            for bi3 in range(Teff // 256):
                                t0, n = bi3 * 256, 256
                                bi = t0 // 512
                                j = mj(t0)
                                xb_, Bxb_ = x1b[bi3 % 2], Bx1b[bi3 % 2]
                                S.dma("pool", xb_[:, :, 0:n], X1T[b][:, :, t0:t0 + n], reads=[BX1T[b]], writes=[Bxb_])
                                for oc in range(8):
                                    S.op("act", lambda: ACT.activation(out=Y[:, oc, 0:n], in_=facc[:, oc, t0:t0 + n], func=AF.Identity, scale=modT[:, 40 + oc, j:j + 1]),
                                         reads=[Bfacc[oc][bi], Bmod], writes=[BY_], partial=(oc > 0))
                                    S.op("dve", lambda: V.scalar_tensor_tensor(out=Y[:, oc, 0:n], in0=xb_[:, oc, 0:n], scalar=ALPHA, in1=Y[:, oc, 0:n], op0=ALU.mult, op1=ALU.add),
                                         reads=[Bxb_, BY_], writes=[BY_], partial=True)
                                xn, Bxn = layernorm(lnt, Y, BY_, n, 1, None)
                                if not last:
                                    S.dma("pool", XT[b][:, :, t0:t0 + n], xn[:, :, 0:n], reads=[Bxn], writes=[BXT[b]], partial=True, semb=Bxn)
                                else:
                                    for tt in range(n // 128):
                                        o_, Bo_ = ot[oi % 2], Bot[oi % 2]; oi += 1
                                        for hh in range(2):
                                            pa, pb_ = P()
                                            for k4 in range(4):
                                                k = hh * 4 + k4
                                                S.op("pe", lambda: PE.transpose(out=pa[:, k4 * 128:(k4 + 1) * 128], in_=xn[:, k, tt * 128:(tt + 1) * 128], identity=ident),
                                                     reads=[Bxn, Bcst], writes=[pb_], partial=(k4 > 0))
                                            if hh:
                                                S.op("act", lambda: ACT.copy(out=o_[:, hh * 512:(hh + 1) * 512], in_=pa), reads=[pb_], writes=[Bo_], partial=True)
                                            else:
                                                S.op("dve", lambda: V.tensor_copy(out=o_[:, hh * 512:(hh + 1) * 512], in_=pa), reads=[pb_], writes=[Bo_])
                                        S.dma("sp", out_d[b, t0 + tt * 128:t0 + (tt + 1) * 128, :], o_, reads=[Bo_], writes=[Bout], partial=True, semb=Bo_)
                            S.barrier()
        S.barrier()
    assert S.nsem < 100, S.nsem
    return nc, S


def _consts():
    c = np.zeros((128, NCONST), np.float32)
    s = np.arange(128)[:, None]; l_ = np.arange(128)[None, :]
    c[:, C_ID:C_ID + 128] = np.eye(128)
    c[:, C_ONE:C_ONE + 128] = 1.0
    c[:, C_TRF:C_TRF + 128] = (s <= l_)
    c[:, C_TRB:C_TRB + 128] = (s >= l_)
    c[:, C_MNF:C_MNF + 128] = np.where(s <= l_, 0.0, NEG)
    c[:, C_MNB:C_MNB + 128] = np.where(s >= l_, 0.0, NEG)
    pmat = np.zeros((128, 128), np.float32)
    for base in (0, 64):
        for i in range(32):
            pmat[base + 32 + i, base + i] = -1.0
            pmat[base + i, base + 32 + i] = 1.0
    c[:, C_PM:C_PM + 128] = pmat
    selm = np.zeros((128, 8, 128), np.float32)
    for e in range(8):
        selm[e, e, :] = 1.0
    c[:, C_SEL:C_SEL + 1024] = selm.reshape(128, 1024)
    c[:, C_MN4F:C_MN4F + 512] = np.tile(c[:, C_MNF:C_MNF + 128], (1, 4))
    c[:, C_MN4B:C_MN4B + 512] = np.tile(c[:, C_MNB:C_MNB + 128], (1, 4))
    return c


def _rope():
    t = np.arange(L)
    inv = (10000.0 ** (-np.arange(32, dtype=np.float32) / 32)).astype(np.float32)
    ang_r = (t // 64).astype(np.float32)[:, None] * inv
    ang_c = (t % 64).astype(np.float32)[:, None] * inv
    cos = np.zeros((128, L), np.float32); sin = np.zeros((128, L), np.float32)
    for base, ang in ((0, ang_r), (64, ang_c)):
        cos[base:base + 32] = np.cos(ang).T; cos[base + 32:base + 64] = np.cos(ang).T
        sin[base:base + 32] = np.sin(ang).T; sin[base + 32:base + 64] = np.sin(ang).T
    return np.ascontiguousarray(np.stack([cos, sin], axis=1))


def _bias_gather_index():
    types_j = [0, 1, 2, 14, 15]
    valid = np.zeros((5, 5, 128, 128), bool)
    ridx = np.zeros((5, 5, 128, 128), np.int64)
    cidx = np.zeros((5, 5, 128, 128), np.int64)
    key = np.arange(128); q = np.arange(128)
    kr_l, kc = key // 64, key % 64
    qr_l, qc = q // 64, q % 64
    for ti, j in enumerate(types_j):
        base = min(max(j - 2, 0), 11)
        for s in range(5):
            kr = 2 * (base + s) + kr_l
            qi = 2 * j + qr_l
            r0 = np.clip(qi - 4, 0, 24)
            cs = np.clip(qc - 8, 0, 48)
            vr = (kr[:, None] >= r0[None, :]) & (kr[:, None] < r0[None, :] + 8)
            vc = (kc[:, None] >= cs[None, :]) & (kc[:, None] < cs[None, :] + 16)
            valid[ti, s] = vr & vc
            ridx[ti, s] = np.clip(kr[:, None] - qi[None, :] + 7, 0, 14)
            cidx[ti, s] = np.clip(kc[:, None] - qc[None, :] + 15, 0, 30)
    return valid, ridx, cidx


def _prep_shared(inp):
    f = lambda a: np.ascontiguousarray(np.asarray(a, dtype=np.float32))
    valid, ridx, cidx = _bias_gather_index()
    rpb = f(inp["na_rpb"])
    g = rpb[:, :, ridx, cidx]
    g = np.where(valid[None, None], g, np.float32(NEG)).astype(np.float32)
    btab = np.ascontiguousarray(g.transpose(0, 1, 4, 2, 3, 5)).reshape(4, 8, 128, 25 * 128)
    prm = np.zeros((4, 128, NP_), np.float32)
    bm = f(inp["b_mod"]).reshape(4, 48, 128).transpose(0, 2, 1)
    prm[:, :, P_BMOD:P_BMOD + 48] = bm
    cw = f(inp["conv_w"]).reshape(4, 5, 8, 128).transpose(0, 3, 2, 1)
    prm[:, :, P_CW:P_CW + 40] = cw.reshape(4, 128, 40)
    prm[:, :, P_CB:P_CB + 8] = f(inp["conv_b"]).reshape(4, 8, 128).transpose(0, 2, 1)
    prm[:, :, P_DTB:P_DTB + 16] = f(inp["dt_bias"]).reshape(4, 1, 16)
    prm[:, :, P_ALOG:P_ALOG + 16] = f(inp["a_log"]).reshape(4, 1, 16)
    prm[:, :, P_DSK:P_DSK + 8] = f(inp["d_skip"]).reshape(4, 1, 8)
    prm[:, :, P_NW:P_NW + 512] = f(inp["ssd_norm_w"]).reshape(4, 1, 512)
    prm[:, :, P_LNG:P_LNG + 16] = f(inp["ln_g"]).reshape(4, 2, 8, 128).transpose(0, 3, 1, 2).reshape(4, 128, 16)
    prm[:, :, P_LNB:P_LNB + 16] = f(inp["ln_b"]).reshape(4, 2, 8, 128).transpose(0, 3, 1, 2).reshape(4, 128, 16)
    router = np.ascontiguousarray(f(inp["router_w"]).reshape(2, 8, 128, 8).transpose(0, 2, 1, 3))
    shared = {"consts": _consts(), "rope": _rope(), "prm": prm, "btab": btab, "router": router}
    for k in ("w_mod", "w_in", "w_out", "ffn_w_gate", "ffn_w_up", "ffn_w_down", "moe_w_gate", "moe_w_up", "moe_w_down"):
        shared[k] = f(inp[k])
    return shared


def _core_inputs(inp, shared, core):
    f = lambda a: np.ascontiguousarray(np.asarray(a, dtype=np.float32))
    b0 = core * NB
    c2 = f(inp["c"])[b0:b0 + NB]
    cc = np.concatenate([c2, f(inp["c_ctx"])[None]], axis=0)
    cT = np.ascontiguousarray(cc.reshape(3, 8, 128).transpose(2, 1, 0))
    m = dict(shared)
    m["x"] = f(inp["x"])[b0:b0 + NB]
    m["ctx"] = f(inp["ctx"])[b0:b0 + NB]
    m["cT"] = cT
    return m


_CACHE = {}


def kernel(**inputs):
    if "nc" not in _CACHE:
        _CACHE["nc"] = build(4)[0]
    nc = _CACHE["nc"]
    shared = _prep_shared(inputs)
    n_cores = 8
    in_maps = [_core_inputs(inputs, shared, c) for c in range(n_cores)]
    res = run_bass_kernel_spmd(nc, in_maps, core_ids=list(range(n_cores)))
    out = np.concatenate([np.asarray(r["out"]) for r in res.results], axis=0)
    return out.astype(np.float32)
```
